# Optimizing a Trainium2 kernel written in Bass

```python
import jax, jax.numpy as jnp
from jax import lax
import numpy as np

D_MODEL = 1024
BATCH = 16
SEQ = 4096
DEPTH = 4

GRID_W = 64
CTX_LEN = 256
CHUNK = 128
DA = 2 * D_MODEL
GA = 16
HEAD_DIM = 64
N_HEADS = D_MODEL // HEAD_DIM
N_KV_HEADS = N_HEADS // 4
Q_PER_KV = N_HEADS // N_KV_HEADS
WINDOW = 128
BLOCK = 128
ROPE_THETA = 10000.0
ROPE_PAIRS_PER_AXIS = HEAD_DIM // 4
N_EXPERTS = 16
CAPACITY_FACTOR = 2
D_FF_EXPERT = 2 * D_MODEL
EPS = 1e-6
N_A_LAYERS = (DEPTH + 1) // 2
N_B_LAYERS = DEPTH // 2

kernel_name = "hybrid_gmlp_swa_ecmoe_dit"


def rmsnorm(x, g):
    xf = x.astype(jnp.float32)
    y = xf * lax.rsqrt(jnp.mean(xf * xf, axis=-1, keepdims=True) + EPS)
    return (y * g.astype(jnp.float32)).astype(x.dtype)


def layernorm(x, g, b):
    xf = x.astype(jnp.float32)
    mu = jnp.mean(xf, axis=-1, keepdims=True)
    var = jnp.mean(jnp.square(xf - mu), axis=-1, keepdims=True)
    y = (xf - mu) * lax.rsqrt(var + EPS)
    return (y * g.astype(jnp.float32) + b.astype(jnp.float32)).astype(x.dtype)


def axial_rope(x, cos, sin):
    half = x.shape[-1] // 2
    shp = (x.shape[1],) + (1,) * (x.ndim - 3) + (half,)
    cs = cos.reshape(shp)
    sn = sin.reshape(shp)
    x1, x2 = x[..., :half], x[..., half:]
    return jnp.concatenate([x1 * cs - x2 * sn, x1 * sn + x2 * cs], axis=-1)


def chunk_gmlp(h, w_in, b_in, ln_g, ln_b, w_s, b_s, w_out):
    B, N, _ = h.shape
    z = jax.nn.gelu(h @ w_in + b_in)
    u, v = z[..., :DA], z[..., DA:]
    v = layernorm(v, ln_g, ln_b)
    vc = v.reshape(B, N // CHUNK, CHUNK, GA, DA // GA)
    sv = jnp.einsum('gpq,bnqgc->bnpgc', w_s, vc) + jnp.swapaxes(b_s, 0, 1)[None, None, :, :, None]
    return (u * sv.reshape(B, N, DA)) @ w_out


def _project_qkv(h, w_qkv):
    B, N, _ = h.shape
    qkv = h @ w_qkv
    nq = N_HEADS * HEAD_DIM
    nk = N_KV_HEADS * HEAD_DIM
    q = qkv[..., :nq].reshape(B, N, N_KV_HEADS, Q_PER_KV, HEAD_DIM)
    k = qkv[..., nq:nq + nk].reshape(B, N, N_KV_HEADS, HEAD_DIM)
    v = qkv[..., nq + nk:].reshape(B, N, N_KV_HEADS, HEAD_DIM)
    return q, k, v


def windowed_gqa(h_lat, h_ctx, w_qkv, sink, w_o, cos, sin, need_ctx_out):
    B, L, _ = h_lat.shape
    Lc = h_ctx.shape[1]
    dt = h_lat.dtype
    scale = HEAD_DIM ** -0.5
    q_l, k_l, v_l = _project_qkv(h_lat, w_qkv)
    q_c, k_c, v_c = _project_qkv(h_ctx, w_qkv)
    q_l = axial_rope(q_l, cos, sin) * scale
    k_l = axial_rope(k_l, cos, sin)
    sink_f = sink.astype(jnp.float32).reshape(N_KV_HEADS, Q_PER_KV)

    nb = L // BLOCK
    qb = q_l.reshape(B, nb, BLOCK, N_KV_HEADS, Q_PER_KV, HEAD_DIM)
    pad = ((0, 0), (BLOCK, BLOCK), (0, 0), (0, 0))
    kp = jnp.pad(k_l, pad).reshape(B, nb + 2, BLOCK, N_KV_HEADS, HEAD_DIM)
    vp = jnp.pad(v_l, pad).reshape(B, nb + 2, BLOCK, N_KV_HEADS, HEAD_DIM)
    kw = jnp.concatenate([kp[:, :-2], kp[:, 1:-1], kp[:, 2:]], axis=2)
    vw = jnp.concatenate([vp[:, :-2], vp[:, 1:-1], vp[:, 2:]], axis=2)
    iq = jnp.arange(BLOCK)[:, None]
    jk = jnp.arange(3 * BLOCK)[None, :]
    band = jnp.abs(jk - BLOCK - iq) <= WINDOW
    pos = jnp.arange(nb)[:, None] * BLOCK - BLOCK + jnp.arange(3 * BLOCK)[None, :]
    valid = (pos >= 0) & (pos < L)
    mask = (band[None] & valid[:, None, :])[None, :, None, None]
    s_w = jnp.einsum('bnqkgd,bnskd->bnkgqs', qb, kw).astype(jnp.float32)
    s_w = jnp.where(mask, s_w, -jnp.inf)
    s_c = jnp.einsum('bnqkgd,bskd->bnkgqs', qb, k_c).astype(jnp.float32)
    sk = sink_f[None, None, :, :, None, None]
    m = jnp.maximum(jnp.maximum(s_w.max(-1, keepdims=True), s_c.max(-1, keepdims=True)), sk)
    p_w = jnp.exp(s_w - m)
    p_c = jnp.exp(s_c - m)
    den = p_w.sum(-1, keepdims=True) + p_c.sum(-1, keepdims=True) + jnp.exp(sk - m)
    o = (jnp.einsum('bnkgqs,bnskd->bnqkgd', (p_w / den).astype(dt), vw)
         + jnp.einsum('bnkgqs,bskd->bnqkgd', (p_c / den).astype(dt), v_c))
    o_lat = o.reshape(B, L, N_HEADS * HEAD_DIM) @ w_o

    if not need_ctx_out:
        return o_lat, None
    s = jnp.einsum('bqkgd,bskd->bkgqs', q_c * scale, k_c).astype(jnp.float32)
    skc = sink_f[None, :, :, None, None]
    mc = jnp.maximum(s.max(-1, keepdims=True), skc)
    pc = jnp.exp(s - mc)
    pc = pc / (pc.sum(-1, keepdims=True) + jnp.exp(skc - mc))
    oc = jnp.einsum('bkgqs,bskd->bqkgd', pc.astype(dt), v_c)
    o_ctx = oc.reshape(B, Lc, N_HEADS * HEAD_DIM) @ w_o
    return o_lat, o_ctx


def expert_choice_ffn(h, w_r, w1, w3, w2):
    B, N, D = h.shape
    cap = (CAPACITY_FACTOR * N) // N_EXPERTS
    aff = jax.nn.softmax((h @ w_r).astype(jnp.float32), axis=-1)
    gates, idx = lax.top_k(jnp.swapaxes(aff, 1, 2), cap)
    xg = jax.vmap(lambda hb, ib: hb[ib])(h, idx)
    a = jnp.einsum('becd,edf->becf', xg, w1)
    b = jnp.einsum('becd,edf->becf', xg, w3)
    y = jnp.einsum('becf,efd->becd', jax.nn.silu(a) * b, w2) * gates[..., None].astype(h.dtype)
    return jax.vmap(lambda ib, yb: jnp.zeros((N, D), yb.dtype).at[ib.reshape(-1)].add(yb.reshape(-1, D)))(idx, y)


def setup_inputs(seed: int = 0) -> dict:
    key = jax.random.key(seed)
    ks = jax.random.split(key, 24)
    f32 = jnp.float32

    def nrm(k, shape, scale):
        return jax.random.normal(k, shape, f32) * scale

    D = D_MODEL
    qkv_w = (N_HEADS + 2 * N_KV_HEADS) * HEAD_DIM
    return {
        "x": nrm(ks[0], (BATCH, SEQ, D), 1.0),
        "c": nrm(ks[1], (BATCH, D), 1.0),
        "ctx": nrm(ks[2], (BATCH, CTX_LEN, D), 1.0),
        "c_ctx": nrm(ks[3], (D,), 1.0),
        "w_mod": nrm(ks[4], (DEPTH, D, 6 * D), 0.5 * D ** -0.5),
        "b_mod": nrm(ks[5], (DEPTH, 6 * D), 0.02),
        "g_norm_mix": 1.0 + nrm(ks[6], (DEPTH, D), 0.02),
        "g_norm_ffn": 1.0 + nrm(ks[7], (DEPTH, D), 0.02),
        "a_w_in": nrm(ks[8], (N_A_LAYERS, D, 2 * DA), D ** -0.5),
        "a_b_in": nrm(ks[9], (N_A_LAYERS, 2 * DA), 0.02),
        "a_ln_g": 1.0 + nrm(ks[10], (N_A_LAYERS, DA), 0.02),
        "a_ln_b": nrm(ks[11], (N_A_LAYERS, DA), 0.02),
        "a_w_s": nrm(ks[12], (N_A_LAYERS, GA, CHUNK, CHUNK), CHUNK ** -0.5),
        "a_b_s": 1.0 + nrm(ks[13], (N_A_LAYERS, GA, CHUNK), 0.02),
        "a_w_out": nrm(ks[14], (N_A_LAYERS, DA, D), DA ** -0.5),
        "b_w_qkv": nrm(ks[15], (N_B_LAYERS, D, qkv_w), D ** -0.5),
        "b_sink": nrm(ks[16], (N_B_LAYERS, N_HEADS), 0.5),
        "b_w_o": nrm(ks[17], (N_B_LAYERS, N_HEADS * HEAD_DIM, D), (N_HEADS * HEAD_DIM) ** -0.5),
        "r_w": nrm(ks[18], (DEPTH, D, N_EXPERTS), D ** -0.5),
        "e_w1": nrm(ks[19], (DEPTH, N_EXPERTS, D, D_FF_EXPERT), D ** -0.5),
        "e_w3": nrm(ks[20], (DEPTH, N_EXPERTS, D, D_FF_EXPERT), D ** -0.5),
        "e_w2": nrm(ks[21], (DEPTH, N_EXPERTS, D_FF_EXPERT, D), D_FF_EXPERT ** -0.5),
        "g_final": 1.0 + nrm(ks[22], (D,), 0.02),
    }


def reference(x, c, ctx, c_ctx, w_mod, b_mod, g_norm_mix, g_norm_ffn,
              a_w_in, a_b_in, a_ln_g, a_ln_b, a_w_s, a_b_s, a_w_out,
              b_w_qkv, b_sink, b_w_o, r_w, e_w1, e_w3, e_w2, g_final):
    L = x.shape[1]
    ROWS = L // GRID_W
    row = jnp.broadcast_to(jnp.arange(ROWS)[:, None], (ROWS, GRID_W)).reshape(-1).astype(jnp.float32)
    col = jnp.broadcast_to(jnp.arange(GRID_W)[None, :], (ROWS, GRID_W)).reshape(-1).astype(jnp.float32)
    inv_freq = ROPE_THETA ** (-jnp.arange(ROPE_PAIRS_PER_AXIS, dtype=jnp.float32) / ROPE_PAIRS_PER_AXIS)
    ang = jnp.concatenate([row[:, None] * inv_freq, col[:, None] * inv_freq], axis=-1)
    cos = jnp.cos(ang).astype(x.dtype)
    sin = jnp.sin(ang).astype(x.dtype)

    sc_lat = jax.nn.silu(c)
    sc_ctx = jax.nn.silu(c_ctx)
    xc = ctx
    for i in range(DEPTH):
        last = i == DEPTH - 1
        is_attn = i % 2 == 1
        j = i // 2
        mod_l = sc_lat @ w_mod[i] + b_mod[i]
        mod_c = sc_ctx @ w_mod[i] + b_mod[i]
        sh1, s1, g1, sh2, s2, g2 = jnp.split(mod_l[:, None, :], 6, axis=-1)
        csh1, cs1, cg1, csh2, cs2, cg2 = jnp.split(mod_c, 6, axis=-1)

        h_l = rmsnorm(x, g_norm_mix[i]) * (1.0 + s1) + sh1
        need_ctx_in = (not last) or is_attn
        h_c = rmsnorm(xc, g_norm_mix[i]) * (1.0 + cs1) + csh1 if need_ctx_in else None
        if is_attn:
            o_l, o_c = windowed_gqa(h_l, h_c, b_w_qkv[j], b_sink[j], b_w_o[j], cos, sin, not last)
        else:
            o_l = chunk_gmlp(h_l, a_w_in[j], a_b_in[j], a_ln_g[j], a_ln_b[j], a_w_s[j], a_b_s[j], a_w_out[j])
            o_c = (chunk_gmlp(h_c, a_w_in[j], a_b_in[j], a_ln_g[j], a_ln_b[j], a_w_s[j], a_b_s[j], a_w_out[j])
                   if not last else None)
        x = x + g1 * o_l

        f_l = rmsnorm(x, g_norm_ffn[i]) * (1.0 + s2) + sh2
        x = x + g2 * expert_choice_ffn(f_l, r_w[i], e_w1[i], e_w3[i], e_w2[i])
        if not last:
            xc = xc + cg1 * o_c
            f_c = rmsnorm(xc, g_norm_ffn[i]) * (1.0 + cs2) + csh2
            xc = xc + cg2 * expert_choice_ffn(f_c, r_w[i], e_w1[i], e_w3[i], e_w2[i])
    return rmsnorm(x, g_final)
```

```python
import numpy as np
import ml_dtypes
from contextlib import ExitStack
import concourse.bass as bass
import concourse.mybir as mybir
from concourse.bass_utils import run_bass_kernel_spmd

F32 = mybir.dt.float32
BF16 = mybir.dt.bfloat16
I32 = mybir.dt.int32
AF = mybir.ActivationFunctionType
ALU = mybir.AluOpType
AX = mybir.AxisListType

EPOCH = 30000
COMPUTE = ("pe", "act", "dve", "pool")
ALL_ENG = ("pe", "act", "dve", "pool", "sp")


class Tok:
    __slots__ = ("sem", "val", "eng", "is_dma")

    def __init__(self, sem, val, eng, is_dma):
        self.sem, self.val, self.eng, self.is_dma = sem, val, eng, is_dma


class Sched:
    def __init__(self, nc, stack):
        self.nc = nc
        self.stack = stack
        self.ops = {e: [] for e in ALL_ENG}
        self.prog_sem = {}
        self.prog_cnt = {}
        self.dma_sem = {}
        self.known = {e: {} for e in ALL_ENG}
        self.lastw = {}
        self.readers = {}
        self.nsem = 0
        self.all_toks = {}
        self.nops = 0
        for e in COMPUTE:
            self._new_epoch(e)

    def _mk_sem(self, name):
        self.nsem += 1
        return self.stack.enter_context(self.nc.semaphore(name))

    def _new_epoch(self, e):
        self.prog_sem[e] = self._mk_sem(f"p_{e}_{self.nsem}")
        self.prog_cnt[e] = 0

    def _collect(self, eng, reads, writes, extra):
        deps = []
        for k in reads:
            t = self.lastw.get(k)
            if t is not None:
                deps.append(t)
        for k in writes:
            t = self.lastw.get(k)
            if t is not None:
                deps.append(t)
            deps.extend(self.readers.get(k, ()))
        deps.extend(extra)
        waits = {}
        kn = self.known[eng]
        for t in deps:
            if t is None:
                continue
            if (not t.is_dma) and t.eng == eng and eng == "pe":
                continue
            name = id(t.sem)
            if kn.get(name, 0) >= t.val:
                continue
            if name not in waits or waits[name].val < t.val:
                waits[name] = t
        for name, t in waits.items():
            kn[name] = t.val
        return list(waits.values())

    def _commit(self, tok, reads, writes):
        for k in reads:
            self.readers.setdefault(k, []).append(tok)
        for k in writes:
            self.lastw[k] = tok
            self.readers[k] = []
        self.all_toks[id(tok.sem)] = tok

    def op(self, eng, fn, reads=(), writes=(), extra=()):
        waits = self._collect(eng, reads, writes, extra)
        if self.prog_cnt[eng] >= EPOCH:
            self._new_epoch(eng)
        self.prog_cnt[eng] += 1
        tok = Tok(self.prog_sem[eng], self.prog_cnt[eng], eng, False)
        self.ops[eng].append((waits, fn, tok.sem, 1))
        self._commit(tok, reads, writes)
        self.nops += 1
        return tok

    def dma(self, eng, fn, key, reads=(), writes=(), extra=(), group=False):
        ent = self.dma_sem.get(key)
        if ent is None:
            ent = [self._mk_sem(f"d_{self.nsem}"), 0, None]
            self.dma_sem[key] = ent
        ex = list(extra)
        if ent[2] is not None and not group:
            ex.append(ent[2])
        waits = self._collect(eng, reads, writes, ex)
        ent[1] += 16
        tok = Tok(ent[0], ent[1], eng, True)
        ent[2] = tok
        self.ops[eng].append((waits, fn, tok.sem, 16))
        self._commit(tok, reads, writes)
        self.nops += 1
        return tok

    def barrier(self, engs=ALL_ENG):
        toks = list(self.all_toks.values())
        for e in engs:
            waits = []
            kn = self.known[e]
            for t in toks:
                name = id(t.sem)
                if kn.get(name, 0) >= t.val:
                    continue
                kn[name] = t.val
                waits.append(t)
            if waits:
                self.ops[e].append((waits, None, None, 0))
        self.lastw = {}
        self.readers = {}

    def emit(self):
        nc = self.nc
        ops = self.ops

        def run(engine, lst):
            for waits, fn, sem, inc in lst:
                for t in waits:
                    engine.wait_ge(t.sem, t.val)
                if fn is None:
                    continue
                inst = fn(engine)
                inst.then_inc(sem, inc)

        with nc.Block() as block:
            @block.tensor
            def _(e):
                run(e, ops["pe"])

            @block.scalar
            def _(e):
                run(e, ops["act"])

            @block.vector
            def _(e):
                run(e, ops["dve"])

            @block.gpsimd
            def _(e):
                run(e, ops["pool"])

            @block.sync
            def _(e):
                run(e, ops["sp"])
        self.ops = {e: [] for e in ALL_ENG}


D = 1024
KC = 8
SEQ = 4096
CTX = 256
NTOK = SEQ + CTX
NT = NTOK // 128
NLT = SEQ // 128
NS = 2
NE = 16
CAP_L = 512
CAP_C = 32
EPS = 1e-6
NBIS = 27


def build(NL=4, do_final=True, dbg=False, skip_moe=False, dbg_route=False, dbg_exp=False):
    nc = bass.Bass("TRN2", target_bir_lowering=False)

    def din(name, shape, dt=F32):
        return nc.dram_tensor(name, list(shape), dt, kind="ExternalInput").ap()

    x_in = din("x", [NS, SEQ, D])
    c_in = din("c", [NS, D])
    ctx_in = din("ctx", [NS, CTX, D])
    cctx_in = din("c_ctx", [D])
    w_mod = din("w_mod", [4, D, 6 * D])
    b_mod = din("b_mod", [4, 6 * D])
    g_nm = din("g_norm_mix", [4, D])
    g_nf = din("g_norm_ffn", [4, D])
    a_w_in = din("a_w_in", [2, D, 4096])
    a_b_in = din("a_b_in", [2, 4096])
    a_ln_g = din("a_ln_g", [2, 2048])
    a_ln_b = din("a_ln_b", [2, 2048])
    a_w_s = din("a_w_s", [2, 16, 128, 128])
    a_b_s = din("a_b_s", [2, 16, 128])
    a_w_out = din("a_w_out", [2, 2048, D])
    b_w_qkv = din("b_w_qkv", [2, D, 1536])
    b_sink = din("b_sink", [2, 16])
    b_w_o = din("b_w_o", [2, D, D])
    r_w = din("r_w", [4, D, NE])
    e_w1 = din("e_w1", [4, NE, D, 2048])
    e_w3 = din("e_w3", [4, NE, D, 2048])
    e_w2 = din("e_w2", [4, NE, 2048, D])
    g_final = din("g_final", [D])
    k_ident = din("k_ident", [128, 128])
    k_cos = din("k_cos", [SEQ, 32])
    k_sin = din("k_sin", [SEQ, 32])
    k_upper = din("k_upper", [128, 128])
    k_iota = din("k_iota", [128, 128])
    k_pt = din("k_pt", [128, NT, 2])
    k_ml = din("k_ml", [128, 128])
    k_mr = din("k_mr", [128, 128])

    out_d = nc.dram_tensor("out", [NS, SEQ, D], F32, kind="ExternalOutput").ap()
    XR = nc.dram_tensor("xr_scratch", [NS, NTOK, D], F32, kind="Internal").ap()
    FD = nc.dram_tensor("f_scratch", [NS, NTOK, D], BF16, kind="Internal").ap()
    XRflat = XR.rearrange("s t d -> (s t) d")
    FDflat = FD.rearrange("s t d -> (s t) d")
    dbg_d = nc.dram_tensor("dbg", [NS, NTOK, D], F32, kind="ExternalOutput").ap() if dbg else None

    with ExitStack() as gst:
        S = Sched(nc, gst)

        uniq = {"n": 0}

        def sbt(st, name, shape, dt):
            uniq["n"] += 1
            return st.enter_context(nc.sbuf_tensor(f"{name}_u{uniq['n']}", list(shape), dt))

        PS = gst.enter_context(nc.psum_tensor("PS", [128, 8, 512], F32))

        def pk(*bs):
            return [f"ps{b}" for b in bs]

        identf = sbt(gst, "identf", [128, 128], F32)
        identb = sbt(gst, "identb", [128, 128], BF16)
        onesb = sbt(gst, "onesb", [128, 128], BF16)
        onesF = sbt(gst, "onesF", [128, 128], F32)
        upperb = sbt(gst, "upperb", [128, 128], BF16)
        iotaf = sbt(gst, "iotaf", [128, 128], F32)
        ptf = sbt(gst, "ptf", [128, NT, 2], F32)
        mlb = sbt(gst, "mlb", [128, 128], BF16)
        mrb = sbt(gst, "mrb", [128, 128], BF16)
        epst = sbt(gst, "epst", [128, 1], F32)
        scT = sbt(gst, "scT", [128, KC, 4], F32)
        AFF = sbt(gst, "AFF", [128, NS, NT, NE], F32)

        S.dma("sp", lambda e: e.dma_start(out=identf[:], in_=k_ident), "c0", writes=["identf"])
        S.dma("sp", lambda e: e.dma_start(out=iotaf[:], in_=k_iota), "c1", writes=["iotaf"])
        S.dma("sp", lambda e: e.dma_start(out=ptf[:], in_=k_pt), "c2", writes=["ptf"])
        S.dma("pool", lambda e: e.dma_start(out=identb[:], in_=k_ident), "c3", writes=["identb"])
        S.dma("pool", lambda e: e.dma_start(out=upperb[:], in_=k_upper), "c4", writes=["upperb"])
        S.dma("pool", lambda e: e.dma_start(out=mlb[:], in_=k_ml), "c5", writes=["mlb"])
        S.dma("pool", lambda e: e.dma_start(out=mrb[:], in_=k_mr), "c6", writes=["mrb"])
        S.op("dve", lambda e: e.memset(onesb[:], 1.0), writes=["onesb"])
        S.op("dve", lambda e: e.memset(onesF[:], 1.0), writes=["onesF"])
        S.op("dve", lambda e: e.memset(epst[:], EPS), writes=["epst"])
        S.op("dve", lambda e: e.memset(AFF[:], 0.0), writes=["AFF"])
        S.op("dve", lambda e: e.memset(scT[:], 0.0), writes=["scT"])
        for s in range(NS):
            S.dma("sp", lambda e, s=s: e.dma_start(out=scT[:, :, s:s + 1], in_=c_in[s].rearrange("(kc p o) -> p kc o", p=128, o=1), allow_slow_non_contiguous=True),
                  "c8", reads=["scT"], writes=["scT"])
        S.dma("sp", lambda e: e.dma_start(out=scT[:, :, 2:3], in_=cctx_in.rearrange("(kc p o) -> p kc o", p=128, o=1), allow_slow_non_contiguous=True),
              "c9", reads=["scT"], writes=["scT"])
        S.op("act", lambda e: e.activation(out=scT[:], in_=scT[:], func=AF.Silu), reads=["scT"], writes=["scT"])
        for s in range(NS):
            S.dma("sp", lambda e, s=s: e.dma_start(out=XR[s, 0:SEQ, :], in_=x_in[s]), f"cx{s}", writes=[f"XR{s}"])
            S.dma("sp", lambda e, s=s: e.dma_start(out=XR[s, SEQ:NTOK, :], in_=ctx_in[s]), f"cc{s}", writes=[f"XR{s}"])
        S.barrier()
        S.emit()

        for L in range(NL):
            is_attn = (L % 2 == 1)
            j = L // 2
            with ExitStack() as lst:
                modT = sbt(lst, "modT", [128, 48, 4], F32)
                gs1T = sbt(lst, "gs1T", [128, KC, 4], F32)
                gs2T = sbt(lst, "gs2T", [128, KC, 4], F32)
                GBt = sbt(lst, "GBt", [128, 3, D], F32)
                G1B = GBt
                G2B = GBt
                rw = sbt(lst, "rw", [128, KC, NE], F32)
                sh1T = modT[:, 0:8, :]
                sh2T = modT[:, 24:32, :]

                def mod_phase(cbs, full):
                  with ExitStack() as ph:
                      scB = sbt(ph, "scB", [128, KC, 3, 128], F32)
                      wmp = [sbt(ph, f"wmp{i}", [128, KC, 512], F32) for i in range(2)]
                      bmT = sbt(ph, "bmT", [128, 48], F32)
                      bmb = sbt(ph, "bmb", [128, 2, D], F32)
                      gnm = sbt(ph, "gnm", [128, 2, KC], F32)
                      tmp4 = sbt(ph, "tmp4", [128, KC, 4], F32)
                      S.dma("sp", lambda e: e.dma_start(out=bmT[:], in_=b_mod[L].rearrange("(c p) -> p c", p=128), allow_slow_non_contiguous=True), "m0", writes=["bmT"])
                      S.dma("sp", lambda e: e.dma_start(out=gnm[:, 0, :], in_=g_nm[L].rearrange("(c p) -> p c", p=128), allow_slow_non_contiguous=True), "m1", writes=["gnm0"])
                      S.dma("sp", lambda e: e.dma_start(out=gnm[:, 1, :], in_=g_nf[L].rearrange("(c p) -> p c", p=128), allow_slow_non_contiguous=True), "m2", writes=["gnm1"])
                      S.dma("sp", lambda e: e.dma_start(out=bmb[:, 0, :], in_=b_mod[L, 2048:3072].rearrange("(o d) -> o d", o=1).to_broadcast([128, D])), "m3", writes=["bmb0"])
                      S.dma("sp", lambda e: e.dma_start(out=bmb[:, 1, :], in_=b_mod[L, 5120:6144].rearrange("(o d) -> o d", o=1).to_broadcast([128, D])), "m4", writes=["bmb1"])
                      S.dma("sp", lambda e: e.dma_start(out=rw[:], in_=r_w[L].rearrange("(kc p) n -> p kc n", p=128)), "m5", writes=["rw"])
                      for r in range(3):
                          S.op("dve", lambda e, r=r: e.tensor_copy(out=scB[:, :, r, :], in_=scT[:, :, r:r + 1].to_broadcast([128, KC, 128])),
                               reads=["scT"], writes=[f"scB{r}"])
                      for cb in cbs:
                          buf = wmp[cb % 2]
                          bk = f"wmp{cb % 2}"
                          S.dma("sp", lambda e, cb=cb, buf=buf: e.dma_start(
                              out=buf[:], in_=w_mod[L][:, cb * 512:(cb + 1) * 512].rearrange("(kc p) n -> p kc n", p=128)),
                              bk, writes=[bk])

                          def f_mod(e, cb=cb, buf=buf):
                              ins = None
                              for jj in range(4):
                                  ci = cb * 4 + jj
                                  for kc in range(KC):
                                      ins = e.matmul(PS[:, 0, ci * 4:ci * 4 + 4], lhsT=buf[:, kc, jj * 128:(jj + 1) * 128],
                                                     rhs=scT[:, kc, :], start=(kc == 0), stop=(kc == KC - 1))
                              return ins
                          if full:
                              S.op("pe", f_mod, reads=[bk, "scT"], writes=[f"modps{cb}"])
                          if (full and cb in (4, 5)) or ((not full) and cb in (10, 11)):
                              gi = 0 if cb < 6 else 1
                              half = cb % 2
                              GB = G1B if gi == 0 else G2B
                              for r in range(3):
                                  bnk = 1 + (r % 2)

                                  def f_g(e, buf=buf, r=r, bnk=bnk):
                                      ins = None
                                      for kc in range(KC):
                                          ins = e.matmul(PS[:, bnk, :], lhsT=scB[:, kc, r, :], rhs=buf[:, kc, :],
                                                         start=(kc == 0), stop=(kc == KC - 1))
                                      return ins
                                  S.op("pe", f_g, reads=[bk, f"scB{r}"], writes=pk(bnk))
                                  S.op("dve", lambda e, GB=GB, r=r, bnk=bnk, gi=gi, half=half: e.tensor_tensor(
                                      out=GB[:, r, half * 512:(half + 1) * 512], in0=PS[:, bnk, :],
                                      in1=bmb[:, gi, half * 512:(half + 1) * 512], op=ALU.add),
                                      reads=pk(bnk) + [f"bmb{gi}"], writes=[f"GB{gi}_{r}_{half}"])
                      if full:
                          S.op("dve", lambda e: e.tensor_tensor(
                              out=modT[:], in0=PS[:, 0, 0:192].rearrange("p (c f) -> p c f", f=4),
                              in1=bmT[:].unsqueeze(2).to_broadcast([128, 48, 4]), op=ALU.add),
                              reads=[f"modps{cb}" for cb in range(12)] + ["bmT"], writes=["modT"])
                          for (gsT, c0, gi) in ((gs1T, 8, 0), (gs2T, 32, 1)):
                              S.op("dve", lambda e, c0=c0: e.tensor_scalar(out=tmp4[:], in0=modT[:, c0:c0 + 8, :], scalar1=1.0, scalar2=None, op0=ALU.add),
                                   reads=["modT"], writes=["tmp4"])
                              S.op("dve", lambda e, gsT=gsT, gi=gi: e.tensor_tensor(
                                  out=gsT[:], in0=tmp4[:], in1=gnm[:, gi, :].unsqueeze(2).to_broadcast([128, KC, 4]), op=ALU.mult),
                                  reads=["tmp4", f"gnm{gi}"], writes=[f"gsT{gi}"])
                      S.barrier()
                      S.emit()

                mod_phase(list(range(12)), True)

                def norm_stats(xt_ap, xkey, ss, rt, rstd, junk, tag):
                    S.op("act", lambda e: e.activation(out=junk, in_=xt_ap, func=AF.Square, accum_out=ss),
                         reads=[xkey], writes=[f"junk{tag}", f"ss{tag}"])
                    S.op("act", lambda e: e.activation(out=rt, in_=ss, func=AF.Sqrt, scale=1.0 / D, bias=epst[:, 0:1]),
                         reads=[f"ss{tag}"], writes=[f"rt{tag}"])
                    S.op("dve", lambda e: e.reciprocal(out=rstd, in_=rt), reads=[f"rt{tag}"], writes=[f"rstd{tag}"])

                def post_tile(s, tile, r, obank, W):
                    rows = slice(tile * 128, (tile + 1) * 128)
                    xb = W["xtB"][0]
                    xbk = "xtB0"
                    W["cntB"] += 1
                    S.dma("sp", lambda e: e.dma_start(out=xb[:], in_=XR[s, rows, :]), xbk, reads=[f"XR{s}_{tile}"], writes=[xbk])
                    osb = W["osb"]
                    S.op("dve", lambda e: e.tensor_tensor(out=osb[:].rearrange("p (a b) -> p a b", a=2), in0=PS[:, obank:obank + 2, :],
                                                         in1=G1B[:, r, :].rearrange("p (a b) -> p a b", a=2), op=ALU.mult),
                         reads=pk(obank, obank + 1), writes=["osb"])
                    S.op("pool", lambda e: e.tensor_tensor(out=xb[:], in0=osb[:], in1=xb[:], op=ALU.add),
                         reads=["osb", xbk], writes=[xbk])
                    S.dma("sp", lambda e: e.dma_start(out=XR[s, rows, :], in_=xb[:]), f"stx{W['cntB'] % 2}", reads=[xbk], writes=[f"XR{s}_{tile}"])
                    norm_stats(xb[:], xbk, W["ss2"][:], W["rt2"][:], W["rstd2"][:], W["junk"][:], "2")
                    xn2 = W["xn2"]
                    S.op("dve", lambda e: e.tensor_scalar(out=xn2[:], in0=xb[:], scalar1=W["rstd2"][:, 0:1], scalar2=None, op0=ALU.mult),
                         reads=[xbk, "rstd2"], writes=["osb"])
                    ft = W["ft"]
                    S.op("pool", lambda e: e.tensor_copy(out=ft[:], in_=xn2[:]), reads=["osb"], writes=["ft"])
                    S.dma("sp", lambda e: e.dma_start(out=FD[s, rows, :], in_=ft[:]), "stf", reads=["ft"], writes=[f"FD{s}_{tile}"])

                    def f_tr(e):
                        ins = None
                        for kc in range(KC):
                            ins = e.transpose(PS[:, 1 + kc // 4, (kc % 4) * 128:(kc % 4 + 1) * 128], xn2[:, kc * 128:(kc + 1) * 128], identf[:])
                        return ins
                    S.op("pe", f_tr, reads=["osb", "identf"], writes=pk(1, 2))
                    fT = W["fT"]

                    def f_ev(e):
                        ins = None
                        for kc in range(KC):
                            ins = e.activation(out=fT[:, kc, :], in_=PS[:, 1 + kc // 4, (kc % 4) * 128:(kc % 4 + 1) * 128], func=AF.Identity,
                                               scale=gs2T[:, kc, r:r + 1], bias=sh2T[:, kc, r:r + 1])
                        return ins
                    S.op("act", f_ev, reads=pk(1, 2) + ["gsT1", "modT"], writes=["fT"])

                    def f_lg(e):
                        ins = None
                        for kc in range(KC):
                            ins = e.matmul(PS[:, 5, 0:NE], lhsT=fT[:, kc, :], rhs=rw[:, kc, :], start=(kc == 0), stop=(kc == KC - 1))
                        return ins
                    S.op("pe", f_lg, reads=["fT", "rw"], writes=pk(5))
                    mx, ex, se = W["mx"], W["ex"], W["se"]
                    S.op("dve", lambda e: e.tensor_reduce(out=mx[:], in_=PS[:, 5, 0:NE], axis=AX.X, op=ALU.max), reads=pk(5), writes=["mx"])
                    S.op("dve", lambda e: e.tensor_scalar(out=mx[:], in0=mx[:], scalar1=-1.0, scalar2=None, op0=ALU.mult), reads=["mx"], writes=["mx"])
                    S.op("act", lambda e: e.activation(out=ex[:], in_=PS[:, 5, 0:NE], func=AF.Exp, bias=mx[:, 0:1], accum_out=se[:]),
                         reads=pk(5) + ["mx"], writes=["ex", "se"])
                    S.op("dve", lambda e: e.reciprocal(out=se[:], in_=se[:]), reads=["se"], writes=["se"])
                    S.op("dve", lambda e: e.tensor_scalar(out=AFF[:, s, tile, :], in0=ex[:], scalar1=se[:, 0:1], scalar2=None, op0=ALU.mult),
                         reads=["ex", "se"], writes=["AFF"])

                def alloc_post(ph):
                    W = {"cntB": 0}
                    W["xtB"] = [sbt(ph, f"xtB{i}", [128, D], F32) for i in range(1)]
                    W["osb"] = sbt(ph, "osb", [128, D], F32)
                    W["xn2"] = W["osb"]
                    W["ft"] = sbt(ph, "ft", [128, D], BF16)
                    W["fT"] = sbt(ph, "fT", [128, KC, 128], F32)
                    W["junk"] = sbt(ph, "junk", [128, D], BF16)
                    for nm in ("ss2", "rt2", "rstd2", "mx", "se"):
                        W[nm] = sbt(ph, nm, [128, 1], F32)
                    W["ex"] = sbt(ph, "ex", [128, NE], F32)
                    return W

                def load_norm_hT(s, tile, r, xtA, cntA, Wn, hT_dst, hkey):
                    rows = slice(tile * 128, (tile + 1) * 128)
                    xa = xtA[cntA % len(xtA)]
                    xak = f"xtA{cntA % len(xtA)}"
                    S.dma("sp", lambda e: e.dma_start(out=xa[:], in_=XR[s, rows, :]), xak, reads=[f"XR{s}_{tile}"], writes=[xak])
                    norm_stats(xa[:], xak, Wn["ss1"][:], Wn["rt1"][:], Wn["rstd1"][:], Wn["junk"][:], "1")
                    xn = Wn["xn"]
                    S.op("dve", lambda e: e.tensor_scalar(out=xn[:], in0=xa[:], scalar1=Wn["rstd1"][:, 0:1], scalar2=None, op0=ALU.mult),
                         reads=[xak, "rstd1"], writes=["xn"])
                    pT = PS[:, 0, :].bitcast(BF16).rearrange("p (k t) -> p k t", k=KC)

                    def f_tr(e):
                        ins = None
                        for kc in range(KC):
                            ins = e.transpose(pT[:, kc, :], xn[:, kc * 128:(kc + 1) * 128], identb[:])
                        return ins
                    S.op("pe", f_tr, reads=["xn", "identb"], writes=pk(0))

                    def f_ev(e):
                        ins = None
                        for kc in range(KC):
                            ins = e.activation(out=hT_dst[:, kc, :], in_=pT[:, kc, :], func=AF.Identity,
                                               scale=gs1T[:, kc, r:r + 1], bias=sh1T[:, kc, r:r + 1])
                        return ins
                    S.op("act", f_ev, reads=pk(0) + ["gsT0", "modT"], writes=[hkey])

                if not is_attn:
                    with ExitStack() as ph:
                        win = sbt(ph, "win", [128, KC, 4096], BF16)
                        wout = sbt(ph, "wout", [128, 16, D], BF16)
                        wsT = sbt(ph, "wsT", [128, 16, 128], BF16)
                        Cc = sbt(ph, "Cc", [128, 16, 128], F32)
                        binu = sbt(ph, "binu", [128, 16], F32)
                        binv = sbt(ph, "binv", [128, 2048], F32)
                        lng = sbt(ph, "lng", [128, 16], F32)
                        onesf = sbt(ph, "onesf", [128, 2], F32)
                        xtA = [sbt(ph, f"xtA{i}", [128, D], F32) for i in range(2)]
                        Wn = {"xn": sbt(ph, "xn", [128, D], BF16)}
                        for nm in ("ss1", "rt1", "rstd1", "sum1", "sum2", "mean", "msq", "var", "rsv"):
                            Wn[nm] = sbt(ph, nm, [128, 1], F32)
                        W = alloc_post(ph)
                        Wn["junk"] = W["junk"]
                        hT = sbt(ph, "hT", [128, KC, 512], BF16)
                        uT = sbt(ph, "uT", [128, 16, 512], BF16)
                        vf = sbt(ph, "vf", [128, 2048], F32)
                        vn = sbt(ph, "vn", [128, 2, 2048], BF16)
                        tmpS = sbt(ph, "tmpS", [128, 512], F32)
                        wsf = vn[:].rearrange("p a b -> p (a b)").bitcast(F32).rearrange("p (g q) -> p g q", g=16)
                        wsTf = vf[:].rearrange("p (g q) -> p g q", g=16)
                        uTf = uT[:].rearrange("p a b -> p (a b)").bitcast(F32)
                        lhs2 = uTf[0:2, 0:2048]
                        rhs2 = uTf[0:2, 2048:4096].rearrange("p (g q) -> p g q", g=16)

                        for kc in range(KC):
                            S.dma("pool", lambda e, kc=kc: e.dma_start(out=win[:, kc, :], in_=a_w_in[j, kc * 128:(kc + 1) * 128, :], max_dma_last_dim=8192),
                                  f"win{kc}", writes=["win"])
                        for cc in range(4):
                            S.dma("pool", lambda e, cc=cc: e.dma_start(
                                out=wout[:, cc * 4:(cc + 1) * 4, :], in_=a_w_out[j, cc * 512:(cc + 1) * 512, :].rearrange("(c p) n -> p c n", p=128)),
                                f"wout{cc}", writes=["wout"])
                        S.dma("sp", lambda e: e.dma_start(out=wsf[:], in_=a_w_s[j].rearrange("g p q -> p g q")), "a0", writes=["wsf"])
                        S.dma("sp", lambda e: e.dma_start(out=binu[:], in_=a_b_in[j, 0:2048].rearrange("(c p) -> p c", p=128), allow_slow_non_contiguous=True), "a1", writes=["binu"])
                        S.dma("sp", lambda e: e.dma_start(out=lng[:], in_=a_ln_g[j].rearrange("(c p) -> p c", p=128), allow_slow_non_contiguous=True), "a2", writes=["lng"])
                        S.dma("sp", lambda e: e.dma_start(out=binv[:], in_=a_b_in[j, 2048:4096].rearrange("(o d) -> o d", o=1).to_broadcast([128, 2048])),
                              "a3", writes=["binv"])
                        S.op("dve", lambda e: e.memset(lhs2[:], 1.0), writes=["lhs2a", "lhs2b"])
                        S.dma("sp", lambda e: e.dma_start(out=lhs2[0:1, :], in_=a_ln_b[j].rearrange("(o d) -> o d", o=1)), "a4", writes=["lhs2a"])
                        S.dma("sp", lambda e: e.dma_start(out=rhs2[1:2, :, :], in_=a_b_s[j].rearrange("(o g) p -> o g p", o=1)), "a5", writes=["rhs2b"])
                        S.op("dve", lambda e: e.memset(onesf[:], 1.0), writes=["onesf"])
                        for g4 in range(4):
                            def f_t(e, g4=g4):
                                ins = None
                                for gg in range(4):
                                    ins = e.transpose(PS[:, 1, gg * 128:(gg + 1) * 128], wsf[:, g4 * 4 + gg, :], identf[:])
                                return ins
                            S.op("pe", f_t, reads=["wsf", "identf"], writes=pk(1))
                            S.op("dve", lambda e, g4=g4: e.tensor_copy(out=wsT[:, g4 * 4:(g4 + 1) * 4, :], in_=PS[:, 1, :].rearrange("p (g q) -> p g q", g=4)),
                                 reads=pk(1), writes=["wsT"])
                        S.op("dve", lambda e: e.tensor_copy(out=wsTf[:], in_=wsT[:]), reads=["wsT"], writes=["wsTf"])
                        for g4 in range(4):
                            S.op("pe", lambda e, g4=g4: e.matmul(PS[0:2, 2, :], lhsT=onesf[:, 0:2], rhs=wsTf[:, g4 * 4:(g4 + 1) * 4, :].rearrange("p g q -> p (g q)"),
                                                              start=True, stop=True), reads=["wsTf", "onesf"], writes=pk(2))
                            S.op("dve", lambda e, g4=g4: e.tensor_copy(out=rhs2[0:1, g4 * 4:(g4 + 1) * 4, :], in_=PS[0:1, 2, :].rearrange("p (g q) -> p g q", g=4)),
                                 reads=pk(2), writes=["rhs2a"])
                        for g in range(16):
                            S.op("pe", lambda e, g=g: e.matmul(PS[:, 3, (g % 4) * 128:(g % 4 + 1) * 128], lhsT=lhs2[:, g * 128:(g + 1) * 128], rhs=rhs2[:, g, :],
                                                            start=True, stop=True),
                                 reads=["lhs2a", "lhs2b", "rhs2a", "rhs2b"], writes=pk(3))
                            if g % 4 == 3:
                                S.op("dve", lambda e, g=g: e.tensor_copy(out=Cc[:, g - 3:g + 1, :], in_=PS[:, 3, :].rearrange("p (g q) -> p g q", g=4)),
                                     reads=pk(3), writes=["Cc"])

                        S.barrier()

                        def do_group(s, gi):
                                tiles = list(range(gi * 4, gi * 4 + 4)) if gi < 8 else [32, 33]
                                nt = len(tiles)
                                ncol = nt * 128
                                r = s if gi < 8 else 2
                                for ti, tile in enumerate(tiles):
                                    load_norm_hT(s, tile, r, xtA, tile, Wn, hT[:, :, ti * 128:(ti + 1) * 128], "hT")
                                for fc in range(16):
                                    bnk = 1 + fc % 2

                                    def f_u(e, fc=fc, bnk=bnk):
                                        ins = None
                                        for kc in range(KC):
                                            ins = e.matmul(PS[:, bnk, 0:ncol], lhsT=win[:, kc, fc * 128:(fc + 1) * 128], rhs=hT[:, kc, 0:ncol],
                                                           start=(kc == 0), stop=(kc == KC - 1))
                                        return ins
                                    S.op("pe", f_u, reads=["win", "hT"], writes=pk(bnk))
                                    S.op("act", lambda e, fc=fc, bnk=bnk: e.activation(out=uT[:, fc, 0:ncol], in_=PS[:, bnk, 0:ncol], func=AF.Gelu, bias=binu[:, fc:fc + 1]),
                                         reads=pk(bnk) + ["binu"], writes=[f"uT{fc}"])
                                for ti in range(nt):
                                    vb = ti % 2
                                    vnb = vn[:, vb, :]
                                    vk = f"vn{vb}"
                                    for vc in range(4):
                                        bnk = 3 + vc % 2

                                        def f_v(e, ti=ti, vc=vc, bnk=bnk):
                                            ins = None
                                            for kc in range(KC):
                                                ins = e.matmul(PS[:, bnk, :], lhsT=hT[:, kc, ti * 128:(ti + 1) * 128],
                                                               rhs=win[:, kc, 2048 + vc * 512:2048 + (vc + 1) * 512], start=(kc == 0), stop=(kc == KC - 1))
                                            return ins
                                        S.op("pe", f_v, reads=["win", "hT"], writes=pk(bnk))
                                        S.op("dve", lambda e, vc=vc, bnk=bnk: e.tensor_tensor(out=vf[:, vc * 512:(vc + 1) * 512], in0=PS[:, bnk, :],
                                                                                           in1=binv[:, vc * 512:(vc + 1) * 512], op=ALU.add),
                                             reads=pk(bnk) + ["binv"], writes=[f"vf{vc}"])
                                    vfk = [f"vf{v}" for v in range(4)]
                                    S.op("act", lambda e: e.activation(out=vf[:], in_=vf[:], func=AF.Gelu, accum_out=Wn["sum1"][:]),
                                         reads=vfk, writes=vfk + ["sum1"])
                                    S.op("act", lambda e, vnb=vnb: e.activation(out=vnb, in_=vf[:], func=AF.Square, accum_out=Wn["sum2"][:]),
                                         reads=vfk, writes=[vk, "sum2"])
                                    S.op("dve", lambda e: e.tensor_scalar(out=Wn["mean"][:], in0=Wn["sum1"][:], scalar1=1.0 / 2048, scalar2=None, op0=ALU.mult),
                                         reads=["sum1"], writes=["mean"])
                                    S.op("dve", lambda e: e.tensor_tensor(out=Wn["msq"][:], in0=Wn["mean"][:], in1=Wn["mean"][:], op=ALU.mult),
                                         reads=["mean"], writes=["msq"])
                                    S.op("dve", lambda e: e.scalar_tensor_tensor(out=Wn["var"][:], in0=Wn["sum2"][:], scalar=1.0 / 2048, in1=Wn["msq"][:],
                                                                                 op0=ALU.mult, op1=ALU.subtract), reads=["sum2", "msq"], writes=["var"])
                                    S.op("act", lambda e: e.activation(out=Wn["rsv"][:], in_=Wn["var"][:], func=AF.Sqrt, bias=epst[:, 0:1]),
                                         reads=["var"], writes=["rsv"])
                                    S.op("dve", lambda e: e.reciprocal(out=Wn["rsv"][:], in_=Wn["rsv"][:]), reads=["rsv"], writes=["rsv"])
                                    S.op("dve", lambda e, vnb=vnb: e.tensor_scalar(out=vnb, in0=vf[:], scalar1=Wn["mean"][:, 0:1], scalar2=Wn["rsv"][:, 0:1],
                                                                                op0=ALU.subtract, op1=ALU.mult),
                                         reads=vfk + ["mean", "rsv"], writes=[vk])
                                    for gq in range(4):
                                        g0 = gq * 4
                                        bnk = 5 if gq % 2 == 0 else 1

                                        def f_s(e, g0=g0, bnk=bnk, vnb=vnb):
                                            ins = None
                                            for gg in range(4):
                                                ins = e.matmul(PS[:, bnk, gg * 128:(gg + 1) * 128], lhsT=vnb[:, (g0 + gg) * 128:(g0 + gg + 1) * 128], rhs=wsT[:, g0 + gg, :],
                                                               start=True, stop=True)
                                            return ins
                                        S.op("pe", f_s, reads=[vk, "wsT"], writes=pk(bnk))
                                        t3 = tmpS[:].rearrange("p (g q) -> p g q", g=4)
                                        S.op("dve", lambda e, g0=g0, bnk=bnk, t3=t3: e.tensor_tensor(
                                            out=t3, in0=PS[:, bnk, :].rearrange("p (g q) -> p g q", g=4),
                                            in1=lng[:, g0:g0 + 4].unsqueeze(2).to_broadcast([128, 4, 128]), op=ALU.mult),
                                            reads=pk(bnk) + ["lng"], writes=["tmpS"])
                                        S.op("pool", lambda e, g0=g0, t3=t3: e.tensor_tensor(out=t3, in0=t3, in1=Cc[:, g0:g0 + 4, :], op=ALU.add),
                                             reads=["tmpS", "Cc"], writes=["tmpS"])
                                        S.op("pool", lambda e, g0=g0, t3=t3, ti=ti: e.tensor_tensor(
                                            out=uT[:, g0:g0 + 4, ti * 128:(ti + 1) * 128], in0=uT[:, g0:g0 + 4, ti * 128:(ti + 1) * 128], in1=t3, op=ALU.mult),
                                            reads=[f"uT{g0 + gg}" for gg in range(4)] + ["tmpS"], writes=[f"uT{g0 + gg}" for gg in range(4)])
                                for ti, tile in enumerate(tiles):
                                    for half in range(2):
                                        def f_o(e, ti=ti, half=half):
                                            ins = None
                                            for cc in range(16):
                                                ins = e.matmul(PS[:, 6 + half, :], lhsT=uT[:, cc, ti * 128:(ti + 1) * 128], rhs=wout[:, cc, half * 512:(half + 1) * 512],
                                                               start=(cc == 0), stop=(cc == 15))
                                            return ins
                                        S.op("pe", f_o, reads=[f"uT{g}" for g in range(16)] + ["wout"], writes=pk(6 + half))
                                    post_tile(s, tile, r, 6, W)

                        for s in range(NS):
                            for gi in range(9):
                                do_group(s, gi)
                        S.barrier()
                        S.emit()
                else:
                    with ExitStack() as ph:
                        wqkv = sbt(ph, "wqkv", [128, KC, 1536], BF16)
                        wo = sbt(ph, "wo", [128, KC, D], BF16)
                        qT = sbt(ph, "qT", [128, KC, NTOK], BF16)
                        kT = sbt(ph, "kT", [128, 4, NTOK], BF16)
                        Vt = sbt(ph, "Vt", [128, NT, 256], BF16)
                        SEa = sbt(ph, "SEa", [128, 16], F32)
                        SE = sbt(ph, "SE", [128, 4, 2], F32)
                        for kc in range(KC):
                            S.dma("pool", lambda e, kc=kc: e.dma_start(out=wqkv[:, kc, :], in_=b_w_qkv[j, kc * 128:(kc + 1) * 128, :]), f"wq{kc % 2}", writes=["wqkv"])
                        for kc in range(KC):
                            S.dma("pool", lambda e, kc=kc: e.dma_start(out=wo[:, kc, :], in_=b_w_o[j, kc * 128:(kc + 1) * 128, :]), f"wo{kc % 2}", writes=["wo"])
                        S.dma("sp", lambda e: e.dma_start(out=SEa[:], in_=b_sink[j].rearrange("(o d) -> o d", o=1).to_broadcast([128, 16])), "b0", writes=["SEa"])
                        S.op("act", lambda e: e.activation(out=SEa[:], in_=SEa[:], func=AF.Exp), reads=["SEa"], writes=["SEa"])
                        sev = SEa[:].rearrange("p (g i o) -> p g i o", g=4, i=2)
                        S.op("dve", lambda e: e.tensor_copy(out=SE[0:64, :, :], in_=sev[0:64, :, :, 0]), reads=["SEa"], writes=["SE0"])
                        S.op("dve", lambda e: e.tensor_copy(out=SE[64:128, :, :], in_=sev[64:128, :, :, 1]), reads=["SEa"], writes=["SE1"])
                        for s in range(NS):
                            with ExitStack() as p1:
                                xtA = [sbt(p1, f"xtA{i}", [128, D], F32) for i in range(1)]
                                Wn = {"xn": sbt(p1, "xn", [128, D], BF16), "junk": sbt(p1, "junk1", [128, D], BF16)}
                                for nm in ("ss1", "rt1", "rstd1"):
                                    Wn[nm] = sbt(p1, nm, [128, 1], F32)
                                hT1 = sbt(p1, "hT1", [128, KC, 128], BF16)
                                cst = [sbt(p1, f"cst{i}", [128, 2, 32], F32) for i in range(2)]
                                Ar = sbt(p1, "Ar", [128, 20, 2, 32], F32)
                                Br = sbt(p1, "Br", [128, 20, 2, 32], F32)
                                qkr = sbt(p1, "qkr", [128, 20, 2, 32], BF16)
                                kd = sbt(p1, "kd", [128, 4, 2, 64], BF16)
                                def do_tile1(s, tile):
                                    r = s if tile < NLT else 2
                                    cols = slice(tile * 128, (tile + 1) * 128)
                                    load_norm_hT(s, tile, r, xtA, tile, Wn, hT1[:, :, :], "hT1")

                                    def f_qkv(e):
                                        ins = None
                                        for blk in range(3):
                                            for kc in range(KC):
                                                ins = e.matmul(PS[:, 1 + blk, :], lhsT=hT1[:, kc, :], rhs=wqkv[:, kc, blk * 512:(blk + 1) * 512],
                                                               start=(kc == 0), stop=(kc == KC - 1))
                                        return ins
                                    S.op("pe", f_qkv, reads=["hT1", "wqkv"], writes=pk(1, 2, 3))
                                    X = PS[:, 1:4, :].rearrange("p b (h two d) -> p (b h) two d", two=2, d=32)
                                    S.op("act", lambda e, tile=tile: e.copy(out=Vt[:, tile, :], in_=PS[:, 3, 256:512]), reads=pk(3), writes=[f"Vt{tile}"])
                                    if tile < NLT:
                                        cs = cst[tile % 2]
                                        ck = f"cst{tile % 2}"
                                        S.dma("sp", lambda e, cs=cs, tile=tile: e.dma_start(out=cs[:, 0, :], in_=k_cos[tile * 128:(tile + 1) * 128, :]), ck + "c", writes=[ck + "c"])
                                        S.dma("sp", lambda e, cs=cs, tile=tile: e.dma_start(out=cs[:, 1, :], in_=k_sin[tile * 128:(tile + 1) * 128, :]), ck + "s", writes=[ck + "s"])
                                        S.op("dve", lambda e, cs=cs: e.tensor_tensor(out=Ar[:], in0=X[:, 0:20, :, :],
                                                                                  in1=cs[:, 0:1, :].unsqueeze(1).to_broadcast([128, 20, 2, 32]), op=ALU.mult),
                                             reads=pk(1, 2, 3) + [ck + "c"], writes=["Ar"])
                                        S.op("dve", lambda e, cs=cs: e.tensor_tensor(out=Br[:, :, 0, :], in0=X[:, 0:20, 1, :],
                                                                                  in1=cs[:, 1:2, :].to_broadcast([128, 20, 32]), op=ALU.mult),
                                             reads=pk(1, 2, 3) + [ck + "s"], writes=["Br0"])
                                        S.op("dve", lambda e, cs=cs: e.tensor_tensor(out=Br[:, :, 1, :], in0=X[:, 0:20, 0, :],
                                                                                  in1=cs[:, 1:2, :].to_broadcast([128, 20, 32]), op=ALU.mult),
                                             reads=pk(1, 2, 3) + [ck + "s"], writes=["Br1"])
                                        S.op("pool", lambda e: e.tensor_tensor(out=qkr[:, :, 0, :], in0=Ar[:, :, 0, :], in1=Br[:, :, 0, :], op=ALU.subtract),
                                             reads=["Ar", "Br0"], writes=["qkr0"])
                                        S.op("pool", lambda e: e.tensor_tensor(out=qkr[:, :, 1, :], in0=Ar[:, :, 1, :], in1=Br[:, :, 1, :], op=ALU.add),
                                             reads=["Ar", "Br1"], writes=["qkr1"])
                                    else:
                                        S.op("dve", lambda e: e.tensor_copy(out=qkr[:], in_=X[:, 0:20, :, :]), reads=pk(1, 2, 3), writes=["qkr0", "qkr1"])
                                    kv = qkr[:, 16:20, :, :].rearrange("p h two d -> p h (two d)")
                                    S.op("pool", lambda e: e.tensor_copy(out=kd[:, :, 0, :], in_=kv), reads=["qkr0", "qkr1"], writes=["kd0"])
                                    S.op("pool", lambda e: e.tensor_copy(out=kd[:, :, 1, :], in_=kv), reads=["qkr0", "qkr1"], writes=["kd1"])
                                    pTq = PS[:, 4, :].bitcast(BF16).rearrange("p (k t) -> p k t", k=KC)
                                    pTk = PS[:, 5, :].bitcast(BF16).rearrange("p (k t) -> p k t", k=KC)
                                    qf = qkr[:].rearrange("p h two d -> p (h two d)")

                                    def f_tq(e):
                                        ins = None
                                        for c in range(KC):
                                            ins = e.transpose(pTq[:, c, :], qf[:, c * 128:(c + 1) * 128], identb[:])
                                        for g in range(4):
                                            ins = e.transpose(pTk[:, g, :], kd[:, g, :, :].rearrange("p a d -> p (a d)"), identb[:])
                                        return ins
                                    S.op("pe", f_tq, reads=["qkr0", "qkr1", "kd0", "kd1", "identb"], writes=pk(4, 5))
                                    S.op("act", lambda e, cols=cols: e.copy(out=qT[:, :, cols], in_=pTq[:, :, :]), reads=pk(4), writes=[f"qT{tile}"])
                                    S.op("dve", lambda e, cols=cols: e.tensor_copy(out=kT[:, :, cols], in_=pTk[:, 0:4, :]), reads=pk(5), writes=[f"kT{tile}"])

                                for tile in range(NT):
                                    do_tile1(s, tile)
                                S.barrier()
                                S.emit()
                            with ExitStack() as p2:
                                W = alloc_post(p2)
                                PTt = [sbt(p2, f"PTt{i}", [128, 5, 2, 256], BF16) for i in range(1)]
                                oT = sbt(p2, "oT", [128, KC, 128], BF16)
                                dsb = sbt(p2, "dsb", [128, 256], F32)
                                cg = {"n": 0}

                                def do_qblock(s, qb):
                                    r = s if qb < NLT else 2
                                    qcols = slice(qb * 128, (qb + 1) * 128)
                                    if qb < NLT:
                                        keys = []
                                        if qb - 1 >= 0:
                                            keys.append((qb - 1, mlb))
                                        keys.append((qb, None))
                                        if qb + 1 < NLT:
                                            keys.append((qb + 1, mrb))
                                        keys += [(32, None), (33, None)]
                                    else:
                                        keys = [(32, None), (33, None)]
                                    nk = len(keys)
                                    for g in range(4):
                                        PT = PTt[cg["n"] % len(PTt)]
                                        ptk = f"PT{cg['n'] % len(PTt)}_"
                                        cg["n"] += 1
                                        for idx, (kj, mk) in enumerate(keys):
                                            kcols = slice(kj * 128, (kj + 1) * 128)
                                            ba = 2 + (idx % 2) * 2
                                            bb = ba + 1

                                            def f_qk(e, g=g, kcols=kcols, ba=ba, bb=bb):
                                                e.matmul(PS[:, ba, 0:256].rearrange("p (a b) -> p a b", a=2), lhsT=kT[0:64, g, kcols], rhs=qT[0:64, 2 * g:2 * g + 2, qcols],
                                                         start=True, stop=True)
                                                return e.matmul(PS[:, bb, 0:256].rearrange("p (a b) -> p a b", a=2), lhsT=kT[64:128, g, kcols], rhs=qT[64:128, 2 * g:2 * g + 2, qcols],
                                                                start=True, stop=True)
                                            S.op("pe", f_qk, reads=[f"kT{kj}", f"qT{qb}"], writes=pk(ba, bb))

                                            def f_ex(e, idx=idx, ba=ba, bb=bb, PT=PT):
                                                e.activation(out=PT[:, idx, 0, :], in_=PS[:, ba, 0:256], func=AF.Exp, scale=0.125)
                                                return e.activation(out=PT[:, idx, 1, :], in_=PS[:, bb, 0:256], func=AF.Exp, scale=0.125)
                                            S.op("act", f_ex, reads=pk(ba, bb), writes=[ptk + str(idx)])
                                            if mk is not None:
                                                S.op("dve", lambda e, idx=idx, mk=mk, PT=PT: e.tensor_tensor(
                                                    out=PT[:, idx, :, :].rearrange("p a (h q) -> p (a h) q", h=2),
                                                    in0=PT[:, idx, :, :].rearrange("p a (h q) -> p (a h) q", h=2),
                                                    in1=mk[:].unsqueeze(1).to_broadcast([128, 4, 128]), op=ALU.mult),
                                                    reads=[ptk + str(idx)], writes=[ptk + str(idx)])

                                        def f_pv(e, g=g, PT=PT):
                                            ins = None
                                            for idx, (kj, mk) in enumerate(keys):
                                                st_, sp_ = (idx == 0), (idx == nk - 1)
                                                e.matmul(PS[0:64, 0, 0:256], lhsT=Vt[:, kj, g * 64:(g + 1) * 64], rhs=PT[:, idx, 0, :], start=st_, stop=sp_)
                                                e.matmul(PS[64:128, 0, 0:256], lhsT=Vt[:, kj, g * 64:(g + 1) * 64], rhs=PT[:, idx, 1, :], start=st_, stop=sp_)
                                                e.matmul(PS[0:64, 1, 0:256], lhsT=onesb[:, 0:64], rhs=PT[:, idx, 0, :], start=st_, stop=sp_)
                                                ins = e.matmul(PS[64:128, 1, 0:256], lhsT=onesb[:, 0:64], rhs=PT[:, idx, 1, :], start=st_, stop=sp_)
                                            return ins
                                        S.op("pe", f_pv, reads=[ptk + str(i) for i in range(nk)] + [f"Vt{kj}" for kj, _ in keys] + ["onesb"], writes=pk(0, 1))
                                        S.op("dve", lambda e, g=g: e.tensor_tensor(out=dsb[:].rearrange("p (i q) -> p i q", i=2), in0=PS[:, 1, 0:256].rearrange("p (i q) -> p i q", i=2),
                                                                                in1=SE[:, g, :].unsqueeze(2).to_broadcast([128, 2, 128]), op=ALU.add),
                                             reads=pk(1) + ["SE0", "SE1"], writes=["dsb"])
                                        S.op("dve", lambda e: e.reciprocal(out=dsb[:], in_=dsb[:]), reads=["dsb"], writes=["dsb"])
                                        S.op("dve", lambda e, g=g: e.tensor_tensor(out=oT[:, 2 * g:2 * g + 2, :], in0=PS[:, 0, 0:256].rearrange("p (i q) -> p i q", i=2),
                                                                                in1=dsb[:].rearrange("p (i q) -> p i q", i=2), op=ALU.mult),
                                             reads=pk(0) + ["dsb"], writes=[f"oT{g}"])
                                    for half in range(2):
                                        def f_wo(e, half=half):
                                            ins = None
                                            for c in range(KC):
                                                ins = e.matmul(PS[:, 6 + half, :], lhsT=oT[:, c, :], rhs=wo[:, c, half * 512:(half + 1) * 512],
                                                               start=(c == 0), stop=(c == KC - 1))
                                            return ins
                                        S.op("pe", f_wo, reads=[f"oT{g}" for g in range(4)] + ["wo"], writes=pk(6 + half))
                                    post_tile(s, qb, r, 6, W)

                                for qb in range(NT):
                                    do_qblock(s, qb)
                                S.barrier()
                                S.emit()

                if skip_moe:
                    continue
                IDX = sbt(lst, "IDX", [128, NS, NE, 4], I32)
                GAT = sbt(lst, "GAT", [128, NS, NE, 4], F32)
                IDXC = sbt(lst, "IDXC", [32, NS, NE], I32)
                GATC = sbt(lst, "GATC", [32, NS, NE], F32)
                with ExitStack() as ph:
                    LO = sbt(ph, "LO", [128, NS, 2, NE], F32)
                    MID = sbt(ph, "MID", [128, NS, 2, NE], F32)
                    GE = sbt(ph, "GE", [128, NS, 2, NE], F32)
                    CMP = sbt(ph, "CMP", [128, NS, NT, NE], BF16)
                    CNTP = sbt(ph, "CNTP", [128, NS, 2, NE], F32)
                    S.op("dve", lambda e: e.memset(LO[:], 0.0), writes=["LO"])
                    for it in range(NBIS):
                        wv = 2.0 ** -(it + 1)
                        S.op("dve", lambda e, wv=wv: e.tensor_scalar(out=MID[:], in0=LO[:], scalar1=wv, scalar2=None, op0=ALU.add), reads=["LO"], writes=["MID"])
                        S.op("dve", lambda e: e.tensor_tensor(out=CMP[:, :, 0:NLT, :], in0=AFF[:, :, 0:NLT, :],
                                                             in1=MID[:, :, 0:1, :].to_broadcast([128, NS, NLT, NE]), op=ALU.is_ge),
                             reads=["AFF", "MID"], writes=["CMPl"])
                        S.op("dve", lambda e: e.tensor_tensor(out=CMP[:, :, NLT:NT, :], in0=AFF[:, :, NLT:NT, :],
                                                             in1=MID[:, :, 1:2, :].to_broadcast([128, NS, 2, NE]), op=ALU.is_ge),
                             reads=["AFF", "MID"], writes=["CMPc"])
                        S.op("dve", lambda e: e.tensor_reduce(out=CNTP[:, :, 0, :], in_=CMP[:, :, 0:NLT, :].rearrange("p s t e -> p s e t"), axis=AX.X, op=ALU.add),
                             reads=["CMPl"], writes=["CNTPl"])
                        S.op("dve", lambda e: e.tensor_reduce(out=CNTP[:, :, 1, :], in_=CMP[:, :, NLT:NT, :].rearrange("p s t e -> p s e t"), axis=AX.X, op=ALU.add),
                             reads=["CMPc"], writes=["CNTPc"])
                        S.op("pe", lambda e: e.matmul(PS[:, 0, 0:64], lhsT=onesF[:], rhs=CNTP[:].rearrange("p s a e -> p (s a e)"), start=True, stop=True),
                             reads=["CNTPl", "CNTPc", "onesF"], writes=pk(0))
                        pc = PS[:, 0, 0:64].rearrange("p (s a e) -> p s a e", s=NS, a=2)
                        S.op("dve", lambda e, pc=pc: e.tensor_scalar(out=GE[:, :, 0, :], in0=pc[:, :, 0, :], scalar1=float(CAP_L), scalar2=None, op0=ALU.is_ge),
                             reads=pk(0), writes=["GEl"])
                        S.op("dve", lambda e, pc=pc: e.tensor_scalar(out=GE[:, :, 1, :], in0=pc[:, :, 1, :], scalar1=float(CAP_C), scalar2=None, op0=ALU.is_ge),
                             reads=pk(0), writes=["GEc"])
                        S.op("dve", lambda e, wv=wv: e.scalar_tensor_tensor(out=LO[:], in0=GE[:], scalar=wv, in1=LO[:], op0=ALU.mult, op1=ALU.add),
                             reads=["GEl", "GEc", "LO"], writes=["LO"])
                    SEL = CMP
                    OFFS = sbt(ph, "OFFS", [128, NS, NT, NE], F32)
                    POS = sbt(ph, "POS", [128, NS, NT, NE], F32)
                    LT = sbt(ph, "LT", [128, NS, NT, NE], F32)
                    LOI = sbt(ph, "LOI", [128, NS, NT, NE], F32)
                    HI = sbt(ph, "HI", [128, NS, NT, NE], F32)
                    VALS = sbt(ph, "VALS", [128, NS, NT, NE, 4], BF16)
                    ALf = sbt(ph, "ALf", [128, NS, NT, NE], F32)
                    Hh = [sbt(ph, f"Hh{i}", [128, NLT, 128], BF16) for i in range(2)]
                    L4 = [sbt(ph, f"L4{i}", [128, NLT, 4], BF16) for i in range(2)]
                    Rr = [sbt(ph, f"Rr{i}", [128, NLT, 4, 4], BF16) for i in range(2)]
                    Hc = sbt(ph, "Hc", [128, 2, NE, 32], BF16)
                    IDXF = sbt(ph, "IDXF", [128, NS, NE, 4], F32)
                    pisb = sbt(ph, "pisb", [128, 2, 16], F32)
                    picsb = sbt(ph, "picsb", [32, 64], F32)
                    IDXCF = sbt(ph, "IDXCF", [32, NS, NE], F32)
                    S.op("dve", lambda e: e.tensor_tensor(out=SEL[:, :, 0:NLT, :], in0=AFF[:, :, 0:NLT, :],
                                                         in1=LO[:, :, 0:1, :].to_broadcast([128, NS, NLT, NE]), op=ALU.is_ge), reads=["AFF", "LO"], writes=["CMPl"])
                    S.op("dve", lambda e: e.tensor_tensor(out=SEL[:, :, NLT:NT, :], in0=AFF[:, :, NLT:NT, :],
                                                         in1=LO[:, :, 1:2, :].to_broadcast([128, NS, 2, NE]), op=ALU.is_ge), reads=["AFF", "LO"], writes=["CMPc"])
                    self_flat = SEL[:].rearrange("p s t e -> p (s t e)")
                    NTOT = NS * NT * NE

                    def f_cs(e):
                        ins = None
                        for (c0, c1, b) in ((0, 512, 0), (512, 1024, 1), (1024, NTOT, 2)):
                            e.matmul(PS[:, b, 0:c1 - c0], lhsT=upperb[:], rhs=self_flat[:, c0:c1], start=True, stop=True)
                            ins = e.matmul(PS[:, 3 + b, 0:c1 - c0], lhsT=onesb[:], rhs=self_flat[:, c0:c1], start=True, stop=True)
                        return ins
                    S.op("pe", f_cs, reads=["CMPl", "CMPc", "upperb", "onesb"], writes=pk(0, 1, 2, 3, 4, 5))
                    Wp = PS[:, 0:3, :].rearrange("p b n -> p (b n)")[:, 0:NTOT].rearrange("p (s t e) -> p s t e", s=NS, t=NT)
                    Tp = PS[:, 3:6, :].rearrange("p b n -> p (b n)")[:, 0:NTOT].rearrange("p (s t e) -> p s t e", s=NS, t=NT)
                    S.op("dve", lambda e: e.memset(OFFS[:], 0.0), writes=["OFFS"])
                    for t in range(1, NLT):
                        S.op("dve", lambda e, t=t: e.tensor_tensor(out=OFFS[:, :, t, :], in0=OFFS[:, :, t - 1, :], in1=Tp[:, :, t - 1, :], op=ALU.add),
                             reads=pk(3, 4, 5) + ["OFFS"], writes=["OFFS"])
                    S.op("dve", lambda e: e.tensor_copy(out=OFFS[:, :, NLT + 1, :], in_=Tp[:, :, NLT, :]), reads=pk(3, 4, 5) + ["OFFS"], writes=["OFFS"])
                    S.op("dve", lambda e: e.tensor_tensor(out=POS[:], in0=Wp, in1=OFFS[:], op=ALU.add), reads=pk(0, 1, 2) + ["OFFS"], writes=["POS"])
                    S.op("dve", lambda e: e.tensor_scalar(out=LT[:, :, 0:NLT, :], in0=POS[:, :, 0:NLT, :], scalar1=float(CAP_L), scalar2=None, op0=ALU.is_lt),
                         reads=["POS"], writes=["LTl"])
                    S.op("dve", lambda e: e.tensor_scalar(out=LT[:, :, NLT:NT, :], in0=POS[:, :, NLT:NT, :], scalar1=float(CAP_C), scalar2=None, op0=ALU.is_lt),
                         reads=["POS"], writes=["LTc"])
                    S.op("dve", lambda e: e.tensor_tensor(out=LT[:], in0=LT[:], in1=SEL[:], op=ALU.mult), reads=["LTl", "LTc", "CMPl", "CMPc"], writes=["LT"])
                    S.op("dve", lambda e: e.scalar_tensor_tensor(out=POS[:], in0=POS[:], scalar=1.0, in1=LT[:], op0=ALU.add, op1=ALU.mult),
                         reads=["POS", "LT"], writes=["POS"])
                    S.op("dve", lambda e: e.tensor_scalar(out=POS[:], in0=POS[:], scalar1=-1.0, scalar2=None, op0=ALU.add), reads=["POS"], writes=["POS"])
                    S.op("dve", lambda e: e.tensor_scalar(out=LOI[:], in0=POS[:], scalar1=128.0, scalar2=None, op0=ALU.is_ge), reads=["POS"], writes=["LOI"])
                    for thr in (256.0, 384.0):
                        S.op("dve", lambda e, thr=thr: e.scalar_tensor_tensor(out=LOI[:], in0=POS[:], scalar=thr, in1=LOI[:], op0=ALU.is_ge, op1=ALU.add),
                             reads=["POS", "LOI"], writes=["LOI"])
                    S.op("dve", lambda e: e.scalar_tensor_tensor(out=HI[:], in0=LOI[:], scalar=-128.0, in1=POS[:], op0=ALU.mult, op1=ALU.add),
                         reads=["POS", "LOI"], writes=["HI"])
                    S.op("dve", lambda e: e.tensor_copy(out=VALS[:, :, :, :, 0:2], in_=ptf[:].unsqueeze(1).unsqueeze(3).to_broadcast([128, NS, NT, NE, 2])),
                         reads=["ptf"], writes=["VALS01"])
                    S.op("dve", lambda e: e.tensor_copy(out=VALS[:, :, :, :, 2], in_=AFF[:]), reads=["AFF"], writes=["VALS2"])
                    S.op("dve", lambda e: e.tensor_tensor(out=ALf[:], in0=AFF[:], in1=VALS[:, :, :, :, 2], op=ALU.subtract), reads=["AFF", "VALS2"], writes=["ALf"])
                    S.op("dve", lambda e: e.tensor_copy(out=VALS[:, :, :, :, 3], in_=ALf[:]), reads=["ALf"], writes=["VALS3"])
                    vkeys = ["VALS01", "VALS2", "VALS3"]
                    cnt = 0
                    for s in range(NS):
                        for ee in range(NE):
                            b = cnt % 2
                            cnt += 1
                            S.op("dve", lambda e, s=s, ee=ee, b=b: e.tensor_tensor(
                                out=Hh[b][:], in0=iotaf[:].unsqueeze(1).to_broadcast([128, NLT, 128]),
                                in1=HI[:, s, 0:NLT, ee:ee + 1].to_broadcast([128, NLT, 128]), op=ALU.is_equal),
                                reads=["HI", "iotaf"], writes=[f"Hh{b}"])
                            S.op("dve", lambda e, s=s, ee=ee, b=b: e.tensor_tensor(
                                out=L4[b][:], in0=iotaf[:, 0:4].unsqueeze(1).to_broadcast([128, NLT, 4]),
                                in1=LOI[:, s, 0:NLT, ee:ee + 1].to_broadcast([128, NLT, 4]), op=ALU.is_equal),
                                reads=["LOI", "iotaf"], writes=[f"L4{b}"])
                            S.op("pool", lambda e, s=s, ee=ee, b=b: e.tensor_tensor(
                                out=Rr[b][:], in0=L4[b][:].unsqueeze(3).to_broadcast([128, NLT, 4, 4]),
                                in1=VALS[:, s, 0:NLT, ee, :].unsqueeze(2).to_broadcast([128, NLT, 4, 4]), op=ALU.mult),
                                reads=[f"L4{b}"] + vkeys, writes=[f"Rr{b}"])
                            bnk = 6 + b

                            def f_oh(e, b=b, bnk=bnk):
                                ins = None
                                for t in range(NLT):
                                    ins = e.matmul(PS[:, bnk, 0:16], lhsT=Hh[b][:, t, :], rhs=Rr[b][:, t, :, :].rearrange("p a v -> p (a v)"),
                                                   start=(t == 0), stop=(t == NLT - 1))
                                return ins
                            S.op("pe", f_oh, reads=[f"Hh{b}", f"Rr{b}"], writes=pk(bnk))
                            S.op("act", lambda e, b=b, bnk=bnk: e.copy(out=pisb[:, b, :], in_=PS[:, bnk, 0:16]), reads=pk(bnk), writes=[f"pisb{b}"])
                            pi = pisb[:, b, :].rearrange("p (a v) -> p a v", a=4)
                            S.op("dve", lambda e, s=s, ee=ee, pi=pi: e.scalar_tensor_tensor(out=IDXF[:, s, ee, :], in0=pi[:, :, 1], scalar=128.0, in1=pi[:, :, 0],
                                                                                         op0=ALU.mult, op1=ALU.add), reads=[f"pisb{b}"], writes=["IDXF"])
                            S.op("dve", lambda e, s=s, ee=ee, pi=pi: e.tensor_tensor(out=GAT[:, s, ee, :], in0=pi[:, :, 2], in1=pi[:, :, 3], op=ALU.add),
                                 reads=[f"pisb{b}"], writes=["GAT"])
                        S.op("dve", lambda e, s=s: e.tensor_tensor(out=Hc[:], in0=iotaf[:, 0:32].unsqueeze(1).unsqueeze(1).to_broadcast([128, 2, NE, 32]),
                                                                in1=POS[:, s, NLT:NT, :].unsqueeze(3).to_broadcast([128, 2, NE, 32]), op=ALU.is_equal),
                             reads=["POS", "iotaf"], writes=["Hc"])

                        def f_c(e, s=s):
                            ins = None
                            for ee in range(NE):
                                for t in range(2):
                                    ins = e.matmul(PS[0:32, 5, ee * 4:(ee + 1) * 4], lhsT=Hc[:, t, ee, :], rhs=VALS[:, s, NLT + t, ee, :],
                                                   start=(t == 0), stop=(t == 1))
                            return ins
                        S.op("pe", f_c, reads=["Hc"] + vkeys, writes=pk(5))
                        S.op("act", lambda e: e.copy(out=picsb[:], in_=PS[0:32, 5, 0:64]), reads=pk(5), writes=["picsb"])
                        pic = picsb[:].rearrange("p (a v) -> p a v", a=NE)
                        S.op("dve", lambda e, s=s, pic=pic: e.scalar_tensor_tensor(out=IDXCF[:, s, :], in0=pic[:, :, 1], scalar=128.0, in1=pic[:, :, 0],
                                                                                op0=ALU.mult, op1=ALU.add), reads=["picsb"], writes=["IDXCF"])
                        S.op("dve", lambda e, s=s, pic=pic: e.tensor_tensor(out=GATC[:, s, :], in0=pic[:, :, 2], in1=pic[:, :, 3], op=ALU.add),
                             reads=["picsb"], writes=["GATC"])
                    S.op("dve", lambda e: e.tensor_scalar(out=IDXF[:, 1, :, :], in0=IDXF[:, 1, :, :], scalar1=float(NTOK), scalar2=None, op0=ALU.add),
                         reads=["IDXF"], writes=["IDXF"])
                    S.op("dve", lambda e: e.tensor_scalar(out=IDXCF[:, 1, :], in0=IDXCF[:, 1, :], scalar1=float(NTOK), scalar2=None, op0=ALU.add),
                         reads=["IDXCF"], writes=["IDXCF"])
                    S.op("dve", lambda e: e.tensor_copy(out=IDX[:], in_=IDXF[:]), reads=["IDXF"], writes=["IDX"])
                    S.op("dve", lambda e: e.tensor_copy(out=IDXC[:], in_=IDXCF[:]), reads=["IDXCF"], writes=["IDXC"])
                    if dbg_route:
                        for s in range(NS):
                            S.dma("sp", lambda e, s=s: e.dma_start(out=dbg_d[1, s * 128:(s + 1) * 128, 0:544], in_=AFF[:, s].rearrange("p t e -> p (t e)")), "dr0", reads=["AFF"], writes=["dbgr0"])
                            S.dma("sp", lambda e, s=s: e.dma_start(out=dbg_d[1, 768 + s * 128:768 + (s + 1) * 128, 0:544], in_=POS[:, s].rearrange("p t e -> p (t e)")), "dr5", reads=["POS"], writes=["dbgr5"])
                            S.dma("sp", lambda e, s=s: e.dma_start(out=dbg_d[1, 1024 + s * 128:1024 + (s + 1) * 128, 0:544], in_=HI[:, s].rearrange("p t e -> p (t e)")), "dr6", reads=["HI"], writes=["dbgr6"])
                        S.dma("sp", lambda e: e.dma_start(out=dbg_d[1, 256:384, 0:128], in_=IDXF[:].rearrange("p s e k -> p (s e k)")), "dr1", reads=["IDXF"], writes=["dbgr1"])
                        S.dma("sp", lambda e: e.dma_start(out=dbg_d[1, 384:512, 0:128], in_=GAT[:].rearrange("p s e k -> p (s e k)")), "dr2", reads=["GAT"], writes=["dbgr2"])
                        S.dma("sp", lambda e: e.dma_start(out=dbg_d[1, 512:640, 0:64], in_=LO[:].rearrange("p s a e -> p (s a e)")), "dr3", reads=["LO"], writes=["dbgr3"])
                        S.dma("sp", lambda e: e.dma_start(out=dbg_d[1, 640:672, 0:32], in_=IDXCF[:].rearrange("p s e -> p (s e)")), "dr4", reads=["IDXCF"], writes=["dbgr4"])
                    S.barrier()
                    S.emit()

                if dbg_route:
                    continue
                mod_phase([10, 11], False)

                with ExitStack() as ph:
                    WP = [sbt(ph, f"WP{i}", [128, 4096], BF16) for i in range(8)]
                    XG = [[sbt(ph, f"XG{p}{s}", [128, 5, D], BF16) for s in range(NS)] for p in range(2)]
                    XgT = [sbt(ph, f"XgT{s}", [128, KC, 544], BF16) for s in range(NS)]
                    gT = [sbt(ph, f"gT{s}", [128, 16, 544], BF16) for s in range(NS)]
                    sa = [sbt(ph, f"sa{i}", [128, 544], BF16) for i in range(2)]
                    ysb = [sbt(ph, f"ysb{i}", [128, D], F32) for i in range(2)]
                    state = {"pc": 0, "sa": 0, "y": 0}
                    piece_slot = {}

                    def issue_piece(ee, i):
                        pidx = ee * 12 + i
                        slot = pidx % 8
                        piece_slot[(ee, i)] = slot
                        if i < 8:
                            wsrc = e_w1 if i % 2 == 0 else e_w3
                            fg = i // 2
                            src = wsrc[L, ee][:, fg * 512:(fg + 1) * 512].rearrange("(kc p) n -> p kc n", p=128)
                            dst = WP[slot][:].rearrange("p (kc n) -> p kc n", kc=KC)
                        else:
                            fq = i - 8
                            src = e_w2[L, ee][fq * 512:(fq + 1) * 512, :].rearrange("(fc p) n -> p fc n", p=128)
                            dst = WP[slot][:].rearrange("p (fc n) -> p fc n", fc=4)
                        S.dma("pool", lambda e, src=src, dst=dst: e.dma_start(out=dst, in_=src), f"wp{slot}", writes=[f"WP{slot}"])

                    def issue_gathers(ee):
                        par = ee % 2
                        for s in range(NS):
                            for k in range(4):
                                S.dma("pool", lambda e, s=s, k=k, par=par, ee=ee: e.indirect_dma_start(
                                    out=XG[par][s][:, k, :], out_offset=None, in_=FDflat,
                                    in_offset=bass.IndirectOffsetOnAxis(ap=IDX[:, s, ee, k:k + 1], axis=0)),
                                    f"xg{par}{s}{k}", reads=["IDX", f"FD{s}"], writes=[f"XG{par}{s}{k}"])
                            S.dma("pool", lambda e, s=s, par=par, ee=ee: e.indirect_dma_start(
                                out=XG[par][s][0:32, 4, :], out_offset=None, in_=FDflat,
                                in_offset=bass.IndirectOffsetOnAxis(ap=IDXC[0:32, s, ee:ee + 1], axis=0)),
                                f"xg{par}{s}4", reads=["IDXC", f"FD{s}"], writes=[f"XG{par}{s}4"])

                    issue_gathers(0)
                    for i in range(8):
                        issue_piece(0, i)
                    def do_expert(ee):
                        par = ee % 2
                        if ee + 1 < NE:
                            issue_gathers(ee + 1)
                        pT = PS[:, 0, :].bitcast(BF16).rearrange("p (k t) -> p k t", k=KC)
                        for s in range(NS):
                            for k in range(5):
                                rows = 128 if k < 4 else 32
                                r = s if k < 4 else 2

                                def f_tr(e, s=s, k=k, rows=rows, par=par):
                                    ins = None
                                    for kc in range(KC):
                                        ins = e.transpose(pT[:, kc, 0:rows], XG[par][s][0:rows, k, kc * 128:(kc + 1) * 128], identb[0:rows, 0:rows])
                                    return ins
                                S.op("pe", f_tr, reads=[f"XG{par}{s}{k}", "identb"], writes=pk(0))

                                def f_ev(e, s=s, k=k, rows=rows, r=r):
                                    ins = None
                                    for kc in range(KC):
                                        ins = e.activation(out=XgT[s][:, kc, k * 128:k * 128 + rows], in_=pT[:, kc, 0:rows], func=AF.Identity,
                                                           scale=gs2T[:, kc, r:r + 1], bias=sh2T[:, kc, r:r + 1])
                                    return ins
                                S.op("act", f_ev, reads=pk(0), writes=[f"XgT{s}_{k}"])
                        xgk = [[f"XgT{s}_{k}" for k in range(5)] for s in range(NS)]
                        if dbg_exp and ee == 0:
                            S.dma("pool", lambda e: e.dma_start(out=dbg_d[1, 0:128, :], in_=XG[0][0][:, 0, :]), "de0", reads=["XG000"], writes=["dbge0"])
                            S.dma("pool", lambda e: e.dma_start(out=dbg_d[1, 128:256, 0:544], in_=XgT[0][:, 0, :]), "de1", reads=xgk[0], writes=["dbge1"])
                            S.dma("pool", lambda e: e.dma_start(out=dbg_d[1, 512:640, :], in_=FD[0, 0:128, :]), "de4", writes=["dbge4"])
                            S.dma("sp", lambda e: e.dma_start(out=dbg_d[1, 640:768, 0:128], in_=IDX[:].rearrange("p s e k -> p (s e k)").bitcast(F32)), "de5", reads=["IDX"], writes=["dbge5"])
                        for fg in range(4):
                            s1 = piece_slot[(ee, 2 * fg)]
                            s3 = piece_slot[(ee, 2 * fg + 1)]
                            W1p = WP[s1][:].rearrange("p (kc n) -> p kc n", kc=KC)
                            W3p = WP[s3][:].rearrange("p (kc n) -> p kc n", kc=KC)
                            for f4 in range(4):
                                fc = fg * 4 + f4
                                for s in range(NS):
                                    sab = sa[state["sa"] % 2]
                                    sak = f"sa{state['sa'] % 2}"
                                    state["sa"] += 1

                                    def f_a(e, Wp_=W1p, f4=f4, s=s, b0=1, b1=2):
                                        ins = None
                                        for kc in range(KC):
                                            e.matmul(PS[:, b0, :], lhsT=Wp_[:, kc, f4 * 128:(f4 + 1) * 128], rhs=XgT[s][:, kc, 0:512], start=(kc == 0), stop=(kc == KC - 1))
                                            ins = e.matmul(PS[:, b1, 0:32], lhsT=Wp_[:, kc, f4 * 128:(f4 + 1) * 128], rhs=XgT[s][:, kc, 512:544], start=(kc == 0), stop=(kc == KC - 1))
                                        return ins
                                    S.op("pe", f_a, reads=[f"WP{s1}"] + xgk[s], writes=pk(1, 2))

                                    def f_si(e, sab=sab):
                                        e.activation(out=sab[:, 0:512], in_=PS[:, 1, :], func=AF.Silu)
                                        return e.activation(out=sab[:, 512:544], in_=PS[:, 2, 0:32], func=AF.Silu)
                                    S.op("act", f_si, reads=pk(1, 2), writes=[sak])
                                    S.op("pe", lambda e, Wp_=W3p, f4=f4, s=s: f_a(e, Wp_, f4, s, 3, 4), reads=[f"WP{s3}"] + xgk[s], writes=pk(3, 4))

                                    def f_mu(e, sab=sab, s=s, fc=fc):
                                        e.tensor_tensor(out=gT[s][:, fc, 0:512], in0=PS[:, 3, :], in1=sab[:, 0:512], op=ALU.mult)
                                        return e.tensor_tensor(out=gT[s][:, fc, 512:544], in0=PS[:, 4, 0:32], in1=sab[:, 512:544], op=ALU.mult)
                                    S.op("dve", f_mu, reads=pk(3, 4) + [sak], writes=[f"gT{s}_{fc}"])
                            if fg < 2:
                                issue_piece(ee, 8 + 2 * fg)
                                issue_piece(ee, 9 + 2 * fg)
                            elif ee + 1 < NE:
                                issue_piece(ee + 1, 2 * (fg - 2))
                                issue_piece(ee + 1, 2 * (fg - 2) + 1)
                        if dbg_exp and ee == 0:
                            S.dma("pool", lambda e: e.dma_start(out=dbg_d[1, 256:384, 0:544], in_=gT[0][:, 0, :]), "de2", reads=["gT0_0"], writes=["dbge2"])
                        w2s = [piece_slot[(ee, 8 + q)] for q in range(4)]
                        W2p = [WP[sl][:].rearrange("p (fc n) -> p fc n", fc=4) for sl in w2s]
                        for s in range(NS):
                            for k in range(5):
                                rows = 128 if k < 4 else 32
                                r = s if k < 4 else 2
                                for half in range(2):
                                    def f_y(e, s=s, k=k, rows=rows, half=half):
                                        ins = None
                                        for fc in range(16):
                                            ins = e.matmul(PS[0:rows, 5 + half, :], lhsT=gT[s][:, fc, k * 128:k * 128 + rows],
                                                           rhs=W2p[fc // 4][:, fc % 4, half * 512:(half + 1) * 512], start=(fc == 0), stop=(fc == 15))
                                        return ins
                                    S.op("pe", f_y, reads=[f"gT{s}_{fc}" for fc in range(16)] + [f"WP{sl}" for sl in w2s], writes=pk(5 + half))
                                yb = ysb[state["y"] % 2]
                                yk = f"ysb{state['y'] % 2}"
                                state["y"] += 1
                                gate_ap = GAT[:, s, ee, k:k + 1] if k < 4 else GATC[0:32, s, ee:ee + 1]
                                S.op("dve", lambda e, yb=yb, rows=rows, gate_ap=gate_ap, r=r: e.scalar_tensor_tensor(
                                    out=yb[0:rows, :].rearrange("p (a b) -> p a b", a=2), in0=PS[0:rows, 5:7, :], scalar=gate_ap,
                                    in1=G2B[0:rows, r, :].rearrange("p (a b) -> p a b", a=2), op0=ALU.mult, op1=ALU.mult),
                                    reads=pk(5, 6) + ["GAT", "GATC"], writes=[yk])
                                idx_ap = IDX[:, s, ee, k:k + 1] if k < 4 else IDXC[0:32, s, ee:ee + 1]
                                if dbg_exp and ee == 0 and s == 0 and k == 0:
                                    S.dma("sp", lambda e, yb=yb: e.dma_start(out=dbg_d[1, 384:512, :], in_=yb[:]), "de3", reads=[yk], writes=["dbge3"])
                                S.dma("pool", lambda e, s=s, yb=yb, rows=rows, idx_ap=idx_ap: e.indirect_dma_start(
                                    out=XRflat, out_offset=bass.IndirectOffsetOnAxis(ap=idx_ap, axis=0), in_=yb[0:rows, :], in_offset=None, compute_op=ALU.add),
                                    f"scat{s}", reads=[yk, "IDX", "IDXC"], writes=[f"XRs{s}_{k}"], group=(k > 0))
                        if ee + 1 < NE:
                            for i in range(4, 8):
                                issue_piece(ee + 1, i)

                    for ee in range(NE):
                        do_expert(ee)
                    S.barrier()
                    S.emit()

        if dbg:
            for s in range(1 if (dbg_route or dbg_exp) else NS):
                S.dma("sp", lambda e, s=s: e.dma_start(out=dbg_d[s], in_=XR[s]), f"dbg{s}", writes=[f"dbg{s}"])
        with ExitStack() as ph:
            xt = [sbt(ph, f"fx{i}", [128, D], F32) for i in range(2)]
            yo = [sbt(ph, f"fy{i}", [128, D], F32) for i in range(2)]
            junk = sbt(ph, "fjunk", [128, D], BF16)
            gfin = sbt(ph, "gfin", [128, D], F32)
            S.dma("sp", lambda e: e.dma_start(out=gfin[:], in_=g_final.rearrange("(o d) -> o d", o=1).to_broadcast([128, D])),
                  "c7", writes=["gfin"])
            ss = sbt(ph, "fss", [128, 1], F32)
            rt = sbt(ph, "frt", [128, 1], F32)
            rstd = sbt(ph, "frstd", [128, 1], F32)
            cnt = 0
            for s in range(NS):
                for tile in range(NLT):
                    b = cnt % 2
                    cnt += 1
                    rows = slice(tile * 128, (tile + 1) * 128)
                    S.dma("sp", lambda e, s=s, rows=rows, b=b: e.dma_start(out=xt[b][:], in_=XR[s, rows, :]), f"fx{b}", writes=[f"fx{b}"])
                    S.op("act", lambda e, b=b: e.activation(out=junk[:], in_=xt[b][:], func=AF.Square, accum_out=ss[:]), reads=[f"fx{b}"], writes=["fjunk", "fss"])
                    S.op("act", lambda e: e.activation(out=rt[:], in_=ss[:], func=AF.Sqrt, scale=1.0 / D, bias=epst[:, 0:1]), reads=["fss"], writes=["frt"])
                    S.op("dve", lambda e: e.reciprocal(out=rstd[:], in_=rt[:]), reads=["frt"], writes=["frstd"])
                    S.op("dve", lambda e, b=b: e.scalar_tensor_tensor(out=yo[b][:], in0=xt[b][:], scalar=rstd[:, 0:1], in1=gfin[:], op0=ALU.mult, op1=ALU.mult),
                         reads=[f"fx{b}", "frstd", "gfin"], writes=[f"fy{b}"])
                    S.dma("sp", lambda e, s=s, rows=rows, b=b: e.dma_start(out=out_d[s, rows, :], in_=yo[b][:]), f"fy{b}", reads=[f"fy{b}"], writes=["out"])
            S.barrier()
            S.emit()
    return nc


def host_consts():
    t = np.arange(SEQ)
    row = (t // 64).astype(np.float32)
    col = (t % 64).astype(np.float32)
    inv = (10000.0 ** (-np.arange(16, dtype=np.float32) / 16)).astype(np.float32)
    ang = np.concatenate([row[:, None] * inv, col[:, None] * inv], axis=-1).astype(np.float32)
    p = np.arange(128)
    pt = np.zeros((128, NT, 2), np.float32)
    pt[:, :, 0] = p[:, None]
    pt[:, :, 1] = np.arange(NT)[None, :]
    return dict(
        k_ident=np.eye(128, dtype=np.float32),
        k_cos=np.cos(ang).astype(np.float32),
        k_sin=np.sin(ang).astype(np.float32),
        k_upper=(p[:, None] < p[None, :]).astype(np.float32),
        k_iota=np.broadcast_to(np.arange(128, dtype=np.float32)[None, :], (128, 128)).copy(),
        k_pt=pt,
        k_ml=(p[:, None] >= p[None, :]).astype(np.float32),
        k_mr=(p[:, None] <= p[None, :]).astype(np.float32),
    )


_NC_CACHE = {}


def kernel(**inputs):
    n = 8
    if "nc" not in _NC_CACHE:
        _NC_CACHE["nc"] = build()
    nc = _NC_CACHE["nc"]
    consts = host_consts()
    shared = {k: np.ascontiguousarray(np.asarray(v, dtype=np.float32)) for k, v in inputs.items() if k not in ("x", "c", "ctx")}
    x = np.asarray(inputs["x"], dtype=np.float32)
    c = np.asarray(inputs["c"], dtype=np.float32)
    ctx = np.asarray(inputs["ctx"], dtype=np.float32)
    in_maps = []
    for i in range(n):
        m = dict(shared)
        m.update(consts)
        m["x"] = np.ascontiguousarray(x[2 * i:2 * i + 2])
        m["c"] = np.ascontiguousarray(c[2 * i:2 * i + 2])
        m["ctx"] = np.ascontiguousarray(ctx[2 * i:2 * i + 2])
        in_maps.append(m)
    res = run_bass_kernel_spmd(nc, in_maps, core_ids=list(range(n)))
    return np.concatenate([r["out"] for r in res.results], axis=0).astype(np.float32)
```

```python
import numpy as np
import ml_dtypes
from contextlib import ExitStack
import concourse.bass as bass
import concourse.mybir as mybir
from concourse.bass_utils import run_bass_kernel_spmd

F32 = mybir.dt.float32
BF16 = mybir.dt.bfloat16
I32 = mybir.dt.int32
AF = mybir.ActivationFunctionType
ALU = mybir.AluOpType
AX = mybir.AxisListType

EPOCH = 30000
COMPUTE = ("pe", "act", "dve", "pool")
ALL_ENG = ("pe", "act", "dve", "pool", "sp")


class Tok:
    __slots__ = ("sem", "val", "eng", "is_dma")

    def __init__(self, sem, val, eng, is_dma):
        self.sem, self.val, self.eng, self.is_dma = sem, val, eng, is_dma


class Sched:
    def __init__(self, nc, stack):
        self.nc = nc
        self.stack = stack
        self.ops = {e: [] for e in ALL_ENG}
        self.prog_sem = {}
        self.prog_cnt = {}
        self.dma_sem = {}
        self.known = {e: {} for e in ALL_ENG}
        self.lastw = {}
        self.readers = {}
        self.nsem = 0
        self.all_toks = {}
        self.nops = 0
        for e in COMPUTE:
            self._new_epoch(e)

    def _mk_sem(self, name):
        self.nsem += 1
        return self.stack.enter_context(self.nc.semaphore(name))

    def _new_epoch(self, e):
        self.prog_sem[e] = self._mk_sem(f"p_{e}_{self.nsem}")
        self.prog_cnt[e] = 0

    def _collect(self, eng, reads, writes, extra):
        deps = []
        for k in reads:
            t = self.lastw.get(k)
            if t is not None:
                deps.append(t)
        for k in writes:
            t = self.lastw.get(k)
            if t is not None:
                deps.append(t)
            deps.extend(self.readers.get(k, ()))
        deps.extend(extra)
        waits = {}
        kn = self.known[eng]
        for t in deps:
            if t is None:
                continue
            if (not t.is_dma) and t.eng == eng and eng == "pe":
                continue
            name = id(t.sem)
            if kn.get(name, 0) >= t.val:
                continue
            if name not in waits or waits[name].val < t.val:
                waits[name] = t
        for name, t in waits.items():
            kn[name] = t.val
        return list(waits.values())

    def _commit(self, tok, reads, writes):
        for k in reads:
            self.readers.setdefault(k, []).append(tok)
        for k in writes:
            self.lastw[k] = tok
            self.readers[k] = []
        self.all_toks[id(tok.sem)] = tok

    def op(self, eng, fn, reads=(), writes=(), extra=()):
        waits = self._collect(eng, reads, writes, extra)
        if self.prog_cnt[eng] >= EPOCH:
            self._new_epoch(eng)
        self.prog_cnt[eng] += 1
        tok = Tok(self.prog_sem[eng], self.prog_cnt[eng], eng, False)
        self.ops[eng].append((waits, fn, tok.sem, 1))
        self._commit(tok, reads, writes)
        self.nops += 1
        return tok

    def dma(self, eng, fn, key, reads=(), writes=(), extra=(), group=False):
        ent = self.dma_sem.get(key)
        if ent is None:
            ent = [self._mk_sem(f"d_{self.nsem}"), 0, None]
            self.dma_sem[key] = ent
        ex = list(extra)
        if ent[2] is not None and not group:
            ex.append(ent[2])
        waits = self._collect(eng, reads, writes, ex)
        ent[1] += 16
        tok = Tok(ent[0], ent[1], eng, True)
        ent[2] = tok
        self.ops[eng].append((waits, fn, tok.sem, 16))
        self._commit(tok, reads, writes)
        self.nops += 1
        return tok

    def barrier(self, engs=ALL_ENG):
        toks = list(self.all_toks.values())
        for e in engs:
            waits = []
            kn = self.known[e]
            for t in toks:
                name = id(t.sem)
                if kn.get(name, 0) >= t.val:
                    continue
                kn[name] = t.val
                waits.append(t)
            if waits:
                self.ops[e].append((waits, None, None, 0))
        self.lastw = {}
        self.readers = {}

    def emit(self):
        nc = self.nc
        ops = self.ops

        def run(engine, lst):
            for waits, fn, sem, inc in lst:
                for t in waits:
                    engine.wait_ge(t.sem, t.val)
                if fn is None:
                    continue
                inst = fn(engine)
                inst.then_inc(sem, inc)

        with nc.Block() as block:
            @block.tensor
            def _(e):
                run(e, ops["pe"])

            @block.scalar
            def _(e):
                run(e, ops["act"])

            @block.vector
            def _(e):
                run(e, ops["dve"])

            @block.gpsimd
            def _(e):
                run(e, ops["pool"])

            @block.sync
            def _(e):
                run(e, ops["sp"])
        self.ops = {e: [] for e in ALL_ENG}


D = 1024
KC = 8
SEQ = 4096
CTX = 256
NTOK = SEQ + CTX
NT = NTOK // 128
NLT = SEQ // 128
NS = 2
NE = 16
CAP_L = 512
CAP_C = 32
EPS = 1e-6
NBIS = 20


def build(NL=4, do_final=True, dbg=False, skip_moe=False, dbg_route=False, dbg_exp=False):
    nc = bass.Bass("TRN2", target_bir_lowering=False)

    def din(name, shape, dt=F32):
        return nc.dram_tensor(name, list(shape), dt, kind="ExternalInput").ap()

    x_in = din("x", [NS, SEQ, D])
    c_in = din("c", [NS, D])
    ctx_in = din("ctx", [NS, CTX, D])
    cctx_in = din("c_ctx", [D])
    w_mod = din("w_mod", [4, D, 6 * D])
    b_mod = din("b_mod", [4, 6 * D])
    g_nm = din("g_norm_mix", [4, D])
    g_nf = din("g_norm_ffn", [4, D])
    a_w_in = din("a_w_in", [2, D, 4096])
    a_b_in = din("a_b_in", [2, 4096])
    a_ln_g = din("a_ln_g", [2, 2048])
    a_ln_b = din("a_ln_b", [2, 2048])
    a_w_s = din("a_w_s", [2, 16, 128, 128])
    a_b_s = din("a_b_s", [2, 16, 128])
    a_w_out = din("a_w_out", [2, 2048, D])
    b_w_qkv = din("b_w_qkv", [2, D, 1536])
    b_sink = din("b_sink", [2, 16])
    b_w_o = din("b_w_o", [2, D, D])
    r_w = din("r_w", [4, D, NE])
    e_w1 = din("e_w1", [4, NE, D, 2048])
    e_w3 = din("e_w3", [4, NE, D, 2048])
    e_w2 = din("e_w2", [4, NE, 2048, D])
    g_final = din("g_final", [D])
    k_ident = din("k_ident", [128, 128])
    k_cos = din("k_cos", [SEQ, 32])
    k_sin = din("k_sin", [SEQ, 32])
    k_upper = din("k_upper", [128, 128])
    k_iota = din("k_iota", [128, 128])
    k_pt = din("k_pt", [128, NT, 2])
    k_ml = din("k_ml", [128, 128])
    k_mr = din("k_mr", [128, 128])

    out_d = nc.dram_tensor("out", [NS, SEQ, D], F32, kind="ExternalOutput").ap()
    XR = nc.dram_tensor("xr_scratch", [NS, NTOK, D], F32, kind="Internal").ap()
    FD = nc.dram_tensor("f_scratch", [NS, NTOK, D], BF16, kind="Internal").ap()
    XRflat = XR.rearrange("s t d -> (s t) d")
    FDflat = FD.rearrange("s t d -> (s t) d")
    dbg_d = nc.dram_tensor("dbg", [NS, NTOK, D], F32, kind="ExternalOutput").ap() if dbg else None

    with ExitStack() as gst:
        S = Sched(nc, gst)

        uniq = {"n": 0}

        def sbt(st, name, shape, dt):
            uniq["n"] += 1
            return st.enter_context(nc.sbuf_tensor(f"{name}_u{uniq['n']}", list(shape), dt))

        PS = gst.enter_context(nc.psum_tensor("PS", [128, 8, 512], F32))

        def pk(*bs):
            return [f"ps{b}" for b in bs]

        identf = sbt(gst, "identf", [128, 128], F32)
        identb = sbt(gst, "identb", [128, 128], BF16)
        onesb = sbt(gst, "onesb", [128, 128], BF16)
        onesF = sbt(gst, "onesF", [128, 128], F32)
        upperb = sbt(gst, "upperb", [128, 128], BF16)
        iotaf = sbt(gst, "iotaf", [128, 128], F32)
        ptf = sbt(gst, "ptf", [128, NT, 2], F32)
        mlb = sbt(gst, "mlb", [128, 128], BF16)
        mrb = sbt(gst, "mrb", [128, 128], BF16)
        epst = sbt(gst, "epst", [128, 1], F32)
        scT = sbt(gst, "scT", [128, KC, 4], F32)
        AFF = sbt(gst, "AFF", [128, NS, NT, NE], F32)

        S.dma("sp", lambda e: e.dma_start(out=identf[:], in_=k_ident), "c0", writes=["identf"])
        S.dma("sp", lambda e: e.dma_start(out=iotaf[:], in_=k_iota), "c1", writes=["iotaf"])
        S.dma("sp", lambda e: e.dma_start(out=ptf[:], in_=k_pt), "c2", writes=["ptf"])
        S.dma("pool", lambda e: e.dma_start(out=identb[:], in_=k_ident), "c3", writes=["identb"])
        S.dma("pool", lambda e: e.dma_start(out=upperb[:], in_=k_upper), "c4", writes=["upperb"])
        S.dma("pool", lambda e: e.dma_start(out=mlb[:], in_=k_ml), "c5", writes=["mlb"])
        S.dma("pool", lambda e: e.dma_start(out=mrb[:], in_=k_mr), "c6", writes=["mrb"])
        S.op("dve", lambda e: e.memset(onesb[:], 1.0), writes=["onesb"])
        S.op("dve", lambda e: e.memset(onesF[:], 1.0), writes=["onesF"])
        S.op("dve", lambda e: e.memset(epst[:], EPS), writes=["epst"])
        S.op("dve", lambda e: e.memset(AFF[:], 0.0), writes=["AFF"])
        S.op("dve", lambda e: e.memset(scT[:], 0.0), writes=["scT"])
        for s in range(NS):
            S.dma("sp", lambda e, s=s: e.dma_start(out=scT[:, :, s:s + 1], in_=c_in[s].rearrange("(kc p o) -> p kc o", p=128, o=1), allow_slow_non_contiguous=True),
                  "c8", reads=["scT"], writes=["scT"])
        S.dma("sp", lambda e: e.dma_start(out=scT[:, :, 2:3], in_=cctx_in.rearrange("(kc p o) -> p kc o", p=128, o=1), allow_slow_non_contiguous=True),
              "c9", reads=["scT"], writes=["scT"])
        S.op("act", lambda e: e.activation(out=scT[:], in_=scT[:], func=AF.Silu), reads=["scT"], writes=["scT"])
        for s in range(NS):
            S.dma("sp", lambda e, s=s: e.dma_start(out=XR[s, 0:SEQ, :], in_=x_in[s]), f"cx{s}", writes=[f"XR{s}"])
            S.dma("sp", lambda e, s=s: e.dma_start(out=XR[s, SEQ:NTOK, :], in_=ctx_in[s]), f"cc{s}", writes=[f"XR{s}"])
        S.barrier()
        S.emit()

        for L in range(NL):
            is_attn = (L % 2 == 1)
            j = L // 2
            with ExitStack() as lst:
                modT = sbt(lst, "modT", [128, 48, 4], F32)
                gs1T = sbt(lst, "gs1T", [128, KC, 4], F32)
                gs2T = sbt(lst, "gs2T", [128, KC, 4], F32)
                GBt = sbt(lst, "GBt", [128, 3, D], F32)
                G1B = GBt
                G2B = GBt
                rw = sbt(lst, "rw", [128, KC, NE], F32)
                sh1T = modT[:, 0:8, :]
                sh2T = modT[:, 24:32, :]

                def mod_phase(cbs, full):
                  with ExitStack() as ph:
                      scB = sbt(ph, "scB", [128, KC, 3, 128], F32)
                      wmp = [sbt(ph, f"wmp{i}", [128, KC, 512], F32) for i in range(2)]
                      bmT = sbt(ph, "bmT", [128, 48], F32)
                      bmb = sbt(ph, "bmb", [128, 2, D], F32)
                      gnm = sbt(ph, "gnm", [128, 2, KC], F32)
                      tmp4 = sbt(ph, "tmp4", [128, KC, 4], F32)
                      S.dma("sp", lambda e: e.dma_start(out=bmT[:], in_=b_mod[L].rearrange("(c p) -> p c", p=128), allow_slow_non_contiguous=True), "m0", writes=["bmT"])
                      S.dma("sp", lambda e: e.dma_start(out=gnm[:, 0, :], in_=g_nm[L].rearrange("(c p) -> p c", p=128), allow_slow_non_contiguous=True), "m1", writes=["gnm0"])
                      S.dma("sp", lambda e: e.dma_start(out=gnm[:, 1, :], in_=g_nf[L].rearrange("(c p) -> p c", p=128), allow_slow_non_contiguous=True), "m2", writes=["gnm1"])
                      S.dma("sp", lambda e: e.dma_start(out=bmb[:, 0, :], in_=b_mod[L, 2048:3072].rearrange("(o d) -> o d", o=1).to_broadcast([128, D])), "m3", writes=["bmb0"])
                      S.dma("sp", lambda e: e.dma_start(out=bmb[:, 1, :], in_=b_mod[L, 5120:6144].rearrange("(o d) -> o d", o=1).to_broadcast([128, D])), "m4", writes=["bmb1"])
                      S.dma("sp", lambda e: e.dma_start(out=rw[:], in_=r_w[L].rearrange("(kc p) n -> p kc n", p=128)), "m5", writes=["rw"])
                      for r in range(3):
                          S.op("dve", lambda e, r=r: e.tensor_copy(out=scB[:, :, r, :], in_=scT[:, :, r:r + 1].to_broadcast([128, KC, 128])),
                               reads=["scT"], writes=[f"scB{r}"])
                      for cb in cbs:
                          buf = wmp[cb % 2]
                          bk = f"wmp{cb % 2}"
                          S.dma("sp", lambda e, cb=cb, buf=buf: e.dma_start(
                              out=buf[:], in_=w_mod[L][:, cb * 512:(cb + 1) * 512].rearrange("(kc p) n -> p kc n", p=128)),
                              bk, writes=[bk])

                          def f_mod(e, cb=cb, buf=buf):
                              ins = None
                              for jj in range(4):
                                  ci = cb * 4 + jj
                                  for kc in range(KC):
                                      ins = e.matmul(PS[:, 0, ci * 4:ci * 4 + 4], lhsT=buf[:, kc, jj * 128:(jj + 1) * 128],
                                                     rhs=scT[:, kc, :], start=(kc == 0), stop=(kc == KC - 1))
                              return ins
                          if full:
                              S.op("pe", f_mod, reads=[bk, "scT"], writes=[f"modps{cb}"])
                          if (full and cb in (4, 5)) or ((not full) and cb in (10, 11)):
                              gi = 0 if cb < 6 else 1
                              half = cb % 2
                              GB = G1B if gi == 0 else G2B
                              for r in range(3):
                                  bnk = 1 + (r % 2)

                                  def f_g(e, buf=buf, r=r, bnk=bnk):
                                      ins = None
                                      for kc in range(KC):
                                          ins = e.matmul(PS[:, bnk, :], lhsT=scB[:, kc, r, :], rhs=buf[:, kc, :],
                                                         start=(kc == 0), stop=(kc == KC - 1))
                                      return ins
                                  S.op("pe", f_g, reads=[bk, f"scB{r}"], writes=pk(bnk))
                                  S.op("dve", lambda e, GB=GB, r=r, bnk=bnk, gi=gi, half=half: e.tensor_tensor(
                                      out=GB[:, r, half * 512:(half + 1) * 512], in0=PS[:, bnk, :],
                                      in1=bmb[:, gi, half * 512:(half + 1) * 512], op=ALU.add),
                                      reads=pk(bnk) + [f"bmb{gi}"], writes=[f"GB{gi}_{r}_{half}"])
                      if full:
                          S.op("dve", lambda e: e.tensor_tensor(
                              out=modT[:], in0=PS[:, 0, 0:192].rearrange("p (c f) -> p c f", f=4),
                              in1=bmT[:].unsqueeze(2).to_broadcast([128, 48, 4]), op=ALU.add),
                              reads=[f"modps{cb}" for cb in range(12)] + ["bmT"], writes=["modT"])
                          for (gsT, c0, gi) in ((gs1T, 8, 0), (gs2T, 32, 1)):
                              S.op("dve", lambda e, c0=c0: e.tensor_scalar(out=tmp4[:], in0=modT[:, c0:c0 + 8, :], scalar1=1.0, scalar2=None, op0=ALU.add),
                                   reads=["modT"], writes=["tmp4"])
                              S.op("dve", lambda e, gsT=gsT, gi=gi: e.tensor_tensor(
                                  out=gsT[:], in0=tmp4[:], in1=gnm[:, gi, :].unsqueeze(2).to_broadcast([128, KC, 4]), op=ALU.mult),
                                  reads=["tmp4", f"gnm{gi}"], writes=[f"gsT{gi}"])
                      S.barrier()
                      S.emit()

                mod_phase(list(range(12)), True)

                def norm_stats(xt_ap, xkey, ss, rt, rstd, junk, tag):
                    S.op("act", lambda e: e.activation(out=junk, in_=xt_ap, func=AF.Square, accum_out=ss),
                         reads=[xkey], writes=[f"junk{tag}", f"ss{tag}"])
                    S.op("act", lambda e: e.activation(out=rt, in_=ss, func=AF.Sqrt, scale=1.0 / D, bias=epst[:, 0:1]),
                         reads=[f"ss{tag}"], writes=[f"rt{tag}"])
                    S.op("dve", lambda e: e.reciprocal(out=rstd, in_=rt), reads=[f"rt{tag}"], writes=[f"rstd{tag}"])

                def post_tile(s, tile, r, obank, W):
                    rows = slice(tile * 128, (tile + 1) * 128)
                    xb = W["xtB"][0]
                    xbk = "xtB0"
                    W["cntB"] += 1
                    S.dma("sp", lambda e: e.dma_start(out=xb[:], in_=XR[s, rows, :]), xbk, reads=[f"XR{s}_{tile}"], writes=[xbk])
                    osb = W["osb"]
                    S.op("dve", lambda e: e.tensor_tensor(out=osb[:].rearrange("p (a b) -> p a b", a=2), in0=PS[:, obank:obank + 2, :],
                                                         in1=G1B[:, r, :].rearrange("p (a b) -> p a b", a=2), op=ALU.mult),
                         reads=pk(obank, obank + 1), writes=["osb"])
                    S.op("pool", lambda e: e.tensor_tensor(out=xb[:], in0=osb[:], in1=xb[:], op=ALU.add),
                         reads=["osb", xbk], writes=[xbk])
                    S.dma("sp", lambda e: e.dma_start(out=XR[s, rows, :], in_=xb[:]), f"stx{W['cntB'] % 2}", reads=[xbk], writes=[f"XR{s}_{tile}"])
                    norm_stats(xb[:], xbk, W["ss2"][:], W["rt2"][:], W["rstd2"][:], W["junk"][:], "2")
                    xn2 = W["xn2"]
                    S.op("dve", lambda e: e.tensor_scalar(out=xn2[:], in0=xb[:], scalar1=W["rstd2"][:, 0:1], scalar2=None, op0=ALU.mult),
                         reads=[xbk, "rstd2"], writes=["osb"])
                    ft = W["ft"]
                    S.op("pool", lambda e: e.tensor_copy(out=ft[:], in_=xn2[:]), reads=["osb"], writes=["ft"])
                    S.dma("sp", lambda e: e.dma_start(out=FD[s, rows, :], in_=ft[:]), "stf", reads=["ft"], writes=[f"FD{s}_{tile}"])

                    def f_tr(e):
                        ins = None
                        for kc in range(KC):
                            ins = e.transpose(PS[:, 1 + kc // 4, (kc % 4) * 128:(kc % 4 + 1) * 128], xn2[:, kc * 128:(kc + 1) * 128], identf[:])
                        return ins
                    S.op("pe", f_tr, reads=["osb", "identf"], writes=pk(1, 2))
                    fT = W["fT"]

                    def f_ev(e):
                        ins = None
                        for kc in range(KC):
                            ins = e.activation(out=fT[:, kc, :], in_=PS[:, 1 + kc // 4, (kc % 4) * 128:(kc % 4 + 1) * 128], func=AF.Identity,
                                               scale=gs2T[:, kc, r:r + 1], bias=sh2T[:, kc, r:r + 1])
                        return ins
                    S.op("act", f_ev, reads=pk(1, 2) + ["gsT1", "modT"], writes=["fT"])

                    def f_lg(e):
                        ins = None
                        for kc in range(KC):
                            ins = e.matmul(PS[:, 5, 0:NE], lhsT=fT[:, kc, :], rhs=rw[:, kc, :], start=(kc == 0), stop=(kc == KC - 1))
                        return ins
                    S.op("pe", f_lg, reads=["fT", "rw"], writes=pk(5))
                    mx, ex, se = W["mx"], W["ex"], W["se"]
                    S.op("dve", lambda e: e.tensor_reduce(out=mx[:], in_=PS[:, 5, 0:NE], axis=AX.X, op=ALU.max), reads=pk(5), writes=["mx"])
                    S.op("dve", lambda e: e.tensor_scalar(out=mx[:], in0=mx[:], scalar1=-1.0, scalar2=None, op0=ALU.mult), reads=["mx"], writes=["mx"])
                    S.op("act", lambda e: e.activation(out=ex[:], in_=PS[:, 5, 0:NE], func=AF.Exp, bias=mx[:, 0:1], accum_out=se[:]),
                         reads=pk(5) + ["mx"], writes=["ex", "se"])
                    S.op("dve", lambda e: e.reciprocal(out=se[:], in_=se[:]), reads=["se"], writes=["se"])
                    S.op("dve", lambda e: e.tensor_scalar(out=AFF[:, s, tile, :], in0=ex[:], scalar1=se[:, 0:1], scalar2=None, op0=ALU.mult),
                         reads=["ex", "se"], writes=["AFF"])

                def alloc_post(ph):
                    W = {"cntB": 0}
                    W["xtB"] = [sbt(ph, f"xtB{i}", [128, D], F32) for i in range(1)]
                    W["osb"] = sbt(ph, "osb", [128, D], F32)
                    W["xn2"] = W["osb"]
                    W["ft"] = sbt(ph, "ft", [128, D], BF16)
                    W["fT"] = sbt(ph, "fT", [128, KC, 128], F32)
                    W["junk"] = sbt(ph, "junk", [128, D], BF16)
                    for nm in ("ss2", "rt2", "rstd2", "mx", "se"):
                        W[nm] = sbt(ph, nm, [128, 1], F32)
                    W["ex"] = sbt(ph, "ex", [128, NE], F32)
                    return W

                def load_norm_hT(s, tile, r, xtA, cntA, Wn, hT_dst, hkey):
                    rows = slice(tile * 128, (tile + 1) * 128)
                    xa = xtA[cntA % len(xtA)]
                    xak = f"xtA{cntA % len(xtA)}"
                    S.dma("sp", lambda e: e.dma_start(out=xa[:], in_=XR[s, rows, :]), xak, reads=[f"XR{s}_{tile}"], writes=[xak])
                    norm_stats(xa[:], xak, Wn["ss1"][:], Wn["rt1"][:], Wn["rstd1"][:], Wn["junk"][:], "1")
                    xn = Wn["xn"]
                    S.op("dve", lambda e: e.tensor_scalar(out=xn[:], in0=xa[:], scalar1=Wn["rstd1"][:, 0:1], scalar2=None, op0=ALU.mult),
                         reads=[xak, "rstd1"], writes=["xn"])
                    pT = PS[:, 0, :].bitcast(BF16).rearrange("p (k t) -> p k t", k=KC)

                    def f_tr(e):
                        ins = None
                        for kc in range(KC):
                            ins = e.transpose(pT[:, kc, :], xn[:, kc * 128:(kc + 1) * 128], identb[:])
                        return ins
                    S.op("pe", f_tr, reads=["xn", "identb"], writes=pk(0))

                    def f_ev(e):
                        ins = None
                        for kc in range(KC):
                            ins = e.activation(out=hT_dst[:, kc, :], in_=pT[:, kc, :], func=AF.Identity,
                                               scale=gs1T[:, kc, r:r + 1], bias=sh1T[:, kc, r:r + 1])
                        return ins
                    S.op("act", f_ev, reads=pk(0) + ["gsT0", "modT"], writes=[hkey])

                if not is_attn:
                    with ExitStack() as ph:
                        win = sbt(ph, "win", [128, KC, 4096], BF16)
                        wout = sbt(ph, "wout", [128, 16, D], BF16)
                        wsT = sbt(ph, "wsT", [128, 16, 128], BF16)
                        Cc = sbt(ph, "Cc", [128, 16, 128], F32)
                        binu = sbt(ph, "binu", [128, 16], F32)
                        binv = sbt(ph, "binv", [128, 2048], F32)
                        lng = sbt(ph, "lng", [128, 16], F32)
                        onesf = sbt(ph, "onesf", [128, 2], F32)
                        xtA = [sbt(ph, f"xtA{i}", [128, D], F32) for i in range(2)]
                        Wn = {"xn": sbt(ph, "xn", [128, D], BF16)}
                        for nm in ("ss1", "rt1", "rstd1", "sum1", "sum2", "mean", "msq", "var", "rsv"):
                            Wn[nm] = sbt(ph, nm, [128, 1], F32)
                        W = alloc_post(ph)
                        Wn["junk"] = W["junk"]
                        hT = sbt(ph, "hT", [128, KC, 512], BF16)
                        uT = sbt(ph, "uT", [128, 16, 512], BF16)
                        vf = sbt(ph, "vf", [128, 2048], F32)
                        vn = sbt(ph, "vn", [128, 2, 2048], BF16)
                        tmpS = sbt(ph, "tmpS", [128, 512], F32)
                        wsf = vn[:].rearrange("p a b -> p (a b)").bitcast(F32).rearrange("p (g q) -> p g q", g=16)
                        wsTf = vf[:].rearrange("p (g q) -> p g q", g=16)
                        uTf = uT[:].rearrange("p a b -> p (a b)").bitcast(F32)
                        lhs2 = uTf[0:2, 0:2048]
                        rhs2 = uTf[0:2, 2048:4096].rearrange("p (g q) -> p g q", g=16)

                        for kc in range(KC):
                            S.dma("pool", lambda e, kc=kc: e.dma_start(out=win[:, kc, :], in_=a_w_in[j, kc * 128:(kc + 1) * 128, :], max_dma_last_dim=8192),
                                  f"win{kc}", writes=["win"])
                        for cc in range(4):
                            S.dma("pool", lambda e, cc=cc: e.dma_start(
                                out=wout[:, cc * 4:(cc + 1) * 4, :], in_=a_w_out[j, cc * 512:(cc + 1) * 512, :].rearrange("(c p) n -> p c n", p=128)),
                                f"wout{cc}", writes=["wout"])
                        S.dma("sp", lambda e: e.dma_start(out=wsf[:], in_=a_w_s[j].rearrange("g p q -> p g q")), "a0", writes=["wsf"])
                        S.dma("sp", lambda e: e.dma_start(out=binu[:], in_=a_b_in[j, 0:2048].rearrange("(c p) -> p c", p=128), allow_slow_non_contiguous=True), "a1", writes=["binu"])
                        S.dma("sp", lambda e: e.dma_start(out=lng[:], in_=a_ln_g[j].rearrange("(c p) -> p c", p=128), allow_slow_non_contiguous=True), "a2", writes=["lng"])
                        S.dma("sp", lambda e: e.dma_start(out=binv[:], in_=a_b_in[j, 2048:4096].rearrange("(o d) -> o d", o=1).to_broadcast([128, 2048])),
                              "a3", writes=["binv"])
                        S.op("dve", lambda e: e.memset(lhs2[:], 1.0), writes=["lhs2a", "lhs2b"])
                        S.dma("sp", lambda e: e.dma_start(out=lhs2[0:1, :], in_=a_ln_b[j].rearrange("(o d) -> o d", o=1)), "a4", writes=["lhs2a"])
                        S.dma("sp", lambda e: e.dma_start(out=rhs2[1:2, :, :], in_=a_b_s[j].rearrange("(o g) p -> o g p", o=1)), "a5", writes=["rhs2b"])
                        S.op("dve", lambda e: e.memset(onesf[:], 1.0), writes=["onesf"])
                        for g4 in range(4):
                            def f_t(e, g4=g4):
                                ins = None
                                for gg in range(4):
                                    ins = e.transpose(PS[:, 1, gg * 128:(gg + 1) * 128], wsf[:, g4 * 4 + gg, :], identf[:])
                                return ins
                            S.op("pe", f_t, reads=["wsf", "identf"], writes=pk(1))
                            S.op("dve", lambda e, g4=g4: e.tensor_copy(out=wsT[:, g4 * 4:(g4 + 1) * 4, :], in_=PS[:, 1, :].rearrange("p (g q) -> p g q", g=4)),
                                 reads=pk(1), writes=["wsT"])
                        S.op("dve", lambda e: e.tensor_copy(out=wsTf[:], in_=wsT[:]), reads=["wsT"], writes=["wsTf"])
                        for g4 in range(4):
                            S.op("pe", lambda e, g4=g4: e.matmul(PS[0:2, 2, :], lhsT=onesf[:, 0:2], rhs=wsTf[:, g4 * 4:(g4 + 1) * 4, :].rearrange("p g q -> p (g q)"),
                                                              start=True, stop=True), reads=["wsTf", "onesf"], writes=pk(2))
                            S.op("dve", lambda e, g4=g4: e.tensor_copy(out=rhs2[0:1, g4 * 4:(g4 + 1) * 4, :], in_=PS[0:1, 2, :].rearrange("p (g q) -> p g q", g=4)),
                                 reads=pk(2), writes=["rhs2a"])
                        for g in range(16):
                            S.op("pe", lambda e, g=g: e.matmul(PS[:, 3, (g % 4) * 128:(g % 4 + 1) * 128], lhsT=lhs2[:, g * 128:(g + 1) * 128], rhs=rhs2[:, g, :],
                                                            start=True, stop=True),
                                 reads=["lhs2a", "lhs2b", "rhs2a", "rhs2b"], writes=pk(3))
                            if g % 4 == 3:
                                S.op("dve", lambda e, g=g: e.tensor_copy(out=Cc[:, g - 3:g + 1, :], in_=PS[:, 3, :].rearrange("p (g q) -> p g q", g=4)),
                                     reads=pk(3), writes=["Cc"])

                        S.barrier()

                        def do_group(s, gi):
                                tiles = list(range(gi * 4, gi * 4 + 4)) if gi < 8 else [32, 33]
                                nt = len(tiles)
                                ncol = nt * 128
                                r = s if gi < 8 else 2
                                for ti, tile in enumerate(tiles):
                                    load_norm_hT(s, tile, r, xtA, tile, Wn, hT[:, :, ti * 128:(ti + 1) * 128], "hT")
                                for fc in range(16):
                                    bnk = 1 + fc % 2

                                    def f_u(e, fc=fc, bnk=bnk):
                                        ins = None
                                        for kc in range(KC):
                                            ins = e.matmul(PS[:, bnk, 0:ncol], lhsT=win[:, kc, fc * 128:(fc + 1) * 128], rhs=hT[:, kc, 0:ncol],
                                                           start=(kc == 0), stop=(kc == KC - 1))
                                        return ins
                                    S.op("pe", f_u, reads=["win", "hT"], writes=pk(bnk))
                                    S.op("act", lambda e, fc=fc, bnk=bnk: e.activation(out=uT[:, fc, 0:ncol], in_=PS[:, bnk, 0:ncol], func=AF.Gelu, bias=binu[:, fc:fc + 1]),
                                         reads=pk(bnk) + ["binu"], writes=[f"uT{fc}_{t}" for t in range(nt)])
                                def v_part(ti):
                                    vb = ti % 2
                                    vnb = vn[:, vb, :]
                                    vk = f"vn{vb}"
                                    for vc in range(4):
                                        bnk = 3 + vc % 2

                                        def f_v(e, ti=ti, vc=vc, bnk=bnk):
                                            ins = None
                                            for kc in range(KC):
                                                ins = e.matmul(PS[:, bnk, :], lhsT=hT[:, kc, ti * 128:(ti + 1) * 128],
                                                               rhs=win[:, kc, 2048 + vc * 512:2048 + (vc + 1) * 512], start=(kc == 0), stop=(kc == KC - 1))
                                            return ins
                                        S.op("pe", f_v, reads=["win", "hT"], writes=pk(bnk))
                                        S.op("dve", lambda e, vc=vc, bnk=bnk: e.tensor_tensor(out=vf[:, vc * 512:(vc + 1) * 512], in0=PS[:, bnk, :],
                                                                                           in1=binv[:, vc * 512:(vc + 1) * 512], op=ALU.add),
                                             reads=pk(bnk) + ["binv"], writes=[f"vf{vc}"])
                                    vfk = [f"vf{v}" for v in range(4)]
                                    S.op("act", lambda e: e.activation(out=vf[:], in_=vf[:], func=AF.Gelu, accum_out=Wn["sum1"][:]),
                                         reads=vfk, writes=vfk + ["sum1"])
                                    S.op("act", lambda e, vnb=vnb: e.activation(out=vnb, in_=vf[:], func=AF.Square, accum_out=Wn["sum2"][:]),
                                         reads=vfk, writes=[vk, "sum2"])
                                    S.op("dve", lambda e: e.tensor_scalar(out=Wn["mean"][:], in0=Wn["sum1"][:], scalar1=1.0 / 2048, scalar2=None, op0=ALU.mult),
                                         reads=["sum1"], writes=["mean"])
                                    S.op("dve", lambda e: e.tensor_tensor(out=Wn["msq"][:], in0=Wn["mean"][:], in1=Wn["mean"][:], op=ALU.mult),
                                         reads=["mean"], writes=["msq"])
                                    S.op("dve", lambda e: e.scalar_tensor_tensor(out=Wn["var"][:], in0=Wn["sum2"][:], scalar=1.0 / 2048, in1=Wn["msq"][:],
                                                                                 op0=ALU.mult, op1=ALU.subtract), reads=["sum2", "msq"], writes=["var"])
                                    S.op("act", lambda e: e.activation(out=Wn["rsv"][:], in_=Wn["var"][:], func=AF.Sqrt, bias=epst[:, 0:1]),
                                         reads=["var"], writes=["rsv"])
                                    S.op("dve", lambda e: e.reciprocal(out=Wn["rsv"][:], in_=Wn["rsv"][:]), reads=["rsv"], writes=["rsv"])
                                    S.op("dve", lambda e, vnb=vnb: e.tensor_scalar(out=vnb, in0=vf[:], scalar1=Wn["mean"][:, 0:1], scalar2=Wn["rsv"][:, 0:1],
                                                                                op0=ALU.subtract, op1=ALU.mult),
                                         reads=vfk + ["mean", "rsv"], writes=[vk])

                                def s_part(ti):
                                    vb = ti % 2
                                    vnb = vn[:, vb, :]
                                    vk = f"vn{vb}"
                                    for gq in range(4):
                                        g0 = gq * 4
                                        bnk = 5 if gq % 2 == 0 else 1

                                        def f_s(e, g0=g0, bnk=bnk, vnb=vnb):
                                            ins = None
                                            for gg in range(4):
                                                ins = e.matmul(PS[:, bnk, gg * 128:(gg + 1) * 128], lhsT=vnb[:, (g0 + gg) * 128:(g0 + gg + 1) * 128], rhs=wsT[:, g0 + gg, :],
                                                               start=True, stop=True)
                                            return ins
                                        S.op("pe", f_s, reads=[vk, "wsT"], writes=pk(bnk))
                                        t3 = tmpS[:].rearrange("p (g q) -> p g q", g=4)
                                        S.op("dve", lambda e, g0=g0, bnk=bnk, t3=t3: e.tensor_tensor(
                                            out=t3, in0=PS[:, bnk, :].rearrange("p (g q) -> p g q", g=4),
                                            in1=lng[:, g0:g0 + 4].unsqueeze(2).to_broadcast([128, 4, 128]), op=ALU.mult),
                                            reads=pk(bnk) + ["lng"], writes=["tmpS"])
                                        S.op("pool", lambda e, g0=g0, t3=t3: e.tensor_tensor(out=t3, in0=t3, in1=Cc[:, g0:g0 + 4, :], op=ALU.add),
                                             reads=["tmpS", "Cc"], writes=["tmpS"])
                                        S.op("pool", lambda e, g0=g0, t3=t3, ti=ti: e.tensor_tensor(
                                            out=uT[:, g0:g0 + 4, ti * 128:(ti + 1) * 128], in0=uT[:, g0:g0 + 4, ti * 128:(ti + 1) * 128], in1=t3, op=ALU.mult),
                                            reads=[f"uT{g0 + gg}_{ti}" for gg in range(4)] + ["tmpS"], writes=[f"uT{g0 + gg}_{ti}" for gg in range(4)])

                                def o_part(ti):
                                    tile = tiles[ti]
                                    for half in range(2):
                                        def f_o(e, ti=ti, half=half):
                                            ins = None
                                            for cc in range(16):
                                                ins = e.matmul(PS[:, 6 + half, :], lhsT=uT[:, cc, ti * 128:(ti + 1) * 128], rhs=wout[:, cc, half * 512:(half + 1) * 512],
                                                               start=(cc == 0), stop=(cc == 15))
                                            return ins
                                        S.op("pe", f_o, reads=[f"uT{g}_{ti}" for g in range(16)] + ["wout"], writes=pk(6 + half))
                                    post_tile(s, tile, r, 6, W)


                                v_part(0)
                                for ti in range(nt):
                                    if ti + 1 < nt:
                                        v_part(ti + 1)
                                    s_part(ti)
                                    if ti >= 1:
                                        o_part(ti - 1)
                                o_part(nt - 1)

                        for s in range(NS):
                            for gi in range(9):
                                do_group(s, gi)
                        S.barrier()
                        S.emit()
                else:
                    with ExitStack() as ph:
                        qT = sbt(ph, "qT", [128, KC, NTOK], BF16)
                        kT = sbt(ph, "kT", [128, 4, NTOK], BF16)
                        Vt = sbt(ph, "Vt", [128, NT, 256], BF16)
                        SEa = sbt(ph, "SEa", [128, 16], F32)
                        SE = sbt(ph, "SE", [128, 4, 2], F32)
                        S.dma("sp", lambda e: e.dma_start(out=SEa[:], in_=b_sink[j].rearrange("(o d) -> o d", o=1).to_broadcast([128, 16])), "b0", writes=["SEa"])
                        S.op("act", lambda e: e.activation(out=SEa[:], in_=SEa[:], func=AF.Exp), reads=["SEa"], writes=["SEa"])
                        sev = SEa[:].rearrange("p (g i o) -> p g i o", g=4, i=2)
                        S.op("dve", lambda e: e.tensor_copy(out=SE[0:64, :, :], in_=sev[0:64, :, :, 0]), reads=["SEa"], writes=["SE0"])
                        S.op("dve", lambda e: e.tensor_copy(out=SE[64:128, :, :], in_=sev[64:128, :, :, 1]), reads=["SEa"], writes=["SE1"])
                        for s in range(NS):
                            with ExitStack() as p1:
                                wqkv = sbt(p1, "wqkv", [128, KC, 1536], BF16)
                                for kc in range(KC):
                                    S.dma("pool", lambda e, kc=kc, wqkv=wqkv: e.dma_start(out=wqkv[:, kc, :], in_=b_w_qkv[j, kc * 128:(kc + 1) * 128, :]), f"wq{kc % 2}", writes=["wqkv"])
                                xtA = [sbt(p1, f"xtA{i}", [128, D], F32) for i in range(2)]
                                Wn = {"xn": sbt(p1, "xn", [128, D], BF16), "junk": sbt(p1, "junk1", [128, D], BF16)}
                                for nm in ("ss1", "rt1", "rstd1"):
                                    Wn[nm] = sbt(p1, nm, [128, 1], F32)
                                hT1s = [sbt(p1, f"hT1{i}", [128, KC, 128], BF16) for i in range(2)]
                                cst = [sbt(p1, f"cst{i}", [128, 2, 32], F32) for i in range(2)]
                                Ar = sbt(p1, "Ar", [128, 20, 2, 32], F32)
                                Br = sbt(p1, "Br", [128, 20, 2, 32], F32)
                                qkrs = [sbt(p1, f"qkr{i}", [128, 20, 2, 32], BF16) for i in range(2)]
                                kds = [sbt(p1, f"kd{i}", [128, 4, 2, 64], BF16) for i in range(2)]

                                def stage_a(s, tile):
                                    r = s if tile < NLT else 2
                                    load_norm_hT(s, tile, r, xtA, tile, Wn, hT1s[tile % 2][:, :, :], f"hT1{tile % 2}")

                                def do_tile1(s, tile):
                                    r = s if tile < NLT else 2
                                    cols = slice(tile * 128, (tile + 1) * 128)
                                    hT1 = hT1s[tile % 2]
                                    hk = f"hT1{tile % 2}"
                                    qkr = qkrs[tile % 2]
                                    kd = kds[tile % 2]
                                    q0, q1, k0, k1 = (f"qkr0_{tile % 2}", f"qkr1_{tile % 2}", f"kd0_{tile % 2}", f"kd1_{tile % 2}")

                                    def f_qkv(e):
                                        ins = None
                                        for blk in range(3):
                                            for kc in range(KC):
                                                ins = e.matmul(PS[:, 1 + blk, :], lhsT=hT1[:, kc, :], rhs=wqkv[:, kc, blk * 512:(blk + 1) * 512],
                                                               start=(kc == 0), stop=(kc == KC - 1))
                                        return ins
                                    S.op("pe", f_qkv, reads=[hk, "wqkv"], writes=pk(1, 2, 3))
                                    X = PS[:, 1:4, :].rearrange("p b (h two d) -> p (b h) two d", two=2, d=32)
                                    S.op("act", lambda e, tile=tile: e.copy(out=Vt[:, tile, :], in_=PS[:, 3, 256:512]), reads=pk(3), writes=[f"Vt{tile}"])
                                    if tile < NLT:
                                        cs = cst[tile % 2]
                                        ck = f"cst{tile % 2}"
                                        S.dma("sp", lambda e, cs=cs, tile=tile: e.dma_start(out=cs[:, 0, :], in_=k_cos[tile * 128:(tile + 1) * 128, :]), ck + "c", writes=[ck + "c"])
                                        S.dma("sp", lambda e, cs=cs, tile=tile: e.dma_start(out=cs[:, 1, :], in_=k_sin[tile * 128:(tile + 1) * 128, :]), ck + "s", writes=[ck + "s"])
                                        S.op("dve", lambda e, cs=cs: e.tensor_tensor(out=Ar[:], in0=X[:, 0:20, :, :],
                                                                                  in1=cs[:, 0:1, :].unsqueeze(1).to_broadcast([128, 20, 2, 32]), op=ALU.mult),
                                             reads=pk(1, 2, 3) + [ck + "c"], writes=["Ar"])
                                        S.op("dve", lambda e, cs=cs: e.tensor_tensor(out=Br[:, :, 0, :], in0=X[:, 0:20, 1, :],
                                                                                  in1=cs[:, 1:2, :].to_broadcast([128, 20, 32]), op=ALU.mult),
                                             reads=pk(1, 2, 3) + [ck + "s"], writes=["Br0"])
                                        S.op("dve", lambda e, cs=cs: e.tensor_tensor(out=Br[:, :, 1, :], in0=X[:, 0:20, 0, :],
                                                                                  in1=cs[:, 1:2, :].to_broadcast([128, 20, 32]), op=ALU.mult),
                                             reads=pk(1, 2, 3) + [ck + "s"], writes=["Br1"])
                                        S.op("pool", lambda e: e.tensor_tensor(out=qkr[:, :, 0, :], in0=Ar[:, :, 0, :], in1=Br[:, :, 0, :], op=ALU.subtract),
                                             reads=["Ar", "Br0"], writes=[q0])
                                        S.op("pool", lambda e: e.tensor_tensor(out=qkr[:, :, 1, :], in0=Ar[:, :, 1, :], in1=Br[:, :, 1, :], op=ALU.add),
                                             reads=["Ar", "Br1"], writes=[q1])
                                    else:
                                        S.op("dve", lambda e: e.tensor_copy(out=qkr[:], in_=X[:, 0:20, :, :]), reads=pk(1, 2, 3), writes=[q0, q1])
                                    kv = qkr[:, 16:20, :, :].rearrange("p h two d -> p h (two d)")
                                    S.op("pool", lambda e: e.tensor_copy(out=kd[:, :, 0, :], in_=kv), reads=[q0, q1], writes=[k0])
                                    S.op("pool", lambda e: e.tensor_copy(out=kd[:, :, 1, :], in_=kv), reads=[q0, q1], writes=[k1])

                                def do_tile1b(s, tile):
                                    cols = slice(tile * 128, (tile + 1) * 128)
                                    qkr = qkrs[tile % 2]
                                    kd = kds[tile % 2]
                                    q0, q1, k0, k1 = (f"qkr0_{tile % 2}", f"qkr1_{tile % 2}", f"kd0_{tile % 2}", f"kd1_{tile % 2}")
                                    pTq = PS[:, 4, :].bitcast(BF16).rearrange("p (k t) -> p k t", k=KC)
                                    pTk = PS[:, 5, :].bitcast(BF16).rearrange("p (k t) -> p k t", k=KC)
                                    qf = qkr[:].rearrange("p h two d -> p (h two d)")

                                    def f_tq(e):
                                        ins = None
                                        for c in range(KC):
                                            ins = e.transpose(pTq[:, c, :], qf[:, c * 128:(c + 1) * 128], identb[:])
                                        for g in range(4):
                                            ins = e.transpose(pTk[:, g, :], kd[:, g, :, :].rearrange("p a d -> p (a d)"), identb[:])
                                        return ins
                                    S.op("pe", f_tq, reads=[q0, q1, k0, k1, "identb"], writes=pk(4, 5))
                                    S.op("act", lambda e, cols=cols: e.copy(out=qT[:, :, cols], in_=pTq[:, :, :]), reads=pk(4), writes=[f"qT{tile}"])
                                    S.op("dve", lambda e, cols=cols: e.tensor_copy(out=kT[:, :, cols], in_=pTk[:, 0:4, :]), reads=pk(5), writes=[f"kT{tile}"])

                                stage_a(s, 0)
                                for tile in range(NT):
                                    if tile + 1 < NT:
                                        stage_a(s, tile + 1)
                                    do_tile1(s, tile)
                                    if tile >= 1:
                                        do_tile1b(s, tile - 1)
                                do_tile1b(s, NT - 1)
                                S.barrier()
                                S.emit()
                            with ExitStack() as p2:
                                W = alloc_post(p2)
                                PTt = [sbt(p2, f"PTt{i}", [128, 5, 2, 256], BF16) for i in range(2)]
                                wo = sbt(p2, "wo", [128, KC, D], BF16)
                                for kc in range(KC):
                                    S.dma("pool", lambda e, kc=kc, wo=wo: e.dma_start(out=wo[:, kc, :], in_=b_w_o[j, kc * 128:(kc + 1) * 128, :]), f"wo{kc % 2}", writes=["wo"])
                                oT = sbt(p2, "oT", [128, KC, 128], BF16)
                                dsb = sbt(p2, "dsb", [128, 256], F32)
                                cg = {"n": 0}

                                def do_qblock(s, qb):
                                    r = s if qb < NLT else 2
                                    qcols = slice(qb * 128, (qb + 1) * 128)
                                    if qb < NLT:
                                        keys = []
                                        if qb - 1 >= 0:
                                            keys.append((qb - 1, mlb))
                                        keys.append((qb, None))
                                        if qb + 1 < NLT:
                                            keys.append((qb + 1, mrb))
                                        keys += [(32, None), (33, None)]
                                    else:
                                        keys = [(32, None), (33, None)]
                                    nk = len(keys)
                                    def qk_part(g):
                                        PT = PTt[g % 2]
                                        ptk = f"PT{g % 2}_"
                                        for idx, (kj, mk) in enumerate(keys):
                                            kcols = slice(kj * 128, (kj + 1) * 128)
                                            ba = 2 + (idx % 2) * 2
                                            bb = ba + 1

                                            def f_qk(e, g=g, kcols=kcols, ba=ba, bb=bb):
                                                e.matmul(PS[:, ba, 0:256].rearrange("p (a b) -> p a b", a=2), lhsT=kT[0:64, g, kcols], rhs=qT[0:64, 2 * g:2 * g + 2, qcols],
                                                         start=True, stop=True)
                                                return e.matmul(PS[:, bb, 0:256].rearrange("p (a b) -> p a b", a=2), lhsT=kT[64:128, g, kcols], rhs=qT[64:128, 2 * g:2 * g + 2, qcols],
                                                                start=True, stop=True)
                                            S.op("pe", f_qk, reads=[f"kT{kj}", f"qT{qb}"], writes=pk(ba, bb))

                                            def f_ex(e, idx=idx, ba=ba, bb=bb, PT=PT):
                                                e.activation(out=PT[:, idx, 0, :], in_=PS[:, ba, 0:256], func=AF.Exp, scale=0.125)
                                                return e.activation(out=PT[:, idx, 1, :], in_=PS[:, bb, 0:256], func=AF.Exp, scale=0.125)
                                            S.op("act", f_ex, reads=pk(ba, bb), writes=[ptk + str(idx)])
                                            if mk is not None:
                                                S.op("dve", lambda e, idx=idx, mk=mk, PT=PT: e.tensor_tensor(
                                                    out=PT[:, idx, :, :].rearrange("p a (h q) -> p (a h) q", h=2),
                                                    in0=PT[:, idx, :, :].rearrange("p a (h q) -> p (a h) q", h=2),
                                                    in1=mk[:].unsqueeze(1).to_broadcast([128, 4, 128]), op=ALU.mult),
                                                    reads=[ptk + str(idx)], writes=[ptk + str(idx)])


                                    def pv_part(g):
                                        PT = PTt[g % 2]
                                        ptk = f"PT{g % 2}_"
                                        def f_pv(e, g=g, PT=PT):
                                            ins = None
                                            for idx, (kj, mk) in enumerate(keys):
                                                st_, sp_ = (idx == 0), (idx == nk - 1)
                                                e.matmul(PS[0:64, 0, 0:256], lhsT=Vt[:, kj, g * 64:(g + 1) * 64], rhs=PT[:, idx, 0, :], start=st_, stop=sp_)
                                                e.matmul(PS[64:128, 0, 0:256], lhsT=Vt[:, kj, g * 64:(g + 1) * 64], rhs=PT[:, idx, 1, :], start=st_, stop=sp_)
                                                e.matmul(PS[0:64, 1, 0:256], lhsT=onesb[:, 0:64], rhs=PT[:, idx, 0, :], start=st_, stop=sp_)
                                                ins = e.matmul(PS[64:128, 1, 0:256], lhsT=onesb[:, 0:64], rhs=PT[:, idx, 1, :], start=st_, stop=sp_)
                                            return ins
                                        S.op("pe", f_pv, reads=[ptk + str(i) for i in range(nk)] + [f"Vt{kj}" for kj, _ in keys] + ["onesb"], writes=pk(0, 1))
                                        S.op("dve", lambda e, g=g: e.tensor_tensor(out=dsb[:].rearrange("p (i q) -> p i q", i=2), in0=PS[:, 1, 0:256].rearrange("p (i q) -> p i q", i=2),
                                                                                in1=SE[:, g, :].unsqueeze(2).to_broadcast([128, 2, 128]), op=ALU.add),
                                             reads=pk(1) + ["SE0", "SE1"], writes=["dsb"])
                                        S.op("dve", lambda e: e.reciprocal(out=dsb[:], in_=dsb[:]), reads=["dsb"], writes=["dsb"])
                                        S.op("dve", lambda e, g=g: e.tensor_tensor(out=oT[:, 2 * g:2 * g + 2, :], in0=PS[:, 0, 0:256].rearrange("p (i q) -> p i q", i=2),
                                                                                in1=dsb[:].rearrange("p (i q) -> p i q", i=2), op=ALU.mult),
                                             reads=pk(0) + ["dsb"], writes=[f"oT{g}"])

                                    qk_part(0)
                                    for g in range(4):
                                        if g + 1 < 4:
                                            qk_part(g + 1)
                                        pv_part(g)
                                    for half in range(2):
                                        def f_wo(e, half=half):
                                            ins = None
                                            for c in range(KC):
                                                ins = e.matmul(PS[:, 6 + half, :], lhsT=oT[:, c, :], rhs=wo[:, c, half * 512:(half + 1) * 512],
                                                               start=(c == 0), stop=(c == KC - 1))
                                            return ins
                                        S.op("pe", f_wo, reads=[f"oT{g}" for g in range(4)] + ["wo"], writes=pk(6 + half))
                                    post_tile(s, qb, r, 6, W)

                                for qb in range(NT):
                                    do_qblock(s, qb)
                                S.barrier()
                                S.emit()

                if skip_moe:
                    continue
                IDX = sbt(lst, "IDX", [128, NS, NE, 4], I32)
                GAT = sbt(lst, "GAT", [128, NS, NE, 4], F32)
                IDXC = sbt(lst, "IDXC", [32, NS, NE], I32)
                GATC = sbt(lst, "GATC", [32, NS, NE], F32)
                with ExitStack() as ph:
                    LO = sbt(ph, "LO", [128, NS, 2, NE], F32)
                    MID = sbt(ph, "MID", [128, NS, 2, NE], F32)
                    GE = sbt(ph, "GE", [128, NS, 2, NE], F32)
                    CMP = sbt(ph, "CMP", [128, NS, NT, NE], BF16)
                    CNTP = sbt(ph, "CNTP", [128, NS, 2, NE], F32)
                    S.op("dve", lambda e: e.memset(LO[:], 0.0), writes=["LO"])
                    for it in range(NBIS):
                        wv = 2.0 ** -(it + 1)
                        S.op("dve", lambda e, wv=wv: e.tensor_scalar(out=MID[:], in0=LO[:], scalar1=wv, scalar2=None, op0=ALU.add), reads=["LO"], writes=["MID"])
                        S.op("dve", lambda e: e.tensor_tensor(out=CMP[:, :, 0:NLT, :], in0=AFF[:, :, 0:NLT, :],
                                                             in1=MID[:, :, 0:1, :].to_broadcast([128, NS, NLT, NE]), op=ALU.is_ge),
                             reads=["AFF", "MID"], writes=["CMPl"])
                        S.op("dve", lambda e: e.tensor_tensor(out=CMP[:, :, NLT:NT, :], in0=AFF[:, :, NLT:NT, :],
                                                             in1=MID[:, :, 1:2, :].to_broadcast([128, NS, 2, NE]), op=ALU.is_ge),
                             reads=["AFF", "MID"], writes=["CMPc"])
                        S.op("dve", lambda e: e.tensor_reduce(out=CNTP[:, :, 0, :], in_=CMP[:, :, 0:NLT, :].rearrange("p s t e -> p s e t"), axis=AX.X, op=ALU.add),
                             reads=["CMPl"], writes=["CNTPl"])
                        S.op("dve", lambda e: e.tensor_reduce(out=CNTP[:, :, 1, :], in_=CMP[:, :, NLT:NT, :].rearrange("p s t e -> p s e t"), axis=AX.X, op=ALU.add),
                             reads=["CMPc"], writes=["CNTPc"])
                        S.op("pe", lambda e: e.matmul(PS[:, 0, 0:64], lhsT=onesF[:], rhs=CNTP[:].rearrange("p s a e -> p (s a e)"), start=True, stop=True),
                             reads=["CNTPl", "CNTPc", "onesF"], writes=pk(0))
                        pc = PS[:, 0, 0:64].rearrange("p (s a e) -> p s a e", s=NS, a=2)
                        S.op("dve", lambda e, pc=pc: e.tensor_scalar(out=GE[:, :, 0, :], in0=pc[:, :, 0, :], scalar1=float(CAP_L), scalar2=None, op0=ALU.is_ge),
                             reads=pk(0), writes=["GEl"])
                        S.op("dve", lambda e, pc=pc: e.tensor_scalar(out=GE[:, :, 1, :], in0=pc[:, :, 1, :], scalar1=float(CAP_C), scalar2=None, op0=ALU.is_ge),
                             reads=pk(0), writes=["GEc"])
                        S.op("dve", lambda e, wv=wv: e.scalar_tensor_tensor(out=LO[:], in0=GE[:], scalar=wv, in1=LO[:], op0=ALU.mult, op1=ALU.add),
                             reads=["GEl", "GEc", "LO"], writes=["LO"])
                    SEL = CMP
                    OFFS = sbt(ph, "OFFS", [128, NS, NT, NE], F32)
                    POS = sbt(ph, "POS", [128, NS, NT, NE], F32)
                    LT = sbt(ph, "LT", [128, NS, NT, NE], F32)
                    LOI = sbt(ph, "LOI", [128, NS, NT, NE], F32)
                    HI = sbt(ph, "HI", [128, NS, NT, NE], F32)
                    VALS = sbt(ph, "VALS", [128, NS, NT, NE, 4], BF16)
                    ALf = sbt(ph, "ALf", [128, NS, NT, NE], F32)
                    Hh = [sbt(ph, f"Hh{i}", [128, NLT, 128], BF16) for i in range(2)]
                    L4 = [sbt(ph, f"L4{i}", [128, NLT, 4], BF16) for i in range(2)]
                    Rr = [sbt(ph, f"Rr{i}", [128, NLT, 4, 4], BF16) for i in range(2)]
                    Hc = sbt(ph, "Hc", [128, 2, NE, 32], BF16)
                    IDXF = sbt(ph, "IDXF", [128, NS, NE, 4], F32)
                    pisb = sbt(ph, "pisb", [128, 2, 16], F32)
                    picsb = sbt(ph, "picsb", [32, 64], F32)
                    IDXCF = sbt(ph, "IDXCF", [32, NS, NE], F32)
                    S.op("dve", lambda e: e.tensor_tensor(out=SEL[:, :, 0:NLT, :], in0=AFF[:, :, 0:NLT, :],
                                                         in1=LO[:, :, 0:1, :].to_broadcast([128, NS, NLT, NE]), op=ALU.is_ge), reads=["AFF", "LO"], writes=["CMPl"])
                    S.op("dve", lambda e: e.tensor_tensor(out=SEL[:, :, NLT:NT, :], in0=AFF[:, :, NLT:NT, :],
                                                         in1=LO[:, :, 1:2, :].to_broadcast([128, NS, 2, NE]), op=ALU.is_ge), reads=["AFF", "LO"], writes=["CMPc"])
                    self_flat = SEL[:].rearrange("p s t e -> p (s t e)")
                    NTOT = NS * NT * NE

                    def f_cs(e):
                        ins = None
                        for (c0, c1, b) in ((0, 512, 0), (512, 1024, 1), (1024, NTOT, 2)):
                            e.matmul(PS[:, b, 0:c1 - c0], lhsT=upperb[:], rhs=self_flat[:, c0:c1], start=True, stop=True)
                            ins = e.matmul(PS[:, 3 + b, 0:c1 - c0], lhsT=onesb[:], rhs=self_flat[:, c0:c1], start=True, stop=True)
                        return ins
                    S.op("pe", f_cs, reads=["CMPl", "CMPc", "upperb", "onesb"], writes=pk(0, 1, 2, 3, 4, 5))
                    Wp = PS[:, 0:3, :].rearrange("p b n -> p (b n)")[:, 0:NTOT].rearrange("p (s t e) -> p s t e", s=NS, t=NT)
                    Tp = PS[:, 3:6, :].rearrange("p b n -> p (b n)")[:, 0:NTOT].rearrange("p (s t e) -> p s t e", s=NS, t=NT)
                    S.op("dve", lambda e: e.memset(OFFS[:], 0.0), writes=["OFFS"])
                    for t in range(1, NLT):
                        S.op("dve", lambda e, t=t: e.tensor_tensor(out=OFFS[:, :, t, :], in0=OFFS[:, :, t - 1, :], in1=Tp[:, :, t - 1, :], op=ALU.add),
                             reads=pk(3, 4, 5) + ["OFFS"], writes=["OFFS"])
                    S.op("dve", lambda e: e.tensor_copy(out=OFFS[:, :, NLT + 1, :], in_=Tp[:, :, NLT, :]), reads=pk(3, 4, 5) + ["OFFS"], writes=["OFFS"])
                    S.op("dve", lambda e: e.tensor_tensor(out=POS[:], in0=Wp, in1=OFFS[:], op=ALU.add), reads=pk(0, 1, 2) + ["OFFS"], writes=["POS"])
                    S.op("dve", lambda e: e.tensor_scalar(out=LT[:, :, 0:NLT, :], in0=POS[:, :, 0:NLT, :], scalar1=float(CAP_L), scalar2=None, op0=ALU.is_lt),
                         reads=["POS"], writes=["LTl"])
                    S.op("dve", lambda e: e.tensor_scalar(out=LT[:, :, NLT:NT, :], in0=POS[:, :, NLT:NT, :], scalar1=float(CAP_C), scalar2=None, op0=ALU.is_lt),
                         reads=["POS"], writes=["LTc"])
                    S.op("dve", lambda e: e.tensor_tensor(out=LT[:], in0=LT[:], in1=SEL[:], op=ALU.mult), reads=["LTl", "LTc", "CMPl", "CMPc"], writes=["LT"])
                    S.op("dve", lambda e: e.scalar_tensor_tensor(out=POS[:], in0=POS[:], scalar=1.0, in1=LT[:], op0=ALU.add, op1=ALU.mult),
                         reads=["POS", "LT"], writes=["POS"])
                    S.op("dve", lambda e: e.tensor_scalar(out=POS[:], in0=POS[:], scalar1=-1.0, scalar2=None, op0=ALU.add), reads=["POS"], writes=["POS"])
                    S.op("dve", lambda e: e.tensor_scalar(out=LOI[:], in0=POS[:], scalar1=128.0, scalar2=None, op0=ALU.is_ge), reads=["POS"], writes=["LOI"])
                    for thr in (256.0, 384.0):
                        S.op("dve", lambda e, thr=thr: e.scalar_tensor_tensor(out=LOI[:], in0=POS[:], scalar=thr, in1=LOI[:], op0=ALU.is_ge, op1=ALU.add),
                             reads=["POS", "LOI"], writes=["LOI"])
                    S.op("dve", lambda e: e.scalar_tensor_tensor(out=HI[:], in0=LOI[:], scalar=-128.0, in1=POS[:], op0=ALU.mult, op1=ALU.add),
                         reads=["POS", "LOI"], writes=["HI"])
                    S.op("dve", lambda e: e.tensor_copy(out=VALS[:, :, :, :, 0:2], in_=ptf[:].unsqueeze(1).unsqueeze(3).to_broadcast([128, NS, NT, NE, 2])),
                         reads=["ptf"], writes=["VALS01"])
                    S.op("dve", lambda e: e.tensor_copy(out=VALS[:, :, :, :, 2], in_=AFF[:]), reads=["AFF"], writes=["VALS2"])
                    S.op("dve", lambda e: e.tensor_tensor(out=ALf[:], in0=AFF[:], in1=VALS[:, :, :, :, 2], op=ALU.subtract), reads=["AFF", "VALS2"], writes=["ALf"])
                    S.op("dve", lambda e: e.tensor_copy(out=VALS[:, :, :, :, 3], in_=ALf[:]), reads=["ALf"], writes=["VALS3"])
                    vkeys = ["VALS01", "VALS2", "VALS3"]
                    cnt = 0
                    for s in range(NS):
                        for ee in range(NE):
                            b = cnt % 2
                            cnt += 1
                            S.op("dve", lambda e, s=s, ee=ee, b=b: e.tensor_tensor(
                                out=Hh[b][:], in0=iotaf[:].unsqueeze(1).to_broadcast([128, NLT, 128]),
                                in1=HI[:, s, 0:NLT, ee:ee + 1].to_broadcast([128, NLT, 128]), op=ALU.is_equal),
                                reads=["HI", "iotaf"], writes=[f"Hh{b}"])
                            S.op("dve", lambda e, s=s, ee=ee, b=b: e.tensor_tensor(
                                out=L4[b][:], in0=iotaf[:, 0:4].unsqueeze(1).to_broadcast([128, NLT, 4]),
                                in1=LOI[:, s, 0:NLT, ee:ee + 1].to_broadcast([128, NLT, 4]), op=ALU.is_equal),
                                reads=["LOI", "iotaf"], writes=[f"L4{b}"])
                            S.op("pool", lambda e, s=s, ee=ee, b=b: e.tensor_tensor(
                                out=Rr[b][:], in0=L4[b][:].unsqueeze(3).to_broadcast([128, NLT, 4, 4]),
                                in1=VALS[:, s, 0:NLT, ee, :].unsqueeze(2).to_broadcast([128, NLT, 4, 4]), op=ALU.mult),
                                reads=[f"L4{b}"] + vkeys, writes=[f"Rr{b}"])
                            bnk = 6 + b

                            def f_oh(e, b=b, bnk=bnk):
                                ins = None
                                for t in range(NLT):
                                    ins = e.matmul(PS[:, bnk, 0:16], lhsT=Hh[b][:, t, :], rhs=Rr[b][:, t, :, :].rearrange("p a v -> p (a v)"),
                                                   start=(t == 0), stop=(t == NLT - 1))
                                return ins
                            S.op("pe", f_oh, reads=[f"Hh{b}", f"Rr{b}"], writes=pk(bnk))
                            S.op("act", lambda e, b=b, bnk=bnk: e.copy(out=pisb[:, b, :], in_=PS[:, bnk, 0:16]), reads=pk(bnk), writes=[f"pisb{b}"])
                            pi = pisb[:, b, :].rearrange("p (a v) -> p a v", a=4)
                            S.op("dve", lambda e, s=s, ee=ee, pi=pi: e.scalar_tensor_tensor(out=IDXF[:, s, ee, :], in0=pi[:, :, 1], scalar=128.0, in1=pi[:, :, 0],
                                                                                         op0=ALU.mult, op1=ALU.add), reads=[f"pisb{b}"], writes=["IDXF"])
                            S.op("dve", lambda e, s=s, ee=ee, pi=pi: e.tensor_tensor(out=GAT[:, s, ee, :], in0=pi[:, :, 2], in1=pi[:, :, 3], op=ALU.add),
                                 reads=[f"pisb{b}"], writes=["GAT"])
                        S.op("dve", lambda e, s=s: e.tensor_tensor(out=Hc[:], in0=iotaf[:, 0:32].unsqueeze(1).unsqueeze(1).to_broadcast([128, 2, NE, 32]),
                                                                in1=POS[:, s, NLT:NT, :].unsqueeze(3).to_broadcast([128, 2, NE, 32]), op=ALU.is_equal),
                             reads=["POS", "iotaf"], writes=["Hc"])

                        def f_c(e, s=s):
                            ins = None
                            for ee in range(NE):
                                for t in range(2):
                                    ins = e.matmul(PS[0:32, 5, ee * 4:(ee + 1) * 4], lhsT=Hc[:, t, ee, :], rhs=VALS[:, s, NLT + t, ee, :],
                                                   start=(t == 0), stop=(t == 1))
                            return ins
                        S.op("pe", f_c, reads=["Hc"] + vkeys, writes=pk(5))
                        S.op("act", lambda e: e.copy(out=picsb[:], in_=PS[0:32, 5, 0:64]), reads=pk(5), writes=["picsb"])
                        pic = picsb[:].rearrange("p (a v) -> p a v", a=NE)
                        S.op("dve", lambda e, s=s, pic=pic: e.scalar_tensor_tensor(out=IDXCF[:, s, :], in0=pic[:, :, 1], scalar=128.0, in1=pic[:, :, 0],
                                                                                op0=ALU.mult, op1=ALU.add), reads=["picsb"], writes=["IDXCF"])
                        S.op("dve", lambda e, s=s, pic=pic: e.tensor_tensor(out=GATC[:, s, :], in0=pic[:, :, 2], in1=pic[:, :, 3], op=ALU.add),
                             reads=["picsb"], writes=["GATC"])
                    S.op("dve", lambda e: e.tensor_scalar(out=IDXF[:, 1, :, :], in0=IDXF[:, 1, :, :], scalar1=float(NTOK), scalar2=None, op0=ALU.add),
                         reads=["IDXF"], writes=["IDXF"])
                    S.op("dve", lambda e: e.tensor_scalar(out=IDXCF[:, 1, :], in0=IDXCF[:, 1, :], scalar1=float(NTOK), scalar2=None, op0=ALU.add),
                         reads=["IDXCF"], writes=["IDXCF"])
                    S.op("dve", lambda e: e.tensor_copy(out=IDX[:], in_=IDXF[:]), reads=["IDXF"], writes=["IDX"])
                    S.op("dve", lambda e: e.tensor_copy(out=IDXC[:], in_=IDXCF[:]), reads=["IDXCF"], writes=["IDXC"])
                    if dbg_route:
                        for s in range(NS):
                            S.dma("sp", lambda e, s=s: e.dma_start(out=dbg_d[1, s * 128:(s + 1) * 128, 0:544], in_=AFF[:, s].rearrange("p t e -> p (t e)")), "dr0", reads=["AFF"], writes=["dbgr0"])
                            S.dma("sp", lambda e, s=s: e.dma_start(out=dbg_d[1, 768 + s * 128:768 + (s + 1) * 128, 0:544], in_=POS[:, s].rearrange("p t e -> p (t e)")), "dr5", reads=["POS"], writes=["dbgr5"])
                            S.dma("sp", lambda e, s=s: e.dma_start(out=dbg_d[1, 1024 + s * 128:1024 + (s + 1) * 128, 0:544], in_=HI[:, s].rearrange("p t e -> p (t e)")), "dr6", reads=["HI"], writes=["dbgr6"])
                        S.dma("sp", lambda e: e.dma_start(out=dbg_d[1, 256:384, 0:128], in_=IDXF[:].rearrange("p s e k -> p (s e k)")), "dr1", reads=["IDXF"], writes=["dbgr1"])
                        S.dma("sp", lambda e: e.dma_start(out=dbg_d[1, 384:512, 0:128], in_=GAT[:].rearrange("p s e k -> p (s e k)")), "dr2", reads=["GAT"], writes=["dbgr2"])
                        S.dma("sp", lambda e: e.dma_start(out=dbg_d[1, 512:640, 0:64], in_=LO[:].rearrange("p s a e -> p (s a e)")), "dr3", reads=["LO"], writes=["dbgr3"])
                        S.dma("sp", lambda e: e.dma_start(out=dbg_d[1, 640:672, 0:32], in_=IDXCF[:].rearrange("p s e -> p (s e)")), "dr4", reads=["IDXCF"], writes=["dbgr4"])
                    S.barrier()
                    S.emit()

                if dbg_route:
                    continue
                mod_phase([10, 11], False)

                with ExitStack() as ph:
                    WP = [sbt(ph, f"WP{i}", [128, 4096], BF16) for i in range(8)]
                    XG = [[sbt(ph, f"XG{p}{s}", [128, 5, D], BF16) for s in range(NS)] for p in range(2)]
                    XgT = [sbt(ph, f"XgT{s}", [128, KC, 544], BF16) for s in range(NS)]
                    gT = [sbt(ph, f"gT{s}", [128, 16, 544], BF16) for s in range(NS)]
                    sa = [sbt(ph, f"sa{i}", [128, 544], BF16) for i in range(2)]
                    ysb = [sbt(ph, f"ysb{i}", [128, D], F32) for i in range(2)]
                    state = {"pc": 0, "sa": 0, "y": 0}
                    piece_slot = {}

                    def issue_piece(ee, i):
                        pidx = ee * 12 + i
                        slot = pidx % 8
                        piece_slot[(ee, i)] = slot
                        if i < 8:
                            wsrc = e_w1 if i % 2 == 0 else e_w3
                            fg = i // 2
                            src = wsrc[L, ee][:, fg * 512:(fg + 1) * 512].rearrange("(kc p) n -> p kc n", p=128)
                            dst = WP[slot][:].rearrange("p (kc n) -> p kc n", kc=KC)
                        else:
                            fq = i - 8
                            src = e_w2[L, ee][fq * 512:(fq + 1) * 512, :].rearrange("(fc p) n -> p fc n", p=128)
                            dst = WP[slot][:].rearrange("p (fc n) -> p fc n", fc=4)
                        S.dma("pool", lambda e, src=src, dst=dst: e.dma_start(out=dst, in_=src), f"wp{slot}", writes=[f"WP{slot}"])

                    def issue_gathers(ee):
                        par = ee % 2
                        for s in range(NS):
                            for k in range(4):
                                S.dma("pool", lambda e, s=s, k=k, par=par, ee=ee: e.indirect_dma_start(
                                    out=XG[par][s][:, k, :], out_offset=None, in_=FDflat,
                                    in_offset=bass.IndirectOffsetOnAxis(ap=IDX[:, s, ee, k:k + 1], axis=0)),
                                    f"xg{par}{s}{k}", reads=["IDX", f"FD{s}"], writes=[f"XG{par}{s}{k}"])
                            S.dma("pool", lambda e, s=s, par=par, ee=ee: e.indirect_dma_start(
                                out=XG[par][s][0:32, 4, :], out_offset=None, in_=FDflat,
                                in_offset=bass.IndirectOffsetOnAxis(ap=IDXC[0:32, s, ee:ee + 1], axis=0)),
                                f"xg{par}{s}4", reads=["IDXC", f"FD{s}"], writes=[f"XG{par}{s}4"])

                    issue_gathers(0)
                    for i in range(8):
                        issue_piece(0, i)
                    def do_expert(ee):
                        par = ee % 2
                        if ee + 1 < NE:
                            issue_gathers(ee + 1)
                        pT = PS[:, 0, :].bitcast(BF16).rearrange("p (k t) -> p k t", k=KC)
                        for s in range(NS):
                            for k in range(5):
                                rows = 128 if k < 4 else 32
                                r = s if k < 4 else 2

                                def f_tr(e, s=s, k=k, rows=rows, par=par):
                                    ins = None
                                    for kc in range(KC):
                                        ins = e.transpose(pT[:, kc, 0:rows], XG[par][s][0:rows, k, kc * 128:(kc + 1) * 128], identb[0:rows, 0:rows])
                                    return ins
                                S.op("pe", f_tr, reads=[f"XG{par}{s}{k}", "identb"], writes=pk(0))

                                def f_ev(e, s=s, k=k, rows=rows, r=r):
                                    ins = None
                                    for kc in range(KC):
                                        ins = e.activation(out=XgT[s][:, kc, k * 128:k * 128 + rows], in_=pT[:, kc, 0:rows], func=AF.Identity,
                                                           scale=gs2T[:, kc, r:r + 1], bias=sh2T[:, kc, r:r + 1])
                                    return ins
                                S.op("act", f_ev, reads=pk(0), writes=[f"XgT{s}_{k}"])
                        xgk = [[f"XgT{s}_{k}" for k in range(5)] for s in range(NS)]
                        if dbg_exp and ee == 0:
                            S.dma("pool", lambda e: e.dma_start(out=dbg_d[1, 0:128, :], in_=XG[0][0][:, 0, :]), "de0", reads=["XG000"], writes=["dbge0"])
                            S.dma("pool", lambda e: e.dma_start(out=dbg_d[1, 128:256, 0:544], in_=XgT[0][:, 0, :]), "de1", reads=xgk[0], writes=["dbge1"])
                            S.dma("pool", lambda e: e.dma_start(out=dbg_d[1, 512:640, :], in_=FD[0, 0:128, :]), "de4", writes=["dbge4"])
                            S.dma("sp", lambda e: e.dma_start(out=dbg_d[1, 640:768, 0:128], in_=IDX[:].rearrange("p s e k -> p (s e k)").bitcast(F32)), "de5", reads=["IDX"], writes=["dbge5"])
                        for fg in range(4):
                            s1 = piece_slot[(ee, 2 * fg)]
                            s3 = piece_slot[(ee, 2 * fg + 1)]
                            W1p = WP[s1][:].rearrange("p (kc n) -> p kc n", kc=KC)
                            W3p = WP[s3][:].rearrange("p (kc n) -> p kc n", kc=KC)
                            for f4 in range(4):
                                fc = fg * 4 + f4
                                for s in range(NS):
                                    sab = sa[state["sa"] % 2]
                                    sak = f"sa{state['sa'] % 2}"
                                    state["sa"] += 1

                                    def f_a(e, Wp_=W1p, f4=f4, s=s, b0=1, b1=2):
                                        ins = None
                                        for kc in range(KC):
                                            e.matmul(PS[:, b0, :], lhsT=Wp_[:, kc, f4 * 128:(f4 + 1) * 128], rhs=XgT[s][:, kc, 0:512], start=(kc == 0), stop=(kc == KC - 1))
                                            ins = e.matmul(PS[:, b1, 0:32], lhsT=Wp_[:, kc, f4 * 128:(f4 + 1) * 128], rhs=XgT[s][:, kc, 512:544], start=(kc == 0), stop=(kc == KC - 1))
                                        return ins
                                    S.op("pe", f_a, reads=[f"WP{s1}"] + xgk[s], writes=pk(1, 2))

                                    def f_si(e, sab=sab):
                                        e.activation(out=sab[:, 0:512], in_=PS[:, 1, :], func=AF.Silu)
                                        return e.activation(out=sab[:, 512:544], in_=PS[:, 2, 0:32], func=AF.Silu)
                                    S.op("act", f_si, reads=pk(1, 2), writes=[sak])
                                    S.op("pe", lambda e, Wp_=W3p, f4=f4, s=s: f_a(e, Wp_, f4, s, 3, 4), reads=[f"WP{s3}"] + xgk[s], writes=pk(3, 4))

                                    def f_mu(e, sab=sab, s=s, fc=fc):
                                        e.tensor_tensor(out=gT[s][:, fc, 0:512], in0=PS[:, 3, :], in1=sab[:, 0:512], op=ALU.mult)
                                        return e.tensor_tensor(out=gT[s][:, fc, 512:544], in0=PS[:, 4, 0:32], in1=sab[:, 512:544], op=ALU.mult)
                                    S.op("dve", f_mu, reads=pk(3, 4) + [sak], writes=[f"gT{s}_{fc}"])
                            if fg < 2:
                                issue_piece(ee, 8 + 2 * fg)
                                issue_piece(ee, 9 + 2 * fg)
                            elif ee + 1 < NE:
                                issue_piece(ee + 1, 2 * (fg - 2))
                                issue_piece(ee + 1, 2 * (fg - 2) + 1)
                        if dbg_exp and ee == 0:
                            S.dma("pool", lambda e: e.dma_start(out=dbg_d[1, 256:384, 0:544], in_=gT[0][:, 0, :]), "de2", reads=["gT0_0"], writes=["dbge2"])
                        w2s = [piece_slot[(ee, 8 + q)] for q in range(4)]
                        W2p = [WP[sl][:].rearrange("p (fc n) -> p fc n", fc=4) for sl in w2s]
                        for s in range(NS):
                            for k in range(5):
                                rows = 128 if k < 4 else 32
                                r = s if k < 4 else 2
                                for half in range(2):
                                    def f_y(e, s=s, k=k, rows=rows, half=half):
                                        ins = None
                                        for fc in range(16):
                                            ins = e.matmul(PS[0:rows, 5 + half, :], lhsT=gT[s][:, fc, k * 128:k * 128 + rows],
                                                           rhs=W2p[fc // 4][:, fc % 4, half * 512:(half + 1) * 512], start=(fc == 0), stop=(fc == 15))
                                        return ins
                                    S.op("pe", f_y, reads=[f"gT{s}_{fc}" for fc in range(16)] + [f"WP{sl}" for sl in w2s], writes=pk(5 + half))
                                yb = ysb[state["y"] % 2]
                                yk = f"ysb{state['y'] % 2}"
                                state["y"] += 1
                                gate_ap = GAT[:, s, ee, k:k + 1] if k < 4 else GATC[0:32, s, ee:ee + 1]
                                S.op("dve", lambda e, yb=yb, rows=rows, gate_ap=gate_ap, r=r: e.scalar_tensor_tensor(
                                    out=yb[0:rows, :].rearrange("p (a b) -> p a b", a=2), in0=PS[0:rows, 5:7, :], scalar=gate_ap,
                                    in1=G2B[0:rows, r, :].rearrange("p (a b) -> p a b", a=2), op0=ALU.mult, op1=ALU.mult),
                                    reads=pk(5, 6) + ["GAT", "GATC"], writes=[yk])
                                idx_ap = IDX[:, s, ee, k:k + 1] if k < 4 else IDXC[0:32, s, ee:ee + 1]
                                if dbg_exp and ee == 0 and s == 0 and k == 0:
                                    S.dma("sp", lambda e, yb=yb: e.dma_start(out=dbg_d[1, 384:512, :], in_=yb[:]), "de3", reads=[yk], writes=["dbge3"])
                                S.dma("pool", lambda e, s=s, yb=yb, rows=rows, idx_ap=idx_ap: e.indirect_dma_start(
                                    out=XRflat, out_offset=bass.IndirectOffsetOnAxis(ap=idx_ap, axis=0), in_=yb[0:rows, :], in_offset=None, compute_op=ALU.add),
                                    f"scat{s}", reads=[yk, "IDX", "IDXC"], writes=[f"XRs{s}_{k}"], group=(k > 0))
                        if ee + 1 < NE:
                            for i in range(4, 8):
                                issue_piece(ee + 1, i)

                    for ee in range(NE):
                        do_expert(ee)
                    S.barrier()
                    S.emit()

        if dbg:
            for s in range(1 if (dbg_route or dbg_exp) else NS):
                S.dma("sp", lambda e, s=s: e.dma_start(out=dbg_d[s], in_=XR[s]), f"dbg{s}", writes=[f"dbg{s}"])
        with ExitStack() as ph:
            xt = [sbt(ph, f"fx{i}", [128, D], F32) for i in range(2)]
            yo = [sbt(ph, f"fy{i}", [128, D], F32) for i in range(2)]
            junk = sbt(ph, "fjunk", [128, D], BF16)
            gfin = sbt(ph, "gfin", [128, D], F32)
            S.dma("sp", lambda e: e.dma_start(out=gfin[:], in_=g_final.rearrange("(o d) -> o d", o=1).to_broadcast([128, D])),
                  "c7", writes=["gfin"])
            ss = sbt(ph, "fss", [128, 1], F32)
            rt = sbt(ph, "frt", [128, 1], F32)
            rstd = sbt(ph, "frstd", [128, 1], F32)
            cnt = 0
            for s in range(NS):
                for tile in range(NLT):
                    b = cnt % 2
                    cnt += 1
                    rows = slice(tile * 128, (tile + 1) * 128)
                    S.dma("sp", lambda e, s=s, rows=rows, b=b: e.dma_start(out=xt[b][:], in_=XR[s, rows, :]), f"fx{b}", writes=[f"fx{b}"])
                    S.op("act", lambda e, b=b: e.activation(out=junk[:], in_=xt[b][:], func=AF.Square, accum_out=ss[:]), reads=[f"fx{b}"], writes=["fjunk", "fss"])
                    S.op("act", lambda e: e.activation(out=rt[:], in_=ss[:], func=AF.Sqrt, scale=1.0 / D, bias=epst[:, 0:1]), reads=["fss"], writes=["frt"])
                    S.op("dve", lambda e: e.reciprocal(out=rstd[:], in_=rt[:]), reads=["frt"], writes=["frstd"])
                    S.op("dve", lambda e, b=b: e.scalar_tensor_tensor(out=yo[b][:], in0=xt[b][:], scalar=rstd[:, 0:1], in1=gfin[:], op0=ALU.mult, op1=ALU.mult),
                         reads=[f"fx{b}", "frstd", "gfin"], writes=[f"fy{b}"])
                    S.dma("sp", lambda e, s=s, rows=rows, b=b: e.dma_start(out=out_d[s, rows, :], in_=yo[b][:]), f"fy{b}", reads=[f"fy{b}"], writes=["out"])
            S.barrier()
            S.emit()
    return nc


def host_consts():
    t = np.arange(SEQ)
    row = (t // 64).astype(np.float32)
    col = (t % 64).astype(np.float32)
    inv = (10000.0 ** (-np.arange(16, dtype=np.float32) / 16)).astype(np.float32)
    ang = np.concatenate([row[:, None] * inv, col[:, None] * inv], axis=-1).astype(np.float32)
    p = np.arange(128)
    pt = np.zeros((128, NT, 2), np.float32)
    pt[:, :, 0] = p[:, None]
    pt[:, :, 1] = np.arange(NT)[None, :]
    return dict(
        k_ident=np.eye(128, dtype=np.float32),
        k_cos=np.cos(ang).astype(np.float32),
        k_sin=np.sin(ang).astype(np.float32),
        k_upper=(p[:, None] < p[None, :]).astype(np.float32),
        k_iota=np.broadcast_to(np.arange(128, dtype=np.float32)[None, :], (128, 128)).copy(),
        k_pt=pt,
        k_ml=(p[:, None] >= p[None, :]).astype(np.float32),
        k_mr=(p[:, None] <= p[None, :]).astype(np.float32),
    )


_NC_CACHE = {}


def kernel(**inputs):
    n = 8
    if "nc" not in _NC_CACHE:
        _NC_CACHE["nc"] = build()
    nc = _NC_CACHE["nc"]
    consts = host_consts()
    shared = {k: np.ascontiguousarray(np.asarray(v, dtype=np.float32)) for k, v in inputs.items() if k not in ("x", "c", "ctx")}
    x = np.asarray(inputs["x"], dtype=np.float32)
    c = np.asarray(inputs["c"], dtype=np.float32)
    ctx = np.asarray(inputs["ctx"], dtype=np.float32)
    in_maps = []
    for i in range(n):
        m = dict(shared)
        m.update(consts)
        m["x"] = np.ascontiguousarray(x[2 * i:2 * i + 2])
        m["c"] = np.ascontiguousarray(c[2 * i:2 * i + 2])
        m["ctx"] = np.ascontiguousarray(ctx[2 * i:2 * i + 2])
        in_maps.append(m)
    res = run_bass_kernel_spmd(nc, in_maps, core_ids=list(range(n)))
    return np.concatenate([r["out"] for r in res.results], axis=0).astype(np.float32)
```

```python
import numpy as np
import ml_dtypes
from contextlib import ExitStack
import concourse.bass as bass
import concourse.mybir as mybir
from concourse.bass_utils import run_bass_kernel_spmd

F32 = mybir.dt.float32
BF16 = mybir.dt.bfloat16
I32 = mybir.dt.int32
AF = mybir.ActivationFunctionType
ALU = mybir.AluOpType
AX = mybir.AxisListType

EPOCH = 30000
COMPUTE = ("pe", "act", "dve", "pool")
ALL_ENG = ("pe", "act", "dve", "pool", "sp")


class Tok:
    __slots__ = ("sem", "val", "eng", "is_dma")

    def __init__(self, sem, val, eng, is_dma):
        self.sem, self.val, self.eng, self.is_dma = sem, val, eng, is_dma


class Sched:
    def __init__(self, nc, stack):
        self.nc = nc
        self.stack = stack
        self.ops = {e: [] for e in ALL_ENG}
        self.prog_sem = {}
        self.prog_cnt = {}
        self.dma_sem = {}
        self.known = {e: {} for e in ALL_ENG}
        self.lastw = {}
        self.readers = {}
        self.nsem = 0
        self.all_toks = {}
        self.nops = 0
        for e in COMPUTE:
            self._new_epoch(e)

    def _mk_sem(self, name):
        self.nsem += 1
        return self.stack.enter_context(self.nc.semaphore(name))

    def _new_epoch(self, e):
        self.prog_sem[e] = self._mk_sem(f"p_{e}_{self.nsem}")
        self.prog_cnt[e] = 0

    def _collect(self, eng, reads, writes, extra):
        deps = []
        for k in reads:
            t = self.lastw.get(k)
            if t is not None:
                deps.append(t)
        for k in writes:
            t = self.lastw.get(k)
            if t is not None:
                deps.append(t)
            deps.extend(self.readers.get(k, ()))
        deps.extend(extra)
        waits = {}
        kn = self.known[eng]
        for t in deps:
            if t is None:
                continue
            if (not t.is_dma) and t.eng == eng and eng == "pe":
                continue
            name = id(t.sem)
            if kn.get(name, 0) >= t.val:
                continue
            if name not in waits or waits[name].val < t.val:
                waits[name] = t
        for name, t in waits.items():
            kn[name] = t.val
        return list(waits.values())

    def _commit(self, tok, reads, writes):
        for k in reads:
            self.readers.setdefault(k, []).append(tok)
        for k in writes:
            self.lastw[k] = tok
            self.readers[k] = []
        self.all_toks[id(tok.sem)] = tok

    def op(self, eng, fn, reads=(), writes=(), extra=()):
        waits = self._collect(eng, reads, writes, extra)
        if self.prog_cnt[eng] >= EPOCH:
            self._new_epoch(eng)
        self.prog_cnt[eng] += 1
        tok = Tok(self.prog_sem[eng], self.prog_cnt[eng], eng, False)
        self.ops[eng].append((waits, fn, tok.sem, 1))
        self._commit(tok, reads, writes)
        self.nops += 1
        return tok

    def dma(self, eng, fn, key, reads=(), writes=(), extra=(), group=False):
        ent = self.dma_sem.get(key)
        if ent is None:
            ent = [self._mk_sem(f"d_{self.nsem}"), 0, None]
            self.dma_sem[key] = ent
        ex = list(extra)
        if ent[2] is not None and not group:
            ex.append(ent[2])
        waits = self._collect(eng, reads, writes, ex)
        ent[1] += 16
        tok = Tok(ent[0], ent[1], eng, True)
        ent[2] = tok
        self.ops[eng].append((waits, fn, tok.sem, 16))
        self._commit(tok, reads, writes)
        self.nops += 1
        return tok

    def barrier(self, engs=ALL_ENG):
        toks = list(self.all_toks.values())
        for e in engs:
            waits = []
            kn = self.known[e]
            for t in toks:
                name = id(t.sem)
                if kn.get(name, 0) >= t.val:
                    continue
                kn[name] = t.val
                waits.append(t)
            if waits:
                self.ops[e].append((waits, None, None, 0))
        self.lastw = {}
        self.readers = {}

    def emit(self):
        nc = self.nc
        ops = self.ops

        def run(engine, lst):
            for waits, fn, sem, inc in lst:
                for t in waits:
                    engine.wait_ge(t.sem, t.val)
                if fn is None:
                    continue
                inst = fn(engine)
                inst.then_inc(sem, inc)

        with nc.Block() as block:
            @block.tensor
            def _(e):
                run(e, ops["pe"])

            @block.scalar
            def _(e):
                run(e, ops["act"])

            @block.vector
            def _(e):
                run(e, ops["dve"])

            @block.gpsimd
            def _(e):
                run(e, ops["pool"])

            @block.sync
            def _(e):
                run(e, ops["sp"])
        self.ops = {e: [] for e in ALL_ENG}


D = 1024
KC = 8
SEQ = 4096
CTX = 256
NTOK = SEQ + CTX
NT = NTOK // 128
NLT = SEQ // 128
NS = 2
NE = 16
CAP_L = 512
CAP_C = 32
EPS = 1e-6
NBIS = 20


def build(NL=4, do_final=True, dbg=False, skip_moe=False, dbg_route=False, dbg_exp=False):
    nc = bass.Bass("TRN2", target_bir_lowering=False)

    def din(name, shape, dt=F32):
        return nc.dram_tensor(name, list(shape), dt, kind="ExternalInput").ap()

    x_in = din("x", [NS, SEQ, D])
    c_in = din("c", [NS, D])
    ctx_in = din("ctx", [NS, CTX, D])
    cctx_in = din("c_ctx", [D])
    w_mod = din("w_mod", [4, D, 6 * D])
    b_mod = din("b_mod", [4, 6 * D])
    g_nm = din("g_norm_mix", [4, D])
    g_nf = din("g_norm_ffn", [4, D])
    a_w_in = din("a_w_in", [2, D, 4096])
    a_b_in = din("a_b_in", [2, 4096])
    a_ln_g = din("a_ln_g", [2, 2048])
    a_ln_b = din("a_ln_b", [2, 2048])
    a_w_s = din("a_w_s", [2, 16, 128, 128])
    a_b_s = din("a_b_s", [2, 16, 128])
    a_w_out = din("a_w_out", [2, 2048, D])
    b_w_qkv = din("b_w_qkv", [2, D, 1536])
    b_sink = din("b_sink", [2, 16])
    b_w_o = din("b_w_o", [2, D, D])
    r_w = din("r_w", [4, D, NE])
    e_w1 = din("e_w1", [4, NE, D, 2048])
    e_w3 = din("e_w3", [4, NE, D, 2048])
    e_w2 = din("e_w2", [4, NE, 2048, D])
    g_final = din("g_final", [D])
    k_ident = din("k_ident", [128, 128])
    k_cos = din("k_cos", [SEQ, 32])
    k_sin = din("k_sin", [SEQ, 32])
    k_upper = din("k_upper", [128, 128])
    k_iota = din("k_iota", [128, 128])
    k_pt = din("k_pt", [128, NT, 2])
    k_ml = din("k_ml", [128, 128])
    k_mr = din("k_mr", [128, 128])

    out_d = nc.dram_tensor("out", [NS, SEQ, D], F32, kind="ExternalOutput").ap()
    XR = nc.dram_tensor("xr_scratch", [NS, NTOK, D], F32, kind="Internal").ap()
    FD = nc.dram_tensor("f_scratch", [NS, NTOK, D], BF16, kind="Internal").ap()
    XRflat = XR.rearrange("s t d -> (s t) d")
    FDflat = FD.rearrange("s t d -> (s t) d")
    dbg_d = nc.dram_tensor("dbg", [NS, NTOK, D], F32, kind="ExternalOutput").ap() if dbg else None

    with ExitStack() as gst:
        S = Sched(nc, gst)

        uniq = {"n": 0}

        def sbt(st, name, shape, dt):
            uniq["n"] += 1
            return st.enter_context(nc.sbuf_tensor(f"{name}_u{uniq['n']}", list(shape), dt))

        PS = gst.enter_context(nc.psum_tensor("PS", [128, 8, 512], F32))

        def pk(*bs):
            return [f"ps{b}" for b in bs]

        identf = sbt(gst, "identf", [128, 128], F32)
        identb = sbt(gst, "identb", [128, 128], BF16)
        onesb = sbt(gst, "onesb", [128, 128], BF16)
        onesF = sbt(gst, "onesF", [128, 128], F32)
        upperb = sbt(gst, "upperb", [128, 128], BF16)
        iotaf = sbt(gst, "iotaf", [128, 128], F32)
        ptf = sbt(gst, "ptf", [128, NT, 2], F32)
        mlb = sbt(gst, "mlb", [128, 128], BF16)
        mrb = sbt(gst, "mrb", [128, 128], BF16)
        epst = sbt(gst, "epst", [128, 1], F32)
        scT = sbt(gst, "scT", [128, KC, 4], F32)
        AFF = sbt(gst, "AFF", [128, NS, NT, NE], F32)

        S.dma("sp", lambda e: e.dma_start(out=identf[:], in_=k_ident), "c0", writes=["identf"])
        S.dma("sp", lambda e: e.dma_start(out=iotaf[:], in_=k_iota), "c1", writes=["iotaf"])
        S.dma("sp", lambda e: e.dma_start(out=ptf[:], in_=k_pt), "c2", writes=["ptf"])
        S.dma("pool", lambda e: e.dma_start(out=identb[:], in_=k_ident), "c3", writes=["identb"])
        S.dma("pool", lambda e: e.dma_start(out=upperb[:], in_=k_upper), "c4", writes=["upperb"])
        S.dma("pool", lambda e: e.dma_start(out=mlb[:], in_=k_ml), "c5", writes=["mlb"])
        S.dma("pool", lambda e: e.dma_start(out=mrb[:], in_=k_mr), "c6", writes=["mrb"])
        S.op("dve", lambda e: e.memset(onesb[:], 1.0), writes=["onesb"])
        S.op("dve", lambda e: e.memset(onesF[:], 1.0), writes=["onesF"])
        S.op("dve", lambda e: e.memset(epst[:], EPS), writes=["epst"])
        S.op("dve", lambda e: e.memset(AFF[:], 0.0), writes=["AFF"])
        S.op("dve", lambda e: e.memset(scT[:], 0.0), writes=["scT"])
        for s in range(NS):
            S.dma("sp", lambda e, s=s: e.dma_start(out=scT[:, :, s:s + 1], in_=c_in[s].rearrange("(kc p o) -> p kc o", p=128, o=1), allow_slow_non_contiguous=True),
                  "c8", reads=["scT"], writes=["scT"])
        S.dma("sp", lambda e: e.dma_start(out=scT[:, :, 2:3], in_=cctx_in.rearrange("(kc p o) -> p kc o", p=128, o=1), allow_slow_non_contiguous=True),
              "c9", reads=["scT"], writes=["scT"])
        S.op("act", lambda e: e.activation(out=scT[:], in_=scT[:], func=AF.Silu), reads=["scT"], writes=["scT"])
        for s in range(NS):
            S.dma("sp", lambda e, s=s: e.dma_start(out=XR[s, 0:SEQ, :], in_=x_in[s]), f"cx{s}", writes=[f"XR{s}"])
            S.dma("sp", lambda e, s=s: e.dma_start(out=XR[s, SEQ:NTOK, :], in_=ctx_in[s]), f"cc{s}", writes=[f"XR{s}"])
        S.barrier()
        S.emit()

        for L in range(NL):
            is_attn = (L % 2 == 1)
            j = L // 2
            with ExitStack() as lst:
                modT = sbt(lst, "modT", [128, 48, 4], F32)
                gs1T = sbt(lst, "gs1T", [128, KC, 4], F32)
                gs2T = sbt(lst, "gs2T", [128, KC, 4], F32)
                GBt = sbt(lst, "GBt", [128, 3, D], F32)
                G1B = GBt
                G2B = GBt
                rw = sbt(lst, "rw", [128, KC, NE], F32)
                sh1T = modT[:, 0:8, :]
                sh2T = modT[:, 24:32, :]

                def mod_phase(cbs, full):
                  with ExitStack() as ph:
                      scB = sbt(ph, "scB", [128, KC, 3, 128], F32)
                      wmp = [sbt(ph, f"wmp{i}", [128, KC, 512], F32) for i in range(2)]
                      bmT = sbt(ph, "bmT", [128, 48], F32)
                      bmb = sbt(ph, "bmb", [128, 2, D], F32)
                      gnm = sbt(ph, "gnm", [128, 2, KC], F32)
                      tmp4 = sbt(ph, "tmp4", [128, KC, 4], F32)
                      S.dma("sp", lambda e: e.dma_start(out=bmT[:], in_=b_mod[L].rearrange("(c p) -> p c", p=128), allow_slow_non_contiguous=True), "m0", writes=["bmT"])
                      S.dma("sp", lambda e: e.dma_start(out=gnm[:, 0, :], in_=g_nm[L].rearrange("(c p) -> p c", p=128), allow_slow_non_contiguous=True), "m1", writes=["gnm0"])
                      S.dma("sp", lambda e: e.dma_start(out=gnm[:, 1, :], in_=g_nf[L].rearrange("(c p) -> p c", p=128), allow_slow_non_contiguous=True), "m2", writes=["gnm1"])
                      S.dma("sp", lambda e: e.dma_start(out=bmb[:, 0, :], in_=b_mod[L, 2048:3072].rearrange("(o d) -> o d", o=1).to_broadcast([128, D])), "m3", writes=["bmb0"])
                      S.dma("sp", lambda e: e.dma_start(out=bmb[:, 1, :], in_=b_mod[L, 5120:6144].rearrange("(o d) -> o d", o=1).to_broadcast([128, D])), "m4", writes=["bmb1"])
                      S.dma("sp", lambda e: e.dma_start(out=rw[:], in_=r_w[L].rearrange("(kc p) n -> p kc n", p=128)), "m5", writes=["rw"])
                      for r in range(3):
                          S.op("dve", lambda e, r=r: e.tensor_copy(out=scB[:, :, r, :], in_=scT[:, :, r:r + 1].to_broadcast([128, KC, 128])),
                               reads=["scT"], writes=[f"scB{r}"])
                      for cb in cbs:
                          buf = wmp[cb % 2]
                          bk = f"wmp{cb % 2}"
                          S.dma("sp", lambda e, cb=cb, buf=buf: e.dma_start(
                              out=buf[:], in_=w_mod[L][:, cb * 512:(cb + 1) * 512].rearrange("(kc p) n -> p kc n", p=128)),
                              bk, writes=[bk])

                          def f_mod(e, cb=cb, buf=buf):
                              ins = None
                              for jj in range(4):
                                  ci = cb * 4 + jj
                                  for kc in range(KC):
                                      ins = e.matmul(PS[:, 0, ci * 4:ci * 4 + 4], lhsT=buf[:, kc, jj * 128:(jj + 1) * 128],
                                                     rhs=scT[:, kc, :], start=(kc == 0), stop=(kc == KC - 1))
                              return ins
                          if full:
                              S.op("pe", f_mod, reads=[bk, "scT"], writes=[f"modps{cb}"])
                          if (full and cb in (4, 5)) or ((not full) and cb in (10, 11)):
                              gi = 0 if cb < 6 else 1
                              half = cb % 2
                              GB = G1B if gi == 0 else G2B
                              for r in range(3):
                                  bnk = 1 + (r % 2)

                                  def f_g(e, buf=buf, r=r, bnk=bnk):
                                      ins = None
                                      for kc in range(KC):
                                          ins = e.matmul(PS[:, bnk, :], lhsT=scB[:, kc, r, :], rhs=buf[:, kc, :],
                                                         start=(kc == 0), stop=(kc == KC - 1))
                                      return ins
                                  S.op("pe", f_g, reads=[bk, f"scB{r}"], writes=pk(bnk))
                                  S.op("dve", lambda e, GB=GB, r=r, bnk=bnk, gi=gi, half=half: e.tensor_tensor(
                                      out=GB[:, r, half * 512:(half + 1) * 512], in0=PS[:, bnk, :],
                                      in1=bmb[:, gi, half * 512:(half + 1) * 512], op=ALU.add),
                                      reads=pk(bnk) + [f"bmb{gi}"], writes=[f"GB{gi}_{r}_{half}"])
                      if full:
                          S.op("dve", lambda e: e.tensor_tensor(
                              out=modT[:], in0=PS[:, 0, 0:192].rearrange("p (c f) -> p c f", f=4),
                              in1=bmT[:].unsqueeze(2).to_broadcast([128, 48, 4]), op=ALU.add),
                              reads=[f"modps{cb}" for cb in range(12)] + ["bmT"], writes=["modT"])
                          for (gsT, c0, gi) in ((gs1T, 8, 0), (gs2T, 32, 1)):
                              S.op("dve", lambda e, c0=c0: e.tensor_scalar(out=tmp4[:], in0=modT[:, c0:c0 + 8, :], scalar1=1.0, scalar2=None, op0=ALU.add),
                                   reads=["modT"], writes=["tmp4"])
                              S.op("dve", lambda e, gsT=gsT, gi=gi: e.tensor_tensor(
                                  out=gsT[:], in0=tmp4[:], in1=gnm[:, gi, :].unsqueeze(2).to_broadcast([128, KC, 4]), op=ALU.mult),
                                  reads=["tmp4", f"gnm{gi}"], writes=[f"gsT{gi}"])
                      S.barrier()
                      S.emit()

                mod_phase(list(range(12)), True)

                def norm_stats(xt_ap, xkey, ss, rt, rstd, junk, tag):
                    S.op("act", lambda e: e.activation(out=junk, in_=xt_ap, func=AF.Square, accum_out=ss),
                         reads=[xkey], writes=[f"junk{tag}", f"ss{tag}"])
                    S.op("act", lambda e: e.activation(out=rt, in_=ss, func=AF.Sqrt, scale=1.0 / D, bias=epst[:, 0:1]),
                         reads=[f"ss{tag}"], writes=[f"rt{tag}"])
                    S.op("dve", lambda e: e.reciprocal(out=rstd, in_=rt), reads=[f"rt{tag}"], writes=[f"rstd{tag}"])

                def post_tile(s, tile, r, obank, W):
                    rows = slice(tile * 128, (tile + 1) * 128)
                    xb = W["xtB"][0]
                    xbk = "xtB0"
                    b = W["cntB"] % 2
                    W["cntB"] += 1
                    osb = W["osbs"][b]
                    ok = f"osb{b}"
                    S.dma("sp", lambda e: e.dma_start(out=xb[:], in_=XR[s, rows, :]), xbk, reads=[f"XR{s}_{tile}"], writes=[xbk])
                    S.op("dve", lambda e: e.tensor_tensor(out=osb[:].rearrange("p (a b) -> p a b", a=2), in0=PS[:, obank:obank + 2, :],
                                                         in1=G1B[:, r, :].rearrange("p (a b) -> p a b", a=2), op=ALU.mult),
                         reads=pk(obank, obank + 1), writes=[ok])
                    S.op("pool", lambda e: e.tensor_tensor(out=xb[:], in0=osb[:], in1=xb[:], op=ALU.add),
                         reads=[ok, xbk], writes=[xbk])
                    S.dma("sp", lambda e: e.dma_start(out=XR[s, rows, :], in_=xb[:]), f"stx{b}", reads=[xbk], writes=[f"XR{s}_{tile}"])
                    norm_stats(xb[:], xbk, W["ss2"][:], W["rt2"][:], W["rstd2"][:], W["junk"][:], "2")
                    S.op("dve", lambda e: e.tensor_scalar(out=osb[:], in0=xb[:], scalar1=W["rstd2"][:, 0:1], scalar2=None, op0=ALU.mult),
                         reads=[xbk, "rstd2"], writes=[ok])
                    ft = W["ft"]
                    S.op("pool", lambda e: e.tensor_copy(out=ft[:], in_=osb[:]), reads=[ok], writes=["ft"])
                    S.dma("sp", lambda e: e.dma_start(out=FD[s, rows, :], in_=ft[:]), "stf", reads=["ft"], writes=[f"FD{s}_{tile}"])
                    prev = W.get("pending")
                    W["pending"] = (s, tile, r, osb, ok)
                    if prev is not None:
                        post_b(W, *prev)

                def flush_post(W):
                    prev = W.get("pending")
                    W["pending"] = None
                    if prev is not None:
                        post_b(W, *prev)

                def post_b(W, s, tile, r, xn2, ok):
                    def f_tr(e):
                        ins = None
                        for kc in range(KC):
                            ins = e.transpose(PS[:, 1 + kc // 4, (kc % 4) * 128:(kc % 4 + 1) * 128], xn2[:, kc * 128:(kc + 1) * 128], identf[:])
                        return ins
                    S.op("pe", f_tr, reads=[ok, "identf"], writes=pk(1, 2))
                    fT = W["fT"]

                    def f_ev(e):
                        ins = None
                        for kc in range(KC):
                            ins = e.activation(out=fT[:, kc, :], in_=PS[:, 1 + kc // 4, (kc % 4) * 128:(kc % 4 + 1) * 128], func=AF.Identity,
                                               scale=gs2T[:, kc, r:r + 1], bias=sh2T[:, kc, r:r + 1])
                        return ins
                    S.op("act", f_ev, reads=pk(1, 2) + ["gsT1", "modT"], writes=["fT"])

                    def f_lg(e):
                        ins = None
                        for kc in range(KC):
                            ins = e.matmul(PS[:, 5, 0:NE], lhsT=fT[:, kc, :], rhs=rw[:, kc, :], start=(kc == 0), stop=(kc == KC - 1))
                        return ins
                    S.op("pe", f_lg, reads=["fT", "rw"], writes=pk(5))
                    mx, ex, se = W["mx"], W["ex"], W["se"]
                    S.op("dve", lambda e: e.tensor_reduce(out=mx[:], in_=PS[:, 5, 0:NE], axis=AX.X, op=ALU.max), reads=pk(5), writes=["mx"])
                    S.op("dve", lambda e: e.tensor_scalar(out=mx[:], in0=mx[:], scalar1=-1.0, scalar2=None, op0=ALU.mult), reads=["mx"], writes=["mx"])
                    S.op("act", lambda e: e.activation(out=ex[:], in_=PS[:, 5, 0:NE], func=AF.Exp, bias=mx[:, 0:1], accum_out=se[:]),
                         reads=pk(5) + ["mx"], writes=["ex", "se"])
                    S.op("dve", lambda e: e.reciprocal(out=se[:], in_=se[:]), reads=["se"], writes=["se"])
                    S.op("dve", lambda e: e.tensor_scalar(out=AFF[:, s, tile, :], in0=ex[:], scalar1=se[:, 0:1], scalar2=None, op0=ALU.mult),
                         reads=["ex", "se"], writes=["AFF"])

                def alloc_post(ph):
                    W = {"cntB": 0}
                    W["xtB"] = [sbt(ph, f"xtB{i}", [128, D], F32) for i in range(1)]
                    W["osbs"] = [sbt(ph, f"osb{i}", [128, D], F32) for i in range(2)]
                    W["pending"] = None
                    W["ft"] = sbt(ph, "ft", [128, D], BF16)
                    W["fT"] = sbt(ph, "fT", [128, KC, 128], F32)
                    W["junk"] = sbt(ph, "junk", [128, D], BF16)
                    for nm in ("ss2", "rt2", "rstd2", "mx", "se"):
                        W[nm] = sbt(ph, nm, [128, 1], F32)
                    W["ex"] = sbt(ph, "ex", [128, NE], F32)
                    return W

                def load_norm_hT(s, tile, r, xtA, cntA, Wn, hT_dst, hkey):
                    rows = slice(tile * 128, (tile + 1) * 128)
                    xa = xtA[cntA % len(xtA)]
                    xak = f"xtA{cntA % len(xtA)}"
                    S.dma("sp", lambda e: e.dma_start(out=xa[:], in_=XR[s, rows, :]), xak, reads=[f"XR{s}_{tile}"], writes=[xak])
                    norm_stats(xa[:], xak, Wn["ss1"][:], Wn["rt1"][:], Wn["rstd1"][:], Wn["junk"][:], "1")
                    xn = Wn["xn"]
                    S.op("dve", lambda e: e.tensor_scalar(out=xn[:], in0=xa[:], scalar1=Wn["rstd1"][:, 0:1], scalar2=None, op0=ALU.mult),
                         reads=[xak, "rstd1"], writes=["xn"])
                    pT = PS[:, 0, :].bitcast(BF16).rearrange("p (k t) -> p k t", k=KC)

                    def f_tr(e):
                        ins = None
                        for kc in range(KC):
                            ins = e.transpose(pT[:, kc, :], xn[:, kc * 128:(kc + 1) * 128], identb[:])
                        return ins
                    S.op("pe", f_tr, reads=["xn", "identb"], writes=pk(0))

                    def f_ev(e):
                        ins = None
                        for kc in range(KC):
                            ins = e.activation(out=hT_dst[:, kc, :], in_=pT[:, kc, :], func=AF.Identity,
                                               scale=gs1T[:, kc, r:r + 1], bias=sh1T[:, kc, r:r + 1])
                        return ins
                    S.op("act", f_ev, reads=pk(0) + ["gsT0", "modT"], writes=[hkey])

                if not is_attn:
                    with ExitStack() as ph:
                        win = sbt(ph, "win", [128, KC, 4096], BF16)
                        wout = sbt(ph, "wout", [128, 16, D], BF16)
                        wsT = sbt(ph, "wsT", [128, 16, 128], BF16)
                        Cc = sbt(ph, "Cc", [128, 16, 128], F32)
                        binu = sbt(ph, "binu", [128, 16], F32)
                        binv = sbt(ph, "binv", [128, 2048], F32)
                        lng = sbt(ph, "lng", [128, 16], F32)
                        onesf = sbt(ph, "onesf", [128, 2], F32)
                        xtA = [sbt(ph, f"xtA{i}", [128, D], F32) for i in range(1)]
                        Wn = {"xn": sbt(ph, "xn", [128, D], BF16)}
                        for nm in ("ss1", "rt1", "rstd1", "sum1", "sum2", "mean", "msq", "var", "rsv"):
                            Wn[nm] = sbt(ph, nm, [128, 1], F32)
                        W = alloc_post(ph)
                        Wn["junk"] = W["junk"]
                        hT = sbt(ph, "hT", [128, KC, 512], BF16)
                        uT = sbt(ph, "uT", [128, 16, 512], BF16)
                        vf = sbt(ph, "vf", [128, 2048], F32)
                        vn = sbt(ph, "vn", [128, 2, 2048], BF16)
                        tmpS = sbt(ph, "tmpS", [128, 512], F32)
                        wsf = vn[:].rearrange("p a b -> p (a b)").bitcast(F32).rearrange("p (g q) -> p g q", g=16)
                        wsTf = vf[:].rearrange("p (g q) -> p g q", g=16)
                        uTf = uT[:].rearrange("p a b -> p (a b)").bitcast(F32)
                        lhs2 = uTf[0:2, 0:2048]
                        rhs2 = uTf[0:2, 2048:4096].rearrange("p (g q) -> p g q", g=16)

                        for kc in range(KC):
                            S.dma("pool", lambda e, kc=kc: e.dma_start(out=win[:, kc, :], in_=a_w_in[j, kc * 128:(kc + 1) * 128, :], max_dma_last_dim=8192),
                                  f"win{kc}", writes=["win"])
                        for cc in range(4):
                            S.dma("pool", lambda e, cc=cc: e.dma_start(
                                out=wout[:, cc * 4:(cc + 1) * 4, :], in_=a_w_out[j, cc * 512:(cc + 1) * 512, :].rearrange("(c p) n -> p c n", p=128)),
                                f"wout{cc}", writes=["wout"])
                        S.dma("sp", lambda e: e.dma_start(out=wsf[:], in_=a_w_s[j].rearrange("g p q -> p g q")), "a0", writes=["wsf"])
                        S.dma("sp", lambda e: e.dma_start(out=binu[:], in_=a_b_in[j, 0:2048].rearrange("(c p) -> p c", p=128), allow_slow_non_contiguous=True), "a1", writes=["binu"])
                        S.dma("sp", lambda e: e.dma_start(out=lng[:], in_=a_ln_g[j].rearrange("(c p) -> p c", p=128), allow_slow_non_contiguous=True), "a2", writes=["lng"])
                        S.dma("sp", lambda e: e.dma_start(out=binv[:], in_=a_b_in[j, 2048:4096].rearrange("(o d) -> o d", o=1).to_broadcast([128, 2048])),
                              "a3", writes=["binv"])
                        S.op("dve", lambda e: e.memset(lhs2[:], 1.0), writes=["lhs2a", "lhs2b"])
                        S.dma("sp", lambda e: e.dma_start(out=lhs2[0:1, :], in_=a_ln_b[j].rearrange("(o d) -> o d", o=1)), "a4", writes=["lhs2a"])
                        S.dma("sp", lambda e: e.dma_start(out=rhs2[1:2, :, :], in_=a_b_s[j].rearrange("(o g) p -> o g p", o=1)), "a5", writes=["rhs2b"])
                        S.op("dve", lambda e: e.memset(onesf[:], 1.0), writes=["onesf"])
                        for g4 in range(4):
                            def f_t(e, g4=g4):
                                ins = None
                                for gg in range(4):
                                    ins = e.transpose(PS[:, 1, gg * 128:(gg + 1) * 128], wsf[:, g4 * 4 + gg, :], identf[:])
                                return ins
                            S.op("pe", f_t, reads=["wsf", "identf"], writes=pk(1))
                            S.op("dve", lambda e, g4=g4: e.tensor_copy(out=wsT[:, g4 * 4:(g4 + 1) * 4, :], in_=PS[:, 1, :].rearrange("p (g q) -> p g q", g=4)),
                                 reads=pk(1), writes=["wsT"])
                        S.op("dve", lambda e: e.tensor_copy(out=wsTf[:], in_=wsT[:]), reads=["wsT"], writes=["wsTf"])
                        for g4 in range(4):
                            S.op("pe", lambda e, g4=g4: e.matmul(PS[0:2, 2, :], lhsT=onesf[:, 0:2], rhs=wsTf[:, g4 * 4:(g4 + 1) * 4, :].rearrange("p g q -> p (g q)"),
                                                              start=True, stop=True), reads=["wsTf", "onesf"], writes=pk(2))
                            S.op("dve", lambda e, g4=g4: e.tensor_copy(out=rhs2[0:1, g4 * 4:(g4 + 1) * 4, :], in_=PS[0:1, 2, :].rearrange("p (g q) -> p g q", g=4)),
                                 reads=pk(2), writes=["rhs2a"])
                        for g in range(16):
                            S.op("pe", lambda e, g=g: e.matmul(PS[:, 3, (g % 4) * 128:(g % 4 + 1) * 128], lhsT=lhs2[:, g * 128:(g + 1) * 128], rhs=rhs2[:, g, :],
                                                            start=True, stop=True),
                                 reads=["lhs2a", "lhs2b", "rhs2a", "rhs2b"], writes=pk(3))
                            if g % 4 == 3:
                                S.op("dve", lambda e, g=g: e.tensor_copy(out=Cc[:, g - 3:g + 1, :], in_=PS[:, 3, :].rearrange("p (g q) -> p g q", g=4)),
                                     reads=pk(3), writes=["Cc"])

                        S.barrier()

                        def do_group(s, gi):
                                tiles = list(range(gi * 4, gi * 4 + 4)) if gi < 8 else [32, 33]
                                nt = len(tiles)
                                ncol = nt * 128
                                r = s if gi < 8 else 2
                                for ti, tile in enumerate(tiles):
                                    load_norm_hT(s, tile, r, xtA, tile, Wn, hT[:, :, ti * 128:(ti + 1) * 128], "hT")
                                for fc in range(16):
                                    bnk = 1 + fc % 2

                                    def f_u(e, fc=fc, bnk=bnk):
                                        ins = None
                                        for kc in range(KC):
                                            ins = e.matmul(PS[:, bnk, 0:ncol], lhsT=win[:, kc, fc * 128:(fc + 1) * 128], rhs=hT[:, kc, 0:ncol],
                                                           start=(kc == 0), stop=(kc == KC - 1))
                                        return ins
                                    S.op("pe", f_u, reads=["win", "hT"], writes=pk(bnk))
                                    S.op("act", lambda e, fc=fc, bnk=bnk: e.activation(out=uT[:, fc, 0:ncol], in_=PS[:, bnk, 0:ncol], func=AF.Gelu, bias=binu[:, fc:fc + 1]),
                                         reads=pk(bnk) + ["binu"], writes=[f"uT{fc}_{t}" for t in range(nt)])
                                def v_part(ti):
                                    vb = ti % 2
                                    vnb = vn[:, vb, :]
                                    vk = f"vn{vb}"
                                    for vc in range(4):
                                        bnk = 3 + vc % 2

                                        def f_v(e, ti=ti, vc=vc, bnk=bnk):
                                            ins = None
                                            for kc in range(KC):
                                                ins = e.matmul(PS[:, bnk, :], lhsT=hT[:, kc, ti * 128:(ti + 1) * 128],
                                                               rhs=win[:, kc, 2048 + vc * 512:2048 + (vc + 1) * 512], start=(kc == 0), stop=(kc == KC - 1))
                                            return ins
                                        S.op("pe", f_v, reads=["win", "hT"], writes=pk(bnk))
                                        S.op("dve", lambda e, vc=vc, bnk=bnk: e.tensor_tensor(out=vf[:, vc * 512:(vc + 1) * 512], in0=PS[:, bnk, :],
                                                                                           in1=binv[:, vc * 512:(vc + 1) * 512], op=ALU.add),
                                             reads=pk(bnk) + ["binv"], writes=[f"vf{vc}"])
                                    vfk = [f"vf{v}" for v in range(4)]
                                    S.op("act", lambda e: e.activation(out=vf[:], in_=vf[:], func=AF.Gelu, accum_out=Wn["sum1"][:]),
                                         reads=vfk, writes=vfk + ["sum1"])
                                    S.op("act", lambda e, vnb=vnb: e.activation(out=vnb, in_=vf[:], func=AF.Square, accum_out=Wn["sum2"][:]),
                                         reads=vfk, writes=[vk, "sum2"])
                                    S.op("dve", lambda e: e.tensor_scalar(out=Wn["mean"][:], in0=Wn["sum1"][:], scalar1=1.0 / 2048, scalar2=None, op0=ALU.mult),
                                         reads=["sum1"], writes=["mean"])
                                    S.op("dve", lambda e: e.tensor_tensor(out=Wn["msq"][:], in0=Wn["mean"][:], in1=Wn["mean"][:], op=ALU.mult),
                                         reads=["mean"], writes=["msq"])
                                    S.op("dve", lambda e: e.scalar_tensor_tensor(out=Wn["var"][:], in0=Wn["sum2"][:], scalar=1.0 / 2048, in1=Wn["msq"][:],
                                                                                 op0=ALU.mult, op1=ALU.subtract), reads=["sum2", "msq"], writes=["var"])
                                    S.op("act", lambda e: e.activation(out=Wn["rsv"][:], in_=Wn["var"][:], func=AF.Sqrt, bias=epst[:, 0:1]),
                                         reads=["var"], writes=["rsv"])
                                    S.op("dve", lambda e: e.reciprocal(out=Wn["rsv"][:], in_=Wn["rsv"][:]), reads=["rsv"], writes=["rsv"])
                                    S.op("dve", lambda e, vnb=vnb: e.tensor_scalar(out=vnb, in0=vf[:], scalar1=Wn["mean"][:, 0:1], scalar2=Wn["rsv"][:, 0:1],
                                                                                op0=ALU.subtract, op1=ALU.mult),
                                         reads=vfk + ["mean", "rsv"], writes=[vk])

                                def s_part(ti):
                                    vb = ti % 2
                                    vnb = vn[:, vb, :]
                                    vk = f"vn{vb}"
                                    for gq in range(4):
                                        g0 = gq * 4
                                        bnk = 5 if gq % 2 == 0 else 1

                                        def f_s(e, g0=g0, bnk=bnk, vnb=vnb):
                                            ins = None
                                            for gg in range(4):
                                                ins = e.matmul(PS[:, bnk, gg * 128:(gg + 1) * 128], lhsT=vnb[:, (g0 + gg) * 128:(g0 + gg + 1) * 128], rhs=wsT[:, g0 + gg, :],
                                                               start=True, stop=True)
                                            return ins
                                        S.op("pe", f_s, reads=[vk, "wsT"], writes=pk(bnk))
                                        t3 = tmpS[:].rearrange("p (g q) -> p g q", g=4)
                                        S.op("dve", lambda e, g0=g0, bnk=bnk, t3=t3: e.tensor_tensor(
                                            out=t3, in0=PS[:, bnk, :].rearrange("p (g q) -> p g q", g=4),
                                            in1=lng[:, g0:g0 + 4].unsqueeze(2).to_broadcast([128, 4, 128]), op=ALU.mult),
                                            reads=pk(bnk) + ["lng"], writes=["tmpS"])
                                        S.op("pool", lambda e, g0=g0, t3=t3: e.tensor_tensor(out=t3, in0=t3, in1=Cc[:, g0:g0 + 4, :], op=ALU.add),
                                             reads=["tmpS", "Cc"], writes=["tmpS"])
                                        S.op("pool", lambda e, g0=g0, t3=t3, ti=ti: e.tensor_tensor(
                                            out=uT[:, g0:g0 + 4, ti * 128:(ti + 1) * 128], in0=uT[:, g0:g0 + 4, ti * 128:(ti + 1) * 128], in1=t3, op=ALU.mult),
                                            reads=[f"uT{g0 + gg}_{ti}" for gg in range(4)] + ["tmpS"], writes=[f"uT{g0 + gg}_{ti}" for gg in range(4)])

                                def o_part(ti):
                                    tile = tiles[ti]
                                    for half in range(2):
                                        def f_o(e, ti=ti, half=half):
                                            ins = None
                                            for cc in range(16):
                                                ins = e.matmul(PS[:, 6 + half, :], lhsT=uT[:, cc, ti * 128:(ti + 1) * 128], rhs=wout[:, cc, half * 512:(half + 1) * 512],
                                                               start=(cc == 0), stop=(cc == 15))
                                            return ins
                                        S.op("pe", f_o, reads=[f"uT{g}_{ti}" for g in range(16)] + ["wout"], writes=pk(6 + half))
                                    post_tile(s, tile, r, 6, W)


                                v_part(0)
                                for ti in range(nt):
                                    if ti + 1 < nt:
                                        v_part(ti + 1)
                                    s_part(ti)
                                    if ti >= 1:
                                        o_part(ti - 1)
                                o_part(nt - 1)

                        for s in range(NS):
                            for gi in range(9):
                                do_group(s, gi)
                        flush_post(W)
                        S.barrier()
                        S.emit()
                else:
                    with ExitStack() as ph:
                        qT = sbt(ph, "qT", [128, KC, NTOK], BF16)
                        kT = sbt(ph, "kT", [128, 4, NTOK], BF16)
                        Vt = sbt(ph, "Vt", [128, NT, 256], BF16)
                        SEa = sbt(ph, "SEa", [128, 16], F32)
                        SE = sbt(ph, "SE", [128, 4, 2], F32)
                        S.dma("sp", lambda e: e.dma_start(out=SEa[:], in_=b_sink[j].rearrange("(o d) -> o d", o=1).to_broadcast([128, 16])), "b0", writes=["SEa"])
                        S.op("act", lambda e: e.activation(out=SEa[:], in_=SEa[:], func=AF.Exp), reads=["SEa"], writes=["SEa"])
                        sev = SEa[:].rearrange("p (g i o) -> p g i o", g=4, i=2)
                        S.op("dve", lambda e: e.tensor_copy(out=SE[0:64, :, :], in_=sev[0:64, :, :, 0]), reads=["SEa"], writes=["SE0"])
                        S.op("dve", lambda e: e.tensor_copy(out=SE[64:128, :, :], in_=sev[64:128, :, :, 1]), reads=["SEa"], writes=["SE1"])
                        for s in range(NS):
                            with ExitStack() as p1:
                                wqkv = sbt(p1, "wqkv", [128, KC, 1536], BF16)
                                for kc in range(KC):
                                    S.dma("pool", lambda e, kc=kc, wqkv=wqkv: e.dma_start(out=wqkv[:, kc, :], in_=b_w_qkv[j, kc * 128:(kc + 1) * 128, :]), f"wq{kc % 2}", writes=["wqkv"])
                                xtA = [sbt(p1, f"xtA{i}", [128, D], F32) for i in range(2)]
                                Wn = {"xn": sbt(p1, "xn", [128, D], BF16), "junk": sbt(p1, "junk1", [128, D], BF16)}
                                for nm in ("ss1", "rt1", "rstd1"):
                                    Wn[nm] = sbt(p1, nm, [128, 1], F32)
                                hT1s = [sbt(p1, f"hT1{i}", [128, KC, 128], BF16) for i in range(2)]
                                cst = [sbt(p1, f"cst{i}", [128, 2, 32], F32) for i in range(2)]
                                Ar = sbt(p1, "Ar", [128, 20, 2, 32], F32)
                                Br = sbt(p1, "Br", [128, 20, 2, 32], F32)
                                qkrs = [sbt(p1, f"qkr{i}", [128, 20, 2, 32], BF16) for i in range(2)]
                                kds = [sbt(p1, f"kd{i}", [128, 4, 2, 64], BF16) for i in range(2)]

                                def stage_a(s, tile):
                                    r = s if tile < NLT else 2
                                    load_norm_hT(s, tile, r, xtA, tile, Wn, hT1s[tile % 2][:, :, :], f"hT1{tile % 2}")

                                def do_tile1(s, tile):
                                    r = s if tile < NLT else 2
                                    cols = slice(tile * 128, (tile + 1) * 128)
                                    hT1 = hT1s[tile % 2]
                                    hk = f"hT1{tile % 2}"
                                    qkr = qkrs[tile % 2]
                                    kd = kds[tile % 2]
                                    q0, q1, k0, k1 = (f"qkr0_{tile % 2}", f"qkr1_{tile % 2}", f"kd0_{tile % 2}", f"kd1_{tile % 2}")

                                    def f_qkv(e):
                                        ins = None
                                        for blk in range(3):
                                            for kc in range(KC):
                                                ins = e.matmul(PS[:, 1 + blk, :], lhsT=hT1[:, kc, :], rhs=wqkv[:, kc, blk * 512:(blk + 1) * 512],
                                                               start=(kc == 0), stop=(kc == KC - 1))
                                        return ins
                                    S.op("pe", f_qkv, reads=[hk, "wqkv"], writes=pk(1, 2, 3))
                                    X = PS[:, 1:4, :].rearrange("p b (h two d) -> p (b h) two d", two=2, d=32)
                                    S.op("act", lambda e, tile=tile: e.copy(out=Vt[:, tile, :], in_=PS[:, 3, 256:512]), reads=pk(3), writes=[f"Vt{tile}"])
                                    if tile < NLT:
                                        cs = cst[tile % 2]
                                        ck = f"cst{tile % 2}"
                                        S.dma("sp", lambda e, cs=cs, tile=tile: e.dma_start(out=cs[:, 0, :], in_=k_cos[tile * 128:(tile + 1) * 128, :]), ck + "c", writes=[ck + "c"])
                                        S.dma("sp", lambda e, cs=cs, tile=tile: e.dma_start(out=cs[:, 1, :], in_=k_sin[tile * 128:(tile + 1) * 128, :]), ck + "s", writes=[ck + "s"])
                                        S.op("dve", lambda e, cs=cs: e.tensor_tensor(out=Ar[:], in0=X[:, 0:20, :, :],
                                                                                  in1=cs[:, 0:1, :].unsqueeze(1).to_broadcast([128, 20, 2, 32]), op=ALU.mult),
                                             reads=pk(1, 2, 3) + [ck + "c"], writes=["Ar"])
                                        S.op("dve", lambda e, cs=cs: e.tensor_tensor(out=Br[:, :, 0, :], in0=X[:, 0:20, 1, :],
                                                                                  in1=cs[:, 1:2, :].to_broadcast([128, 20, 32]), op=ALU.mult),
                                             reads=pk(1, 2, 3) + [ck + "s"], writes=["Br0"])
                                        S.op("dve", lambda e, cs=cs: e.tensor_tensor(out=Br[:, :, 1, :], in0=X[:, 0:20, 0, :],
                                                                                  in1=cs[:, 1:2, :].to_broadcast([128, 20, 32]), op=ALU.mult),
                                             reads=pk(1, 2, 3) + [ck + "s"], writes=["Br1"])
                                        S.op("pool", lambda e: e.tensor_tensor(out=qkr[:, :, 0, :], in0=Ar[:, :, 0, :], in1=Br[:, :, 0, :], op=ALU.subtract),
                                             reads=["Ar", "Br0"], writes=[q0])
                                        S.op("pool", lambda e: e.tensor_tensor(out=qkr[:, :, 1, :], in0=Ar[:, :, 1, :], in1=Br[:, :, 1, :], op=ALU.add),
                                             reads=["Ar", "Br1"], writes=[q1])
                                    else:
                                        S.op("dve", lambda e: e.tensor_copy(out=qkr[:], in_=X[:, 0:20, :, :]), reads=pk(1, 2, 3), writes=[q0, q1])
                                    kv = qkr[:, 16:20, :, :].rearrange("p h two d -> p h (two d)")
                                    S.op("pool", lambda e: e.tensor_copy(out=kd[:, :, 0, :], in_=kv), reads=[q0, q1], writes=[k0])
                                    S.op("pool", lambda e: e.tensor_copy(out=kd[:, :, 1, :], in_=kv), reads=[q0, q1], writes=[k1])

                                def do_tile1b(s, tile):
                                    cols = slice(tile * 128, (tile + 1) * 128)
                                    qkr = qkrs[tile % 2]
                                    kd = kds[tile % 2]
                                    q0, q1, k0, k1 = (f"qkr0_{tile % 2}", f"qkr1_{tile % 2}", f"kd0_{tile % 2}", f"kd1_{tile % 2}")
                                    pTq = PS[:, 4, :].bitcast(BF16).rearrange("p (k t) -> p k t", k=KC)
                                    pTk = PS[:, 5, :].bitcast(BF16).rearrange("p (k t) -> p k t", k=KC)
                                    qf = qkr[:].rearrange("p h two d -> p (h two d)")

                                    def f_tq(e):
                                        ins = None
                                        for c in range(KC):
                                            ins = e.transpose(pTq[:, c, :], qf[:, c * 128:(c + 1) * 128], identb[:])
                                        for g in range(4):
                                            ins = e.transpose(pTk[:, g, :], kd[:, g, :, :].rearrange("p a d -> p (a d)"), identb[:])
                                        return ins
                                    S.op("pe", f_tq, reads=[q0, q1, k0, k1, "identb"], writes=pk(4, 5))
                                    S.op("act", lambda e, cols=cols: e.copy(out=qT[:, :, cols], in_=pTq[:, :, :]), reads=pk(4), writes=[f"qT{tile}"])
                                    S.op("dve", lambda e, cols=cols: e.tensor_copy(out=kT[:, :, cols], in_=pTk[:, 0:4, :]), reads=pk(5), writes=[f"kT{tile}"])

                                stage_a(s, 0)
                                for tile in range(NT):
                                    if tile + 1 < NT:
                                        stage_a(s, tile + 1)
                                    do_tile1(s, tile)
                                    if tile >= 1:
                                        do_tile1b(s, tile - 1)
                                do_tile1b(s, NT - 1)
                                S.barrier()
                                S.emit()
                            with ExitStack() as p2:
                                W = alloc_post(p2)
                                PTt = [sbt(p2, f"PTt{i}", [128, 5, 2, 256], BF16) for i in range(2)]
                                wo = sbt(p2, "wo", [128, KC, D], BF16)
                                for kc in range(KC):
                                    S.dma("pool", lambda e, kc=kc, wo=wo: e.dma_start(out=wo[:, kc, :], in_=b_w_o[j, kc * 128:(kc + 1) * 128, :]), f"wo{kc % 2}", writes=["wo"])
                                oT = sbt(p2, "oT", [128, KC, 128], BF16)
                                dsb = sbt(p2, "dsb", [128, 256], F32)
                                cg = {"n": 0}

                                def do_qblock(s, qb):
                                    r = s if qb < NLT else 2
                                    qcols = slice(qb * 128, (qb + 1) * 128)
                                    if qb < NLT:
                                        keys = []
                                        if qb - 1 >= 0:
                                            keys.append((qb - 1, mlb))
                                        keys.append((qb, None))
                                        if qb + 1 < NLT:
                                            keys.append((qb + 1, mrb))
                                        keys += [(32, None), (33, None)]
                                    else:
                                        keys = [(32, None), (33, None)]
                                    nk = len(keys)
                                    def qk_part(g):
                                        PT = PTt[g % 2]
                                        ptk = f"PT{g % 2}_"
                                        for idx, (kj, mk) in enumerate(keys):
                                            kcols = slice(kj * 128, (kj + 1) * 128)
                                            ba = 2 + (idx % 2) * 2
                                            bb = ba + 1

                                            def f_qk(e, g=g, kcols=kcols, ba=ba, bb=bb):
                                                e.matmul(PS[:, ba, 0:256].rearrange("p (a b) -> p a b", a=2), lhsT=kT[0:64, g, kcols], rhs=qT[0:64, 2 * g:2 * g + 2, qcols],
                                                         start=True, stop=True)
                                                return e.matmul(PS[:, bb, 0:256].rearrange("p (a b) -> p a b", a=2), lhsT=kT[64:128, g, kcols], rhs=qT[64:128, 2 * g:2 * g + 2, qcols],
                                                                start=True, stop=True)
                                            S.op("pe", f_qk, reads=[f"kT{kj}", f"qT{qb}"], writes=pk(ba, bb))

                                            def f_ex(e, idx=idx, ba=ba, bb=bb, PT=PT):
                                                e.activation(out=PT[:, idx, 0, :], in_=PS[:, ba, 0:256], func=AF.Exp, scale=0.125)
                                                return e.activation(out=PT[:, idx, 1, :], in_=PS[:, bb, 0:256], func=AF.Exp, scale=0.125)
                                            S.op("act", f_ex, reads=pk(ba, bb), writes=[ptk + str(idx)])
                                            if mk is not None:
                                                S.op("dve", lambda e, idx=idx, mk=mk, PT=PT: e.tensor_tensor(
                                                    out=PT[:, idx, :, :].rearrange("p a (h q) -> p (a h) q", h=2),
                                                    in0=PT[:, idx, :, :].rearrange("p a (h q) -> p (a h) q", h=2),
                                                    in1=mk[:].unsqueeze(1).to_broadcast([128, 4, 128]), op=ALU.mult),
                                                    reads=[ptk + str(idx)], writes=[ptk + str(idx)])


                                    def pv_part(g):
                                        PT = PTt[g % 2]
                                        ptk = f"PT{g % 2}_"
                                        def f_pv(e, g=g, PT=PT):
                                            ins = None
                                            for idx, (kj, mk) in enumerate(keys):
                                                st_, sp_ = (idx == 0), (idx == nk - 1)
                                                e.matmul(PS[0:64, 0, 0:256], lhsT=Vt[:, kj, g * 64:(g + 1) * 64], rhs=PT[:, idx, 0, :], start=st_, stop=sp_)
                                                e.matmul(PS[64:128, 0, 0:256], lhsT=Vt[:, kj, g * 64:(g + 1) * 64], rhs=PT[:, idx, 1, :], start=st_, stop=sp_)
                                                e.matmul(PS[0:64, 1, 0:256], lhsT=onesb[:, 0:64], rhs=PT[:, idx, 0, :], start=st_, stop=sp_)
                                                ins = e.matmul(PS[64:128, 1, 0:256], lhsT=onesb[:, 0:64], rhs=PT[:, idx, 1, :], start=st_, stop=sp_)
                                            return ins
                                        S.op("pe", f_pv, reads=[ptk + str(i) for i in range(nk)] + [f"Vt{kj}" for kj, _ in keys] + ["onesb"], writes=pk(0, 1))
                                        S.op("dve", lambda e, g=g: e.tensor_tensor(out=dsb[:].rearrange("p (i q) -> p i q", i=2), in0=PS[:, 1, 0:256].rearrange("p (i q) -> p i q", i=2),
                                                                                in1=SE[:, g, :].unsqueeze(2).to_broadcast([128, 2, 128]), op=ALU.add),
                                             reads=pk(1) + ["SE0", "SE1"], writes=["dsb"])
                                        S.op("dve", lambda e: e.reciprocal(out=dsb[:], in_=dsb[:]), reads=["dsb"], writes=["dsb"])
                                        S.op("dve", lambda e, g=g: e.tensor_tensor(out=oT[:, 2 * g:2 * g + 2, :], in0=PS[:, 0, 0:256].rearrange("p (i q) -> p i q", i=2),
                                                                                in1=dsb[:].rearrange("p (i q) -> p i q", i=2), op=ALU.mult),
                                             reads=pk(0) + ["dsb"], writes=[f"oT{g}"])

                                    qk_part(0)
                                    for g in range(4):
                                        if g + 1 < 4:
                                            qk_part(g + 1)
                                        pv_part(g)
                                    for half in range(2):
                                        def f_wo(e, half=half):
                                            ins = None
                                            for c in range(KC):
                                                ins = e.matmul(PS[:, 6 + half, :], lhsT=oT[:, c, :], rhs=wo[:, c, half * 512:(half + 1) * 512],
                                                               start=(c == 0), stop=(c == KC - 1))
                                            return ins
                                        S.op("pe", f_wo, reads=[f"oT{g}" for g in range(4)] + ["wo"], writes=pk(6 + half))
                                    post_tile(s, qb, r, 6, W)

                                for qb in range(NT):
                                    do_qblock(s, qb)
                                flush_post(W)
                                S.barrier()
                                S.emit()

                if skip_moe:
                    continue
                IDX = sbt(lst, "IDX", [128, NS, NE, 4], I32)
                GAT = sbt(lst, "GAT", [128, NS, NE, 4], F32)
                IDXC = sbt(lst, "IDXC", [32, NS, NE], I32)
                GATC = sbt(lst, "GATC", [32, NS, NE], F32)
                with ExitStack() as ph:
                    LO = sbt(ph, "LO", [128, NS, 2, NE], F32)
                    MID = sbt(ph, "MID", [128, NS, 2, NE], F32)
                    GE = sbt(ph, "GE", [128, NS, 2, NE], F32)
                    CMP = sbt(ph, "CMP", [128, NS, NT, NE], BF16)
                    CNTP = sbt(ph, "CNTP", [128, NS, 2, NE], F32)
                    S.op("dve", lambda e: e.memset(LO[:], 0.0), writes=["LO"])
                    for it in range(NBIS):
                        wv = 2.0 ** -(it + 1)
                        S.op("dve", lambda e, wv=wv: e.tensor_scalar(out=MID[:], in0=LO[:], scalar1=wv, scalar2=None, op0=ALU.add), reads=["LO"], writes=["MID"])
                        S.op("dve", lambda e: e.tensor_tensor(out=CMP[:, :, 0:NLT, :], in0=AFF[:, :, 0:NLT, :],
                                                             in1=MID[:, :, 0:1, :].to_broadcast([128, NS, NLT, NE]), op=ALU.is_ge),
                             reads=["AFF", "MID"], writes=["CMPl"])
                        S.op("dve", lambda e: e.tensor_tensor(out=CMP[:, :, NLT:NT, :], in0=AFF[:, :, NLT:NT, :],
                                                             in1=MID[:, :, 1:2, :].to_broadcast([128, NS, 2, NE]), op=ALU.is_ge),
                             reads=["AFF", "MID"], writes=["CMPc"])
                        S.op("dve", lambda e: e.tensor_reduce(out=CNTP[:, :, 0, :], in_=CMP[:, :, 0:NLT, :].rearrange("p s t e -> p s e t"), axis=AX.X, op=ALU.add),
                             reads=["CMPl"], writes=["CNTPl"])
                        S.op("dve", lambda e: e.tensor_reduce(out=CNTP[:, :, 1, :], in_=CMP[:, :, NLT:NT, :].rearrange("p s t e -> p s e t"), axis=AX.X, op=ALU.add),
                             reads=["CMPc"], writes=["CNTPc"])
                        S.op("pe", lambda e: e.matmul(PS[:, 0, 0:64], lhsT=onesF[:], rhs=CNTP[:].rearrange("p s a e -> p (s a e)"), start=True, stop=True),
                             reads=["CNTPl", "CNTPc", "onesF"], writes=pk(0))
                        pc = PS[:, 0, 0:64].rearrange("p (s a e) -> p s a e", s=NS, a=2)
                        S.op("dve", lambda e, pc=pc: e.tensor_scalar(out=GE[:, :, 0, :], in0=pc[:, :, 0, :], scalar1=float(CAP_L), scalar2=None, op0=ALU.is_ge),
                             reads=pk(0), writes=["GEl"])
                        S.op("dve", lambda e, pc=pc: e.tensor_scalar(out=GE[:, :, 1, :], in0=pc[:, :, 1, :], scalar1=float(CAP_C), scalar2=None, op0=ALU.is_ge),
                             reads=pk(0), writes=["GEc"])
                        S.op("dve", lambda e, wv=wv: e.scalar_tensor_tensor(out=LO[:], in0=GE[:], scalar=wv, in1=LO[:], op0=ALU.mult, op1=ALU.add),
                             reads=["GEl", "GEc", "LO"], writes=["LO"])
                    SEL = CMP
                    OFFS = sbt(ph, "OFFS", [128, NS, NT, NE], F32)
                    POS = sbt(ph, "POS", [128, NS, NT, NE], F32)
                    LT = sbt(ph, "LT", [128, NS, NT, NE], F32)
                    LOI = sbt(ph, "LOI", [128, NS, NT, NE], F32)
                    HI = sbt(ph, "HI", [128, NS, NT, NE], F32)
                    VALS = sbt(ph, "VALS", [128, NS, NT, NE, 4], BF16)
                    ALf = sbt(ph, "ALf", [128, NS, NT, NE], F32)
                    Hh = [sbt(ph, f"Hh{i}", [128, NLT, 128], BF16) for i in range(2)]
                    L4 = [sbt(ph, f"L4{i}", [128, NLT, 4], BF16) for i in range(2)]
                    Rr = [sbt(ph, f"Rr{i}", [128, NLT, 4, 4], BF16) for i in range(2)]
                    Hc = sbt(ph, "Hc", [128, 2, NE, 32], BF16)
                    IDXF = sbt(ph, "IDXF", [128, NS, NE, 4], F32)
                    pisb = sbt(ph, "pisb", [128, 2, 16], F32)
                    picsb = sbt(ph, "picsb", [32, 64], F32)
                    IDXCF = sbt(ph, "IDXCF", [32, NS, NE], F32)
                    S.op("dve", lambda e: e.tensor_tensor(out=SEL[:, :, 0:NLT, :], in0=AFF[:, :, 0:NLT, :],
                                                         in1=LO[:, :, 0:1, :].to_broadcast([128, NS, NLT, NE]), op=ALU.is_ge), reads=["AFF", "LO"], writes=["CMPl"])
                    S.op("dve", lambda e: e.tensor_tensor(out=SEL[:, :, NLT:NT, :], in0=AFF[:, :, NLT:NT, :],
                                                         in1=LO[:, :, 1:2, :].to_broadcast([128, NS, 2, NE]), op=ALU.is_ge), reads=["AFF", "LO"], writes=["CMPc"])
                    self_flat = SEL[:].rearrange("p s t e -> p (s t e)")
                    NTOT = NS * NT * NE

                    def f_cs(e):
                        ins = None
                        for (c0, c1, b) in ((0, 512, 0), (512, 1024, 1), (1024, NTOT, 2)):
                            e.matmul(PS[:, b, 0:c1 - c0], lhsT=upperb[:], rhs=self_flat[:, c0:c1], start=True, stop=True)
                            ins = e.matmul(PS[:, 3 + b, 0:c1 - c0], lhsT=onesb[:], rhs=self_flat[:, c0:c1], start=True, stop=True)
                        return ins
                    S.op("pe", f_cs, reads=["CMPl", "CMPc", "upperb", "onesb"], writes=pk(0, 1, 2, 3, 4, 5))
                    Wp = PS[:, 0:3, :].rearrange("p b n -> p (b n)")[:, 0:NTOT].rearrange("p (s t e) -> p s t e", s=NS, t=NT)
                    Tp = PS[:, 3:6, :].rearrange("p b n -> p (b n)")[:, 0:NTOT].rearrange("p (s t e) -> p s t e", s=NS, t=NT)
                    S.op("dve", lambda e: e.memset(OFFS[:], 0.0), writes=["OFFS"])
                    for t in range(1, NLT):
                        S.op("dve", lambda e, t=t: e.tensor_tensor(out=OFFS[:, :, t, :], in0=OFFS[:, :, t - 1, :], in1=Tp[:, :, t - 1, :], op=ALU.add),
                             reads=pk(3, 4, 5) + ["OFFS"], writes=["OFFS"])
                    S.op("dve", lambda e: e.tensor_copy(out=OFFS[:, :, NLT + 1, :], in_=Tp[:, :, NLT, :]), reads=pk(3, 4, 5) + ["OFFS"], writes=["OFFS"])
                    S.op("dve", lambda e: e.tensor_tensor(out=POS[:], in0=Wp, in1=OFFS[:], op=ALU.add), reads=pk(0, 1, 2) + ["OFFS"], writes=["POS"])
                    S.op("dve", lambda e: e.tensor_scalar(out=LT[:, :, 0:NLT, :], in0=POS[:, :, 0:NLT, :], scalar1=float(CAP_L), scalar2=None, op0=ALU.is_lt),
                         reads=["POS"], writes=["LTl"])
                    S.op("dve", lambda e: e.tensor_scalar(out=LT[:, :, NLT:NT, :], in0=POS[:, :, NLT:NT, :], scalar1=float(CAP_C), scalar2=None, op0=ALU.is_lt),
                         reads=["POS"], writes=["LTc"])
                    S.op("dve", lambda e: e.tensor_tensor(out=LT[:], in0=LT[:], in1=SEL[:], op=ALU.mult), reads=["LTl", "LTc", "CMPl", "CMPc"], writes=["LT"])
                    S.op("dve", lambda e: e.scalar_tensor_tensor(out=POS[:], in0=POS[:], scalar=1.0, in1=LT[:], op0=ALU.add, op1=ALU.mult),
                         reads=["POS", "LT"], writes=["POS"])
                    S.op("dve", lambda e: e.tensor_scalar(out=POS[:], in0=POS[:], scalar1=-1.0, scalar2=None, op0=ALU.add), reads=["POS"], writes=["POS"])
                    S.op("dve", lambda e: e.tensor_scalar(out=LOI[:], in0=POS[:], scalar1=128.0, scalar2=None, op0=ALU.is_ge), reads=["POS"], writes=["LOI"])
                    for thr in (256.0, 384.0):
                        S.op("dve", lambda e, thr=thr: e.scalar_tensor_tensor(out=LOI[:], in0=POS[:], scalar=thr, in1=LOI[:], op0=ALU.is_ge, op1=ALU.add),
                             reads=["POS", "LOI"], writes=["LOI"])
                    S.op("dve", lambda e: e.scalar_tensor_tensor(out=HI[:], in0=LOI[:], scalar=-128.0, in1=POS[:], op0=ALU.mult, op1=ALU.add),
                         reads=["POS", "LOI"], writes=["HI"])
                    S.op("dve", lambda e: e.tensor_copy(out=VALS[:, :, :, :, 0:2], in_=ptf[:].unsqueeze(1).unsqueeze(3).to_broadcast([128, NS, NT, NE, 2])),
                         reads=["ptf"], writes=["VALS01"])
                    S.op("dve", lambda e: e.tensor_copy(out=VALS[:, :, :, :, 2], in_=AFF[:]), reads=["AFF"], writes=["VALS2"])
                    S.op("dve", lambda e: e.tensor_tensor(out=ALf[:], in0=AFF[:], in1=VALS[:, :, :, :, 2], op=ALU.subtract), reads=["AFF", "VALS2"], writes=["ALf"])
                    S.op("dve", lambda e: e.tensor_copy(out=VALS[:, :, :, :, 3], in_=ALf[:]), reads=["ALf"], writes=["VALS3"])
                    vkeys = ["VALS01", "VALS2", "VALS3"]
                    cnt = 0
                    for s in range(NS):
                        for ee in range(NE):
                            b = cnt % 2
                            cnt += 1
                            S.op("dve", lambda e, s=s, ee=ee, b=b: e.tensor_tensor(
                                out=Hh[b][:], in0=iotaf[:].unsqueeze(1).to_broadcast([128, NLT, 128]),
                                in1=HI[:, s, 0:NLT, ee:ee + 1].to_broadcast([128, NLT, 128]), op=ALU.is_equal),
                                reads=["HI", "iotaf"], writes=[f"Hh{b}"])
                            S.op("dve", lambda e, s=s, ee=ee, b=b: e.tensor_tensor(
                                out=L4[b][:], in0=iotaf[:, 0:4].unsqueeze(1).to_broadcast([128, NLT, 4]),
                                in1=LOI[:, s, 0:NLT, ee:ee + 1].to_broadcast([128, NLT, 4]), op=ALU.is_equal),
                                reads=["LOI", "iotaf"], writes=[f"L4{b}"])
                            S.op("pool", lambda e, s=s, ee=ee, b=b: e.tensor_tensor(
                                out=Rr[b][:], in0=L4[b][:].unsqueeze(3).to_broadcast([128, NLT, 4, 4]),
                                in1=VALS[:, s, 0:NLT, ee, :].unsqueeze(2).to_broadcast([128, NLT, 4, 4]), op=ALU.mult),
                                reads=[f"L4{b}"] + vkeys, writes=[f"Rr{b}"])
                            bnk = 6 + b

                            def f_oh(e, b=b, bnk=bnk):
                                ins = None
                                for t in range(NLT):
                                    ins = e.matmul(PS[:, bnk, 0:16], lhsT=Hh[b][:, t, :], rhs=Rr[b][:, t, :, :].rearrange("p a v -> p (a v)"),
                                                   start=(t == 0), stop=(t == NLT - 1))
                                return ins
                            S.op("pe", f_oh, reads=[f"Hh{b}", f"Rr{b}"], writes=pk(bnk))
                            S.op("act", lambda e, b=b, bnk=bnk: e.copy(out=pisb[:, b, :], in_=PS[:, bnk, 0:16]), reads=pk(bnk), writes=[f"pisb{b}"])
                            pi = pisb[:, b, :].rearrange("p (a v) -> p a v", a=4)
                            S.op("dve", lambda e, s=s, ee=ee, pi=pi: e.scalar_tensor_tensor(out=IDXF[:, s, ee, :], in0=pi[:, :, 1], scalar=128.0, in1=pi[:, :, 0],
                                                                                         op0=ALU.mult, op1=ALU.add), reads=[f"pisb{b}"], writes=["IDXF"])
                            S.op("dve", lambda e, s=s, ee=ee, pi=pi: e.tensor_tensor(out=GAT[:, s, ee, :], in0=pi[:, :, 2], in1=pi[:, :, 3], op=ALU.add),
                                 reads=[f"pisb{b}"], writes=["GAT"])
                        S.op("dve", lambda e, s=s: e.tensor_tensor(out=Hc[:], in0=iotaf[:, 0:32].unsqueeze(1).unsqueeze(1).to_broadcast([128, 2, NE, 32]),
                                                                in1=POS[:, s, NLT:NT, :].unsqueeze(3).to_broadcast([128, 2, NE, 32]), op=ALU.is_equal),
                             reads=["POS", "iotaf"], writes=["Hc"])

                        def f_c(e, s=s):
                            ins = None
                            for ee in range(NE):
                                for t in range(2):
                                    ins = e.matmul(PS[0:32, 5, ee * 4:(ee + 1) * 4], lhsT=Hc[:, t, ee, :], rhs=VALS[:, s, NLT + t, ee, :],
                                                   start=(t == 0), stop=(t == 1))
                            return ins
                        S.op("pe", f_c, reads=["Hc"] + vkeys, writes=pk(5))
                        S.op("act", lambda e: e.copy(out=picsb[:], in_=PS[0:32, 5, 0:64]), reads=pk(5), writes=["picsb"])
                        pic = picsb[:].rearrange("p (a v) -> p a v", a=NE)
                        S.op("dve", lambda e, s=s, pic=pic: e.scalar_tensor_tensor(out=IDXCF[:, s, :], in0=pic[:, :, 1], scalar=128.0, in1=pic[:, :, 0],
                                                                                op0=ALU.mult, op1=ALU.add), reads=["picsb"], writes=["IDXCF"])
                        S.op("dve", lambda e, s=s, pic=pic: e.tensor_tensor(out=GATC[:, s, :], in0=pic[:, :, 2], in1=pic[:, :, 3], op=ALU.add),
                             reads=["picsb"], writes=["GATC"])
                    S.op("dve", lambda e: e.tensor_scalar(out=IDXF[:, 1, :, :], in0=IDXF[:, 1, :, :], scalar1=float(NTOK), scalar2=None, op0=ALU.add),
                         reads=["IDXF"], writes=["IDXF"])
                    S.op("dve", lambda e: e.tensor_scalar(out=IDXCF[:, 1, :], in0=IDXCF[:, 1, :], scalar1=float(NTOK), scalar2=None, op0=ALU.add),
                         reads=["IDXCF"], writes=["IDXCF"])
                    S.op("dve", lambda e: e.tensor_copy(out=IDX[:], in_=IDXF[:]), reads=["IDXF"], writes=["IDX"])
                    S.op("dve", lambda e: e.tensor_copy(out=IDXC[:], in_=IDXCF[:]), reads=["IDXCF"], writes=["IDXC"])
                    if dbg_route:
                        for s in range(NS):
                            S.dma("sp", lambda e, s=s: e.dma_start(out=dbg_d[1, s * 128:(s + 1) * 128, 0:544], in_=AFF[:, s].rearrange("p t e -> p (t e)")), "dr0", reads=["AFF"], writes=["dbgr0"])
                            S.dma("sp", lambda e, s=s: e.dma_start(out=dbg_d[1, 768 + s * 128:768 + (s + 1) * 128, 0:544], in_=POS[:, s].rearrange("p t e -> p (t e)")), "dr5", reads=["POS"], writes=["dbgr5"])
                            S.dma("sp", lambda e, s=s: e.dma_start(out=dbg_d[1, 1024 + s * 128:1024 + (s + 1) * 128, 0:544], in_=HI[:, s].rearrange("p t e -> p (t e)")), "dr6", reads=["HI"], writes=["dbgr6"])
                        S.dma("sp", lambda e: e.dma_start(out=dbg_d[1, 256:384, 0:128], in_=IDXF[:].rearrange("p s e k -> p (s e k)")), "dr1", reads=["IDXF"], writes=["dbgr1"])
                        S.dma("sp", lambda e: e.dma_start(out=dbg_d[1, 384:512, 0:128], in_=GAT[:].rearrange("p s e k -> p (s e k)")), "dr2", reads=["GAT"], writes=["dbgr2"])
                        S.dma("sp", lambda e: e.dma_start(out=dbg_d[1, 512:640, 0:64], in_=LO[:].rearrange("p s a e -> p (s a e)")), "dr3", reads=["LO"], writes=["dbgr3"])
                        S.dma("sp", lambda e: e.dma_start(out=dbg_d[1, 640:672, 0:32], in_=IDXCF[:].rearrange("p s e -> p (s e)")), "dr4", reads=["IDXCF"], writes=["dbgr4"])
                    S.barrier()
                    S.emit()

                if dbg_route:
                    continue
                mod_phase([10, 11], False)

                with ExitStack() as ph:
                    WP = [sbt(ph, f"WP{i}", [128, 4096], BF16) for i in range(8)]
                    XG = [[sbt(ph, f"XG{p}{s}", [128, 5, D], BF16) for s in range(NS)] for p in range(2)]
                    XgT = [sbt(ph, f"XgT{s}", [128, KC, 544], BF16) for s in range(NS)]
                    gT = [sbt(ph, f"gT{s}", [128, 16, 544], BF16) for s in range(NS)]
                    sa = [sbt(ph, f"sa{i}", [128, 544], BF16) for i in range(2)]
                    ysb = [sbt(ph, f"ysb{i}", [128, D], F32) for i in range(2)]
                    state = {"pc": 0, "sa": 0, "y": 0}
                    piece_slot = {}

                    def issue_piece(ee, i):
                        pidx = ee * 12 + i
                        slot = pidx % 8
                        piece_slot[(ee, i)] = slot
                        if i < 8:
                            wsrc = e_w1 if i % 2 == 0 else e_w3
                            fg = i // 2
                            src = wsrc[L, ee][:, fg * 512:(fg + 1) * 512].rearrange("(kc p) n -> p kc n", p=128)
                            dst = WP[slot][:].rearrange("p (kc n) -> p kc n", kc=KC)
                        else:
                            fq = i - 8
                            src = e_w2[L, ee][fq * 512:(fq + 1) * 512, :].rearrange("(fc p) n -> p fc n", p=128)
                            dst = WP[slot][:].rearrange("p (fc n) -> p fc n", fc=4)
                        S.dma("pool", lambda e, src=src, dst=dst: e.dma_start(out=dst, in_=src), f"wp{slot}", writes=[f"WP{slot}"])

                    def issue_gathers(ee):
                        par = ee % 2
                        for s in range(NS):
                            for k in range(4):
                                S.dma("pool", lambda e, s=s, k=k, par=par, ee=ee: e.indirect_dma_start(
                                    out=XG[par][s][:, k, :], out_offset=None, in_=FDflat,
                                    in_offset=bass.IndirectOffsetOnAxis(ap=IDX[:, s, ee, k:k + 1], axis=0)),
                                    f"xg{par}{s}{k}", reads=["IDX", f"FD{s}"], writes=[f"XG{par}{s}{k}"])
                            S.dma("pool", lambda e, s=s, par=par, ee=ee: e.indirect_dma_start(
                                out=XG[par][s][0:32, 4, :], out_offset=None, in_=FDflat,
                                in_offset=bass.IndirectOffsetOnAxis(ap=IDXC[0:32, s, ee:ee + 1], axis=0)),
                                f"xg{par}{s}4", reads=["IDXC", f"FD{s}"], writes=[f"XG{par}{s}4"])

                    issue_gathers(0)
                    for i in range(8):
                        issue_piece(0, i)
                    def do_expert(ee):
                        par = ee % 2
                        if ee + 1 < NE:
                            issue_gathers(ee + 1)
                        pT = PS[:, 0, :].bitcast(BF16).rearrange("p (k t) -> p k t", k=KC)
                        for s in range(NS):
                            for k in range(5):
                                rows = 128 if k < 4 else 32
                                r = s if k < 4 else 2

                                def f_tr(e, s=s, k=k, rows=rows, par=par):
                                    ins = None
                                    for kc in range(KC):
                                        ins = e.transpose(pT[:, kc, 0:rows], XG[par][s][0:rows, k, kc * 128:(kc + 1) * 128], identb[0:rows, 0:rows])
                                    return ins
                                S.op("pe", f_tr, reads=[f"XG{par}{s}{k}", "identb"], writes=pk(0))

                                def f_ev(e, s=s, k=k, rows=rows, r=r):
                                    ins = None
                                    for kc in range(KC):
                                        ins = e.activation(out=XgT[s][:, kc, k * 128:k * 128 + rows], in_=pT[:, kc, 0:rows], func=AF.Identity,
                                                           scale=gs2T[:, kc, r:r + 1], bias=sh2T[:, kc, r:r + 1])
                                    return ins
                                S.op("act", f_ev, reads=pk(0), writes=[f"XgT{s}_{k}"])
                        xgk = [[f"XgT{s}_{k}" for k in range(5)] for s in range(NS)]
                        if dbg_exp and ee == 0:
                            S.dma("pool", lambda e: e.dma_start(out=dbg_d[1, 0:128, :], in_=XG[0][0][:, 0, :]), "de0", reads=["XG000"], writes=["dbge0"])
                            S.dma("pool", lambda e: e.dma_start(out=dbg_d[1, 128:256, 0:544], in_=XgT[0][:, 0, :]), "de1", reads=xgk[0], writes=["dbge1"])
                            S.dma("pool", lambda e: e.dma_start(out=dbg_d[1, 512:640, :], in_=FD[0, 0:128, :]), "de4", writes=["dbge4"])
                            S.dma("sp", lambda e: e.dma_start(out=dbg_d[1, 640:768, 0:128], in_=IDX[:].rearrange("p s e k -> p (s e k)").bitcast(F32)), "de5", reads=["IDX"], writes=["dbge5"])
                        for fg in range(4):
                            s1 = piece_slot[(ee, 2 * fg)]
                            s3 = piece_slot[(ee, 2 * fg + 1)]
                            W1p = WP[s1][:].rearrange("p (kc n) -> p kc n", kc=KC)
                            W3p = WP[s3][:].rearrange("p (kc n) -> p kc n", kc=KC)
                            for f4 in range(4):
                                fc = fg * 4 + f4
                                for s in range(NS):
                                    sab = sa[state["sa"] % 2]
                                    sak = f"sa{state['sa'] % 2}"
                                    state["sa"] += 1

                                    def f_a(e, Wp_=W1p, f4=f4, s=s, b0=1, b1=2):
                                        ins = None
                                        for kc in range(KC):
                                            e.matmul(PS[:, b0, :], lhsT=Wp_[:, kc, f4 * 128:(f4 + 1) * 128], rhs=XgT[s][:, kc, 0:512], start=(kc == 0), stop=(kc == KC - 1))
                                            ins = e.matmul(PS[:, b1, 0:32], lhsT=Wp_[:, kc, f4 * 128:(f4 + 1) * 128], rhs=XgT[s][:, kc, 512:544], start=(kc == 0), stop=(kc == KC - 1))
                                        return ins
                                    S.op("pe", f_a, reads=[f"WP{s1}"] + xgk[s], writes=pk(1, 2))

                                    def f_si(e, sab=sab):
                                        e.activation(out=sab[:, 0:512], in_=PS[:, 1, :], func=AF.Silu)
                                        return e.activation(out=sab[:, 512:544], in_=PS[:, 2, 0:32], func=AF.Silu)
                                    S.op("act", f_si, reads=pk(1, 2), writes=[sak])
                                    S.op("pe", lambda e, Wp_=W3p, f4=f4, s=s: f_a(e, Wp_, f4, s, 3, 4), reads=[f"WP{s3}"] + xgk[s], writes=pk(3, 4))

                                    def f_mu(e, sab=sab, s=s, fc=fc):
                                        e.tensor_tensor(out=gT[s][:, fc, 0:512], in0=PS[:, 3, :], in1=sab[:, 0:512], op=ALU.mult)
                                        return e.tensor_tensor(out=gT[s][:, fc, 512:544], in0=PS[:, 4, 0:32], in1=sab[:, 512:544], op=ALU.mult)
                                    S.op("dve", f_mu, reads=pk(3, 4) + [sak], writes=[f"gT{s}_{fc}"])
                            if fg < 2:
                                issue_piece(ee, 8 + 2 * fg)
                                issue_piece(ee, 9 + 2 * fg)
                            elif ee + 1 < NE:
                                issue_piece(ee + 1, 2 * (fg - 2))
                                issue_piece(ee + 1, 2 * (fg - 2) + 1)
                        if dbg_exp and ee == 0:
                            S.dma("pool", lambda e: e.dma_start(out=dbg_d[1, 256:384, 0:544], in_=gT[0][:, 0, :]), "de2", reads=["gT0_0"], writes=["dbge2"])
                        w2s = [piece_slot[(ee, 8 + q)] for q in range(4)]
                        W2p = [WP[sl][:].rearrange("p (fc n) -> p fc n", fc=4) for sl in w2s]
                        for s in range(NS):
                            for k in range(5):
                                rows = 128 if k < 4 else 32
                                r = s if k < 4 else 2
                                for half in range(2):
                                    def f_y(e, s=s, k=k, rows=rows, half=half):
                                        ins = None
                                        for fc in range(16):
                                            ins = e.matmul(PS[0:rows, 5 + half, :], lhsT=gT[s][:, fc, k * 128:k * 128 + rows],
                                                           rhs=W2p[fc // 4][:, fc % 4, half * 512:(half + 1) * 512], start=(fc == 0), stop=(fc == 15))
                                        return ins
                                    S.op("pe", f_y, reads=[f"gT{s}_{fc}" for fc in range(16)] + [f"WP{sl}" for sl in w2s], writes=pk(5 + half))
                                yb = ysb[state["y"] % 2]
                                yk = f"ysb{state['y'] % 2}"
                                state["y"] += 1
                                gate_ap = GAT[:, s, ee, k:k + 1] if k < 4 else GATC[0:32, s, ee:ee + 1]
                                S.op("dve", lambda e, yb=yb, rows=rows, gate_ap=gate_ap, r=r: e.scalar_tensor_tensor(
                                    out=yb[0:rows, :].rearrange("p (a b) -> p a b", a=2), in0=PS[0:rows, 5:7, :], scalar=gate_ap,
                                    in1=G2B[0:rows, r, :].rearrange("p (a b) -> p a b", a=2), op0=ALU.mult, op1=ALU.mult),
                                    reads=pk(5, 6) + ["GAT", "GATC"], writes=[yk])
                                idx_ap = IDX[:, s, ee, k:k + 1] if k < 4 else IDXC[0:32, s, ee:ee + 1]
                                if dbg_exp and ee == 0 and s == 0 and k == 0:
                                    S.dma("sp", lambda e, yb=yb: e.dma_start(out=dbg_d[1, 384:512, :], in_=yb[:]), "de3", reads=[yk], writes=["dbge3"])
                                S.dma("pool", lambda e, s=s, yb=yb, rows=rows, idx_ap=idx_ap: e.indirect_dma_start(
                                    out=XRflat, out_offset=bass.IndirectOffsetOnAxis(ap=idx_ap, axis=0), in_=yb[0:rows, :], in_offset=None, compute_op=ALU.add),
                                    f"scat{s}", reads=[yk, "IDX", "IDXC"], writes=[f"XRs{s}_{k}"], group=(k > 0))
                        if ee + 1 < NE:
                            for i in range(4, 8):
                                issue_piece(ee + 1, i)

                    for ee in range(NE):
                        do_expert(ee)
                    S.barrier()
                    S.emit()

        if dbg:
            for s in range(1 if (dbg_route or dbg_exp) else NS):
                S.dma("sp", lambda e, s=s: e.dma_start(out=dbg_d[s], in_=XR[s]), f"dbg{s}", writes=[f"dbg{s}"])
        with ExitStack() as ph:
            xt = [sbt(ph, f"fx{i}", [128, D], F32) for i in range(2)]
            yo = [sbt(ph, f"fy{i}", [128, D], F32) for i in range(2)]
            junk = sbt(ph, "fjunk", [128, D], BF16)
            gfin = sbt(ph, "gfin", [128, D], F32)
            S.dma("sp", lambda e: e.dma_start(out=gfin[:], in_=g_final.rearrange("(o d) -> o d", o=1).to_broadcast([128, D])),
                  "c7", writes=["gfin"])
            ss = sbt(ph, "fss", [128, 1], F32)
            rt = sbt(ph, "frt", [128, 1], F32)
            rstd = sbt(ph, "frstd", [128, 1], F32)
            cnt = 0
            for s in range(NS):
                for tile in range(NLT):
                    b = cnt % 2
                    cnt += 1
                    rows = slice(tile * 128, (tile + 1) * 128)
                    S.dma("sp", lambda e, s=s, rows=rows, b=b: e.dma_start(out=xt[b][:], in_=XR[s, rows, :]), f"fx{b}", writes=[f"fx{b}"])
                    S.op("act", lambda e, b=b: e.activation(out=junk[:], in_=xt[b][:], func=AF.Square, accum_out=ss[:]), reads=[f"fx{b}"], writes=["fjunk", "fss"])
                    S.op("act", lambda e: e.activation(out=rt[:], in_=ss[:], func=AF.Sqrt, scale=1.0 / D, bias=epst[:, 0:1]), reads=["fss"], writes=["frt"])
                    S.op("dve", lambda e: e.reciprocal(out=rstd[:], in_=rt[:]), reads=["frt"], writes=["frstd"])
                    S.op("dve", lambda e, b=b: e.scalar_tensor_tensor(out=yo[b][:], in0=xt[b][:], scalar=rstd[:, 0:1], in1=gfin[:], op0=ALU.mult, op1=ALU.mult),
                         reads=[f"fx{b}", "frstd", "gfin"], writes=[f"fy{b}"])
                    S.dma("sp", lambda e, s=s, rows=rows, b=b: e.dma_start(out=out_d[s, rows, :], in_=yo[b][:]), f"fy{b}", reads=[f"fy{b}"], writes=["out"])
            S.barrier()
            S.emit()
    return nc


def host_consts():
    t = np.arange(SEQ)
    row = (t // 64).astype(np.float32)
    col = (t % 64).astype(np.float32)
    inv = (10000.0 ** (-np.arange(16, dtype=np.float32) / 16)).astype(np.float32)
    ang = np.concatenate([row[:, None] * inv, col[:, None] * inv], axis=-1).astype(np.float32)
    p = np.arange(128)
    pt = np.zeros((128, NT, 2), np.float32)
    pt[:, :, 0] = p[:, None]
    pt[:, :, 1] = np.arange(NT)[None, :]
    return dict(
        k_ident=np.eye(128, dtype=np.float32),
        k_cos=np.cos(ang).astype(np.float32),
        k_sin=np.sin(ang).astype(np.float32),
        k_upper=(p[:, None] < p[None, :]).astype(np.float32),
        k_iota=np.broadcast_to(np.arange(128, dtype=np.float32)[None, :], (128, 128)).copy(),
        k_pt=pt,
        k_ml=(p[:, None] >= p[None, :]).astype(np.float32),
        k_mr=(p[:, None] <= p[None, :]).astype(np.float32),
    )


_NC_CACHE = {}


def kernel(**inputs):
    n = 8
    if "nc" not in _NC_CACHE:
        _NC_CACHE["nc"] = build()
    nc = _NC_CACHE["nc"]
    consts = host_consts()
    shared = {k: np.ascontiguousarray(np.asarray(v, dtype=np.float32)) for k, v in inputs.items() if k not in ("x", "c", "ctx")}
    x = np.asarray(inputs["x"], dtype=np.float32)
    c = np.asarray(inputs["c"], dtype=np.float32)
    ctx = np.asarray(inputs["ctx"], dtype=np.float32)
    in_maps = []
    for i in range(n):
        m = dict(shared)
        m.update(consts)
        m["x"] = np.ascontiguousarray(x[2 * i:2 * i + 2])
        m["c"] = np.ascontiguousarray(c[2 * i:2 * i + 2])
        m["ctx"] = np.ascontiguousarray(ctx[2 * i:2 * i + 2])
        in_maps.append(m)
    res = run_bass_kernel_spmd(nc, in_maps, core_ids=list(range(n)))
    return np.concatenate([r["out"] for r in res.results], axis=0).astype(np.float32)
```
